# Optimizing a Trainium2 kernel written in Bass

```python
import math
import jax, jax.numpy as jnp
from jax import lax
import numpy as np

D_MODEL = 1024
BATCH = 8
SEQ = 8192
DEPTH = 2

N_MEM = 256
N_EVEN = (DEPTH + 1) // 2
N_ODD = DEPTH // 2
EPS = 1e-6

RET_HEADS = 6
RET_DK = 64
RET_DV = 128
RET_QK = RET_HEADS * RET_DK
RET_V = RET_HEADS * RET_DV
RET_CHUNK = 128
ROPE_THETA = 10000.0

S5_WIDTH = D_MODEL // 4
S5_GROUP = 16
S5_GROUPS = S5_WIDTH // S5_GROUP
S5_STATE = 64

EVEN_IN = 2 * RET_QK + 2 * RET_V + S5_WIDTH
EVEN_MIX = RET_V + S5_WIDTH

M2_DINNER = 2 * D_MODEL
M2_HEADDIM = 64
M2_HEADS = M2_DINNER // M2_HEADDIM
M2_GROUPS = 4
M2_HPG = M2_HEADS // M2_GROUPS
M2_STATE = 128
M2_CONV = 4
M2_CHUNK = 128
M2_CONV_DIM = M2_DINNER + 2 * M2_GROUPS * M2_STATE
M2_IN = M2_DINNER + M2_CONV_DIM + M2_HEADS

XA_HEADS = 4
XA_HEAD_DIM = D_MODEL // XA_HEADS

FFN_DENSE = 2816
N_EXPERTS = 8
TOP_K = 2
FFN_EXPERT = 3584
MOE_BLOCK = 1024

F32 = jnp.float32

kernel_name = 'hybrid_retention_s5_mamba2_moe'


def rmsnorm(x, w):
    xf = x.astype(F32)
    y = xf * lax.rsqrt(jnp.mean(xf * xf, axis=-1, keepdims=True) + EPS)
    return (y * w.astype(F32)).astype(x.dtype)


def to_chunks(a, size):
    b, l = a.shape[:2]
    a = a.reshape((b, l // size, size) + a.shape[2:])
    return jnp.moveaxis(a, 1, 0)


def from_chunks(a):
    nc, b, size = a.shape[:3]
    return jnp.moveaxis(a, 0, 1).reshape((b, nc * size) + a.shape[3:])


def rotary(a, positions):
    half = a.shape[-1] // 2
    inv_freq = ROPE_THETA ** (-jnp.arange(half, dtype=F32) / half)
    ang = positions.astype(F32)[:, :, None, None] * inv_freq
    cos, sin = jnp.cos(ang), jnp.sin(ang)
    a1, a2 = a[..., :half], a[..., half:]
    return jnp.concatenate([a1 * cos - a2 * sin, a1 * sin + a2 * cos], axis=-1)


def retention(q, k, v):
    b = q.shape[0]
    T = RET_CHUNK
    log_gamma = jnp.log1p(-(2.0 ** (-5.0 - jnp.arange(RET_HEADS, dtype=F32))))
    idx = jnp.arange(T, dtype=F32)
    diff = idx[:, None] - idx[None, :]
    decay_inner = jnp.where(diff[None] >= 0,
                            jnp.exp(jnp.maximum(diff, 0.0)[None] * log_gamma[:, None, None]), 0.0)
    decay_cross = jnp.exp((idx[:, None] + 1.0) * log_gamma)
    decay_state = jnp.exp((T - 1.0 - idx)[:, None] * log_gamma)
    decay_chunk = jnp.exp(T * log_gamma)

    def step(state, inp):
        qc, kc, vc = inp
        scores = jnp.einsum('bthd,bshd->bhts', qc, kc) * decay_inner[None]
        y = jnp.einsum('bhts,bshv->bthv', scores, vc)
        y = y + jnp.einsum('bthd,bhdv->bthv', qc, state) * decay_cross[None, :, :, None]
        state = state * decay_chunk[None, :, None, None] + jnp.einsum(
            'bshd,bshv->bhdv', kc * decay_state[None, :, :, None], vc)
        return state, y

    state0 = jnp.zeros((b, RET_HEADS, RET_DK, RET_DV), F32)
    _, y = lax.scan(step, state0, (to_chunks(q, T), to_chunks(k, T), to_chunks(v, T)))
    return from_chunks(y)


def s5_layer(u, lam_re, lam_im, log_dt, b_re, b_im, c_re, c_im, d_skip, w_glu, b_glu):
    bsz, l = u.shape[:2]
    uf = u.astype(F32).reshape(bsz, l, S5_GROUPS, S5_GROUP)
    dt = jnp.exp(log_dt.astype(F32))[:, None]
    lr, li = lam_re.astype(F32), lam_im.astype(F32)
    mag = jnp.exp(lr * dt)
    ab_re, ab_im = mag * jnp.cos(li * dt), mag * jnp.sin(li * dt)
    den = lr * lr + li * li
    nr, ni = ab_re - 1.0, ab_im
    f_re = (nr * lr + ni * li) / den
    f_im = (ni * lr - nr * li) / den
    br, bi = b_re.astype(F32), b_im.astype(F32)
    bb_re = f_re[..., None] * br - f_im[..., None] * bi
    bb_im = f_re[..., None] * bi + f_im[..., None] * br
    xr = jnp.einsum('blgc,gpc->lbgp', uf, bb_re)
    xi = jnp.einsum('blgc,gpc->lbgp', uf, bb_im)
    ar = jnp.broadcast_to(ab_re[None, None], xr.shape)
    ai = jnp.broadcast_to(ab_im[None, None], xr.shape)

    def combine(e1, e2):
        a1r, a1i, b1r, b1i = e1
        a2r, a2i, b2r, b2i = e2
        return (a2r * a1r - a2i * a1i, a2r * a1i + a2i * a1r,
                a2r * b1r - a2i * b1i + b2r, a2r * b1i + a2i * b1r + b2i)

    _, _, sr, si = lax.associative_scan(combine, (ar, ai, xr, xi), axis=0)
    y = jnp.einsum('lbgp,gcp->blgc', sr, c_re.astype(F32)) - jnp.einsum('lbgp,gcp->blgc', si, c_im.astype(F32))
    y = y.reshape(bsz, l, S5_WIDTH) + d_skip.astype(F32) * uf.reshape(bsz, l, S5_WIDTH)
    y = jax.nn.gelu(y)
    y = y * jax.nn.sigmoid(jnp.dot(y, w_glu.astype(F32)) + b_glu.astype(F32))
    return y.astype(u.dtype)


def even_mixer(h, positions, w_in, lam_re, lam_im, log_dt, b_re, b_im, c_re, c_im,
               d_skip, w_glu, b_glu, w_out):
    b, l, _ = h.shape
    proj = jnp.dot(h, w_in)
    q, k, v, g, u = jnp.split(
        proj, [RET_QK, 2 * RET_QK, 2 * RET_QK + RET_V, 2 * RET_QK + 2 * RET_V], axis=-1)
    q = rotary(q.reshape(b, l, RET_HEADS, RET_DK).astype(F32), positions)
    k = rotary(k.reshape(b, l, RET_HEADS, RET_DK).astype(F32), positions) * (RET_DK ** -0.5)
    v = v.reshape(b, l, RET_HEADS, RET_DV).astype(F32)
    y = retention(q, k, v)
    y = y * lax.rsqrt(jnp.mean(y * y, axis=-1, keepdims=True) + EPS)
    y_ret = (y.reshape(b, l, RET_V) * jax.nn.silu(g.astype(F32))).astype(h.dtype)
    y_s5 = s5_layer(u, lam_re, lam_im, log_dt, b_re, b_im, c_re, c_im, d_skip, w_glu, b_glu)
    return jnp.dot(jnp.concatenate([y_ret, y_s5], axis=-1), w_out)


def ssd_chunked(xdt, adt, bm, cm):
    b = xdt.shape[0]
    T = M2_CHUNK
    mask = jnp.tril(jnp.ones((T, T), dtype=bool))[None, :, :, None, None]

    def step(state, inp):
        xc, ac, bc, cc = inp
        acum = jnp.cumsum(ac, axis=1)
        seg = acum[:, :, None] - acum[:, None, :]
        L = jnp.exp(jnp.where(mask, seg, -jnp.inf))
        cb = jnp.einsum('btgn,bsgn->bgts', cc, bc)
        y_diag = jnp.einsum('bgts,btsgk,bsgkp->btgkp', cb, L, xc)
        y_off = jnp.einsum('btgn,bgkpn->btgkp', cc, state) * jnp.exp(acum)[..., None]
        decay = jnp.exp(acum[:, -1:] - acum)
        state = state * jnp.exp(acum[:, -1])[..., None, None] + jnp.einsum(
            'bsgn,bsgk,bsgkp->bgkpn', bc, decay, xc)
        return state, y_diag + y_off

    state0 = jnp.zeros((b, M2_GROUPS, M2_HPG, M2_HEADDIM, M2_STATE), F32)
    _, y = lax.scan(step, state0, (to_chunks(xdt, T), to_chunks(adt, T), to_chunks(bm, T), to_chunks(cm, T)))
    return from_chunks(y)


def mamba2_mixer(h, w_in, conv_w, conv_b, dt_bias, a_log, d_skip, norm_w, w_out):
    b, l, _ = h.shape
    proj = jnp.dot(h, w_in)
    z, xbc, dt = jnp.split(proj, [M2_DINNER, M2_DINNER + M2_CONV_DIM], axis=-1)
    xbc = lax.conv_general_dilated(xbc, conv_w[:, None, :], window_strides=(1,),
                                   padding=((M2_CONV - 1, 0),),
                                   dimension_numbers=('NWC', 'WIO', 'NWC'),
                                   feature_group_count=M2_CONV_DIM) + conv_b
    xbc = jax.nn.silu(xbc)
    xs, bm, cm = jnp.split(xbc, [M2_DINNER, M2_DINNER + M2_GROUPS * M2_STATE], axis=-1)
    xs = xs.reshape(b, l, M2_GROUPS, M2_HPG, M2_HEADDIM).astype(F32)
    bm = bm.reshape(b, l, M2_GROUPS, M2_STATE).astype(F32)
    cm = cm.reshape(b, l, M2_GROUPS, M2_STATE).astype(F32)
    dt = jax.nn.softplus(dt.astype(F32) + dt_bias.astype(F32)).reshape(b, l, M2_GROUPS, M2_HPG)
    a = -jnp.exp(a_log.astype(F32)).reshape(M2_GROUPS, M2_HPG)
    y = ssd_chunked(xs * dt[..., None], dt * a, bm, cm)
    y = y + d_skip.astype(F32).reshape(M2_GROUPS, M2_HPG)[:, :, None] * xs
    y = y.reshape(b, l, M2_DINNER) * jax.nn.silu(z.astype(F32))
    y = rmsnorm(y, norm_w).astype(h.dtype)
    return jnp.dot(y, w_out)


def cross_attention(h, mem_n, wq, wk, wv, wo):
    b, l, _ = h.shape
    q = jnp.dot(h, wq).reshape(b, l, XA_HEADS, XA_HEAD_DIM).astype(F32)
    k = jnp.dot(mem_n, wk).reshape(b, N_MEM, XA_HEADS, XA_HEAD_DIM).astype(F32)
    v = jnp.dot(mem_n, wv).reshape(b, N_MEM, XA_HEADS, XA_HEAD_DIM).astype(F32)
    p = jax.nn.softmax(jnp.einsum('bthd,bshd->bhts', q, k) * (XA_HEAD_DIM ** -0.5), axis=-1)
    o = jnp.einsum('bhts,bshd->bthd', p, v).reshape(b, l, D_MODEL).astype(h.dtype)
    return jnp.dot(o, wo)


def swiglu(h, w1, w3, w2):
    return jnp.dot(jax.nn.silu(jnp.dot(h, w1)) * jnp.dot(h, w3), w2)


def moe_swiglu(h, router, w1, w3, w2):
    b, l, d = h.shape
    n_tok = b * l
    blk = math.gcd(n_tok, MOE_BLOCK)
    ht = h.reshape(n_tok // blk, blk, d)

    def block(hb):
        logits = jnp.dot(hb, router).astype(F32)
        top_v, top_i = lax.top_k(logits, TOP_K)
        gates = jax.nn.softmax(top_v, axis=-1)
        comb = jnp.einsum('tk,tke->te', gates, jax.nn.one_hot(top_i, N_EXPERTS, dtype=F32))
        a = jnp.einsum('td,edf->etf', hb, w1)
        c = jnp.einsum('td,edf->etf', hb, w3)
        e_out = jnp.einsum('etf,efd->etd', jax.nn.silu(a) * c, w2)
        return jnp.einsum('te,etd->td', comb.astype(hb.dtype), e_out)

    return lax.map(block, ht).reshape(b, l, d)


def setup_inputs(seed: int = 0) -> dict:
    key = jax.random.key(seed)
    ks = iter(jax.random.split(key, 64))

    def nrm(shape, scale):
        return jax.random.normal(next(ks), shape, F32) * scale

    def gain(shape):
        return 1.0 + nrm(shape, 0.02)

    x = nrm((BATCH, SEQ, D_MODEL), 1.0)
    mem = nrm((BATCH, N_MEM, D_MODEL), 1.0)
    positions = (jax.random.randint(next(ks), (BATCH, 1), 0, 4096, dtype=jnp.int32)
                 + jnp.arange(SEQ, dtype=jnp.int32)[None, :])
    dt_m2 = jnp.exp(jax.random.uniform(next(ks), (N_ODD, M2_HEADS), F32, math.log(0.001), math.log(0.1)))
    return {
        'x': x,
        'mem': mem,
        'positions': positions,
        'mem_norm': gain((D_MODEL,)),
        'norm_mix': gain((DEPTH, D_MODEL)),
        'norm_xattn': gain((DEPTH, D_MODEL)),
        'norm_ffn': gain((DEPTH, D_MODEL)),
        'xa_wq': nrm((DEPTH, D_MODEL, D_MODEL), D_MODEL ** -0.5),
        'xa_wk': nrm((DEPTH, D_MODEL, D_MODEL), D_MODEL ** -0.5),
        'xa_wv': nrm((DEPTH, D_MODEL, D_MODEL), D_MODEL ** -0.5),
        'xa_wo': nrm((DEPTH, D_MODEL, D_MODEL), D_MODEL ** -0.5),
        'ev_w_in': nrm((N_EVEN, D_MODEL, EVEN_IN), D_MODEL ** -0.5),
        'ev_s5_lam_re': -0.5 + nrm((N_EVEN, S5_GROUPS, S5_STATE), 0.01),
        'ev_s5_lam_im': math.pi * jnp.arange(S5_STATE, dtype=F32) + nrm((N_EVEN, S5_GROUPS, S5_STATE), 0.01),
        'ev_s5_log_dt': jax.random.uniform(next(ks), (N_EVEN, S5_GROUPS), F32, math.log(0.001), math.log(0.1)),
        'ev_s5_b_re': nrm((N_EVEN, S5_GROUPS, S5_STATE, S5_GROUP), (2 * S5_GROUP) ** -0.5),
        'ev_s5_b_im': nrm((N_EVEN, S5_GROUPS, S5_STATE, S5_GROUP), (2 * S5_GROUP) ** -0.5),
        'ev_s5_c_re': nrm((N_EVEN, S5_GROUPS, S5_GROUP, S5_STATE), (2 * S5_STATE) ** -0.5),
        'ev_s5_c_im': nrm((N_EVEN, S5_GROUPS, S5_GROUP, S5_STATE), (2 * S5_STATE) ** -0.5),
        'ev_s5_d': nrm((N_EVEN, S5_WIDTH), 1.0),
        'ev_s5_w_glu': nrm((N_EVEN, S5_WIDTH, S5_WIDTH), S5_WIDTH ** -0.5),
        'ev_s5_b_glu': nrm((N_EVEN, S5_WIDTH), 0.01),
        'ev_w_out': nrm((N_EVEN, EVEN_MIX, D_MODEL), EVEN_MIX ** -0.5),
        'ev_ffn_w1': nrm((N_EVEN, D_MODEL, FFN_DENSE), D_MODEL ** -0.5),
        'ev_ffn_w3': nrm((N_EVEN, D_MODEL, FFN_DENSE), D_MODEL ** -0.5),
        'ev_ffn_w2': nrm((N_EVEN, FFN_DENSE, D_MODEL), FFN_DENSE ** -0.5),
        'od_w_in': nrm((N_ODD, D_MODEL, M2_IN), D_MODEL ** -0.5),
        'od_conv_w': nrm((N_ODD, M2_CONV, M2_CONV_DIM), M2_CONV ** -0.5),
        'od_conv_b': nrm((N_ODD, M2_CONV_DIM), 0.01),
        'od_dt_bias': dt_m2 + jnp.log(-jnp.expm1(-dt_m2)),
        'od_a_log': jnp.log(jax.random.uniform(next(ks), (N_ODD, M2_HEADS), F32, 1.0, 16.0)),
        'od_d': 1.0 + nrm((N_ODD, M2_HEADS), 0.1),
        'od_norm': gain((N_ODD, M2_DINNER)),
        'od_w_out': nrm((N_ODD, M2_DINNER, D_MODEL), M2_DINNER ** -0.5),
        'od_router': nrm((N_ODD, D_MODEL, N_EXPERTS), D_MODEL ** -0.5),
        'od_moe_w1': nrm((N_ODD, N_EXPERTS, D_MODEL, FFN_EXPERT), D_MODEL ** -0.5),
        'od_moe_w3': nrm((N_ODD, N_EXPERTS, D_MODEL, FFN_EXPERT), D_MODEL ** -0.5),
        'od_moe_w2': nrm((N_ODD, N_EXPERTS, FFN_EXPERT, D_MODEL), FFN_EXPERT ** -0.5),
        'final_norm': gain((D_MODEL,)),
    }


def reference(x, mem, positions, mem_norm, norm_mix, norm_xattn, norm_ffn,
              xa_wq, xa_wk, xa_wv, xa_wo,
              ev_w_in, ev_s5_lam_re, ev_s5_lam_im, ev_s5_log_dt, ev_s5_b_re, ev_s5_b_im,
              ev_s5_c_re, ev_s5_c_im, ev_s5_d, ev_s5_w_glu, ev_s5_b_glu, ev_w_out,
              ev_ffn_w1, ev_ffn_w3, ev_ffn_w2,
              od_w_in, od_conv_w, od_conv_b, od_dt_bias, od_a_log, od_d, od_norm, od_w_out,
              od_router, od_moe_w1, od_moe_w3, od_moe_w2, final_norm):
    mem_n = rmsnorm(mem, mem_norm)
    h = x
    for layer in range(DEPTH):
        i = layer // 2
        hn = rmsnorm(h, norm_mix[layer])
        if layer % 2 == 0:
            h = h + even_mixer(hn, positions, ev_w_in[i], ev_s5_lam_re[i], ev_s5_lam_im[i],
                               ev_s5_log_dt[i], ev_s5_b_re[i], ev_s5_b_im[i], ev_s5_c_re[i],
                               ev_s5_c_im[i], ev_s5_d[i], ev_s5_w_glu[i], ev_s5_b_glu[i], ev_w_out[i])
        else:
            h = h + mamba2_mixer(hn, od_w_in[i], od_conv_w[i], od_conv_b[i], od_dt_bias[i],
                                 od_a_log[i], od_d[i], od_norm[i], od_w_out[i])
        h = h + cross_attention(rmsnorm(h, norm_xattn[layer]), mem_n,
                                xa_wq[layer], xa_wk[layer], xa_wv[layer], xa_wo[layer])
        hn = rmsnorm(h, norm_ffn[layer])
        if layer % 2 == 0:
            h = h + swiglu(hn, ev_ffn_w1[i], ev_ffn_w3[i], ev_ffn_w2[i])
        else:
            h = h + moe_swiglu(hn, od_router[i], od_moe_w1[i], od_moe_w3[i], od_moe_w2[i])
    return rmsnorm(h, final_norm)
```

```python
import math
from contextlib import ExitStack
import numpy as np
import concourse.bass as bass
import concourse.mybir as mybir
from concourse.bass_utils import run_bass_kernel_spmd

F32 = mybir.dt.float32
BF16 = mybir.dt.bfloat16
I32 = mybir.dt.int32
AF = mybir.ActivationFunctionType
ALU = mybir.AluOpType
AX = mybir.AxisListType

SAME_ENGINE_SYNC = {"pe": False, "dve": True, "act": False, "pool": True, "sp": False}
STRICT_ELEMS = 256
N_DMA_SLOTS = 8
SCHEDULE = True


class Buf:
    def __init__(self, h, name, strict=False):
        self.h = h
        self.name = name
        self.writers = {}
        self.readers = {}
        self.strict = strict

    def __getitem__(self, idx):
        return self.h[idx]


class _CallRec:
    def __getattr__(self, name):
        def f(*a, **k):
            self.__dict__["call"] = (name, a, k)
            return self
        return f


class Prog:
    def __init__(self, nc):
        self.nc = nc
        self.es = ExitStack()
        self.es_global = self.es
        self._consts = {}
        self.eng = {"pe": nc.tensor, "dve": nc.vector, "act": nc.scalar, "pool": nc.gpsimd, "sp": nc.sync}
        self.sem = {}
        self.cnt = {}
        self.waited = {}
        for e in self.eng:
            self.sem[e] = self.es.enter_context(nc.semaphore("s_" + e))
            self.cnt[e] = 0
            self.waited[e] = {}
        self.dma_sem = {}
        self.dma_cnt = {}
        for q in ("sp", "act", "pool"):
            self.dma_sem[q] = [self.es.enter_context(nc.semaphore("d_%s%d" % (q, i))) for i in range(N_DMA_SLOTS)]
            self.dma_cnt[q] = 0
        self.semobj = dict(self.sem)
        for q in self.dma_sem:
            for i, s in enumerate(self.dma_sem[q]):
                self.semobj[("dma", q, i)] = s
        self._psum = []
        self._psum_i = 0
        self._ident = None
        self.n_inst = 0
        self.n_wait = 0
        self.uid = 0
        self._rec = None
        self.psum_banks()
        self.identity_bf16()
        for cv in (1e-6, 1.0, 0.0, -math.pi, math.pi, 0.5, 0.25):
            self.const(cv)

    def sbuf(self, name, shape, dtype):
        self.uid += 1
        h = self.es.enter_context(self.nc.sbuf_tensor("%s_%d" % (name, self.uid), list(shape), dtype))
        return Buf(h, name, strict=(int(np.prod(shape[1:])) <= STRICT_ELEMS))

    def psum(self, name, shape, dtype=F32):
        self.uid += 1
        h = self.es.enter_context(self.nc.psum_tensor("%s_%d" % (name, self.uid), list(shape), dtype))
        return Buf(h, name)

    def dram(self, name, shape, dtype, kind="Internal"):
        h = self.nc.dram_tensor(name, list(shape), dtype, kind=kind)
        return Buf(h.ap(), name)

    def pool_of(self, name, shape, dtype, n, space="sbuf"):
        bufs = [(self.sbuf if space == "sbuf" else self.psum)("%s%d" % (name, i), shape, dtype) for i in range(n)]
        return Rot(bufs)

    N_FBANK = 6

    def psum_banks(self):
        if not self._psum:
            g = self.es_global
            self._psum = [Buf(g.enter_context(self.nc.psum_tensor("bank%d" % i, [128, 512], F32)), "bank") for i in range(self.N_FBANK)]
            self._psumt = [Buf(g.enter_context(self.nc.psum_tensor("tbank%d" % i, [128, 1024], BF16)), "tbank") for i in range(8 - self.N_FBANK)]
            self._psumt_i = 0
        return self._psum

    def psum_bank(self):
        banks = self.psum_banks()
        b = banks[self._psum_i % len(banks)]
        self._psum_i += 1
        return b

    def psum_tbank(self):
        self.psum_banks()
        b = self._psumt[self._psumt_i % len(self._psumt)]
        self._psumt_i += 1
        return b

    def _deps(self, reads, writes):
        deps = {}
        self._strict_keys = set()
        for b in reads:
            for k, v in b.writers.items():
                if deps.get(k, 0) < v:
                    deps[k] = v
                if b.strict:
                    self._strict_keys.add(k)
        for b in writes:
            for d in (b.writers, b.readers):
                for k, v in d.items():
                    if deps.get(k, 0) < v:
                        deps[k] = v
                    if b.strict:
                        self._strict_keys.add(k)
        return deps

    def _emit_waits(self, e, deps):
        eng = self.eng[e]
        w = self.waited[e]
        for k, v in deps.items():
            if k == e and not SAME_ENGINE_SYNC[e]:
                continue
            if k == e and SAME_ENGINE_SYNC[e] == "strict" and k not in self._strict_keys:
                continue
            if w.get(k, 0) >= v:
                continue
            eng.wait_ge(self.semobj[k], v)
            w[k] = v
            self.n_wait += 1

    def _record(self, key, val, reads, writes):
        for b in reads:
            if b.readers.get(key, 0) < val:
                b.readers[key] = val
        for b in writes:
            if b.readers:
                b.writers = {key: val}
                b.readers = {}
            else:
                b.writers[key] = val

    def op(self, e, fn, reads=(), writes=(), cost=None):
        if self._rec is not None:
            r = _CallRec()
            fn(r)
            name, a, k = r.call
            self._rec.append((e, (lambda eng, name=name, a=a, k=k: getattr(eng, name)(*a, **k)), tuple(reads), tuple(writes), None, cost))
            return
        deps = self._deps(reads, writes)
        self._emit_waits(e, deps)
        inst = fn(self.eng[e])
        self.cnt[e] += 1
        inst.then_inc(self.sem[e], 1)
        self._record(e, self.cnt[e], reads, writes)
        self.n_inst += 1

    def pe(self, fn, reads=(), writes=()):
        self.op("pe", fn, reads, writes)

    def dve(self, fn, reads=(), writes=()):
        self.op("dve", fn, reads, writes)

    def act(self, fn, reads=(), writes=()):
        self.op("act", fn, reads, writes)

    def gp(self, fn, reads=(), writes=()):
        self.op("pool", fn, reads, writes)

    def dma(self, out, in_, reads=(), writes=(), q="sp", **kw):
        if self._rec is not None:
            self._rec.append((q, None, tuple(reads), tuple(writes), (out, in_, kw), None))
            return
        k = self.dma_cnt[q]
        self.dma_cnt[q] += 1
        slot = k % N_DMA_SLOTS
        key = ("dma", q, slot)
        deps = self._deps(reads, writes)
        prev = 16 * (k // N_DMA_SLOTS)
        if prev > 0:
            deps[key] = max(deps.get(key, 0), prev)
        self._emit_waits(q, deps)
        inst = self.eng[q].dma_start(out=out, in_=in_, **kw)
        inst.then_inc(self.semobj[key], 16)
        self._record(key, prev + 16, reads, writes)
        self.n_inst += 1

    def identity_bf16(self):
        if self._ident is None:
            idf = Buf(self.es_global.enter_context(self.nc.sbuf_tensor("identf", [128, 128], F32)), "identf")
            idb = Buf(self.es_global.enter_context(self.nc.sbuf_tensor("identb", [128, 128], BF16)), "identb")
            self.gp(lambda e: e.memset(idf[:], 1.0), writes=[idf])
            self.gp(lambda e: e.affine_select(idf[:], idf[:], [[-1, 128]], ALU.is_equal, 0.0, base=0, channel_multiplier=1),
                    reads=[idf], writes=[idf])
            self.gp(lambda e: e.tensor_copy(idb[:], idf[:]), reads=[idf], writes=[idb])
            self._ident = idb
            self._identf = idf
        return self._ident

    EST = {"pe": 0.25, "act": 0.55, "dve": 0.65, "pool": 1.2, "sp": 0.15}
    LAT = {"pe": 0.3, "act": 0.5, "dve": 0.5, "pool": 0.8, "sp": 2.5}

    def begin_sched(self):
        if SCHEDULE:
            self._rec = []

    def end_sched(self):
        rec = self._rec
        self._rec = None
        if not rec:
            return
        n = len(rec)
        preds = [None] * n
        lastw = {}
        readers = {}
        for i, (e, fn, reads, writes, dmaargs, cost) in enumerate(rec):
            p = set()
            for b in reads:
                w = lastw.get(id(b))
                if w is not None:
                    p.update(w)
            for b in writes:
                w = lastw.get(id(b))
                if w is not None:
                    p.update(w)
                r = readers.get(id(b))
                if r:
                    p.update(r)
            p.discard(i)
            preds[i] = p
            for b in reads:
                readers.setdefault(id(b), []).append(i)
            for b in writes:
                if readers.get(id(b)):
                    lastw[id(b)] = [i]
                    readers[id(b)] = []
                else:
                    lastw.setdefault(id(b), []).append(i)
        succs = [[] for _ in range(n)]
        npred = [0] * n
        for i in range(n):
            npred[i] = len(preds[i])
            for j in preds[i]:
                succs[j].append(i)
        import heapq
        fin = [0.0] * n
        rdy = [0.0] * n
        efree = {}
        ready = {}
        for i in range(n):
            if npred[i] == 0:
                ready.setdefault(rec[i][0], []).append(i)
        for e in ready:
            heapq.heapify(ready[e])
        done = 0
        while done < n:
            best, beste, bestt = None, None, None
            for e, hq in ready.items():
                if not hq:
                    continue
                i = hq[0]
                t = max(efree.get(e, 0.0), rdy[i])
                if bestt is None or t < bestt or (t == bestt and i < best):
                    best, beste, bestt = i, e, t
            i = heapq.heappop(ready[beste])
            e, fn, reads, writes, dmaargs, cost = rec[i]
            c = cost if cost is not None else self.EST[e]
            efree[e] = bestt + c
            fin[i] = bestt + c + self.LAT[e]
            if dmaargs is None:
                self.op(e, fn, reads, writes)
            else:
                self.dma(dmaargs[0], dmaargs[1], reads=reads, writes=writes, q=e, **dmaargs[2])
            done += 1
            for j in succs[i]:
                npred[j] -= 1
                if rdy[j] < fin[i]:
                    rdy[j] = fin[i]
                if npred[j] == 0:
                    heapq.heappush(ready.setdefault(rec[j][0], []), j)

    def barrier(self):
        final = {}
        for e in self.eng:
            if self.cnt[e] > 0:
                final[e] = self.cnt[e]
        for q in self.dma_cnt:
            k = self.dma_cnt[q]
            for slot in range(N_DMA_SLOTS):
                n = (k - slot + N_DMA_SLOTS - 1) // N_DMA_SLOTS
                if n > 0:
                    final[("dma", q, slot)] = 16 * n
        for e in self.eng:
            self._emit_waits(e, {k: v for k, v in final.items() if k != e})

    def phase(self):
        return _Phase(self)

    def const(self, val):
        key = float(val)
        if key not in self._consts:
            c = Buf(self.es_global.enter_context(self.nc.sbuf_tensor("const_%d" % len(self._consts), [128, 1], F32)), "const")
            self.gp(lambda e: e.memset(c[:], key), writes=[c])
            self._consts[key] = c
        return self._consts[key]

    def finish(self):
        final = {}
        for e in self.eng:
            if self.cnt[e] > 0:
                final[e] = self.cnt[e]
        for q in self.dma_cnt:
            k = self.dma_cnt[q]
            for slot in range(N_DMA_SLOTS):
                n = (k - slot + N_DMA_SLOTS - 1) // N_DMA_SLOTS
                if n > 0:
                    final[("dma", q, slot)] = 16 * n
        self._emit_waits("sp", {k: v for k, v in final.items() if k != "sp"})
        self.es_global.close()


class _Phase:
    def __init__(self, P):
        self.P = P

    def __enter__(self):
        self.saved = self.P.es
        self.stack = ExitStack()
        self.P.es = self.stack
        return self

    def __exit__(self, *a):
        self.P.end_sched()
        self.P.barrier()
        self.P.es = self.saved
        self.stack.close()
        return False


class Rot:
    def __init__(self, bufs):
        self.bufs = bufs
        self.i = 0

    def next(self):
        b = self.bufs[self.i % len(self.bufs)]
        self.i += 1
        return b


EPS = 1e-6
D = 1024
KC = 8


class Ctx:
    def __init__(self, P, stage_w=2048, stage_n=3, junk_w=1024):
        self.P = P
        self.ident = P.identity_bf16()
        self.junk = P.pool_of("junk", [128, junk_w], BF16, 2)
        self.small = P.pool_of("small", [128, 4], F32, 4)
        self.xs = P.pool_of("xs", [128, 1024], BF16, 2)
        self.stage = P.pool_of("stage", [128, stage_w], F32, stage_n)
        self.stage_w = stage_w
        self.eps = P.const(EPS)


def load_gain(P, vec_ap, n):
    g = P.sbuf("gain", [128, n], F32)
    P.dma(g[:], vec_ap.rearrange("(c p) -> p c", p=128), writes=[g], allow_slow_non_contiguous=True)
    return g


def load_w(P, C, dst, src_ap, gain=None, kc0=0, nkc=None, f0=0, nf=None, dst_f0=0, eng="act"):
    K, F = src_ap.shape
    if nkc is None:
        nkc = K // 128
    if nf is None:
        nf = F
    FS = C.stage_w
    for kc in range(nkc):
        for c0 in range(0, nf, FS):
            n = min(FS, nf - c0)
            st = C.stage.next()
            P.dma(st[:, 0:n], src_ap[(kc0 + kc) * 128:(kc0 + kc + 1) * 128, f0 + c0:f0 + c0 + n], writes=[st])
            o = dst[:, kc, dst_f0 + c0:dst_f0 + c0 + n]
            if eng == "act":
                if gain is not None:
                    gap = gain[:, kc0 + kc:kc0 + kc + 1]
                    P.act(lambda e, o=o, st=st, n=n, gap=gap: e.activation(o, st[:, 0:n], AF.Copy, scale=gap), reads=[st, gain], writes=[dst])
                else:
                    P.act(lambda e, o=o, st=st, n=n: e.activation(o, st[:, 0:n], AF.Copy), reads=[st], writes=[dst])
            elif gain is not None:
                gap = gain[:, kc0 + kc:kc0 + kc + 1]
                P.op(eng, lambda e, o=o, st=st, n=n, gap=gap: e.tensor_scalar(o, st[:, 0:n], gap, None, ALU.mult),
                     reads=[st, gain], writes=[dst])
            else:
                P.op(eng, lambda e, o=o, st=st, n=n: e.tensor_copy(o, st[:, 0:n]), reads=[st], writes=[dst])


def norm_tile(P, C, src_ap, src_buf, hnT, col0, width=1024, eps_scale=None):
    nch = width // 128
    junk = C.junk.next()
    ss = C.small.next()
    srcs = list(src_buf) if isinstance(src_buf, (list, tuple)) else [src_buf]
    P.act(lambda e: e.activation(junk[:, 0:width], src_ap, AF.Square, accum_out=ss[:, 0:1]),
          reads=srcs, writes=[junk, ss])
    P.act(lambda e: e.activation(ss[:, 1:2], ss[:, 0:1], AF.Sqrt, scale=1.0 / width, bias=C.eps[:, 0:1]),
          reads=[ss, C.eps], writes=[ss])
    P.dve(lambda e: e.reciprocal(ss[:, 2:3], ss[:, 1:2]), reads=[ss], writes=[ss])
    xs = C.xs.next() if width <= 1024 else C.xs2.next()
    P.dve(lambda e: e.tensor_scalar(xs[:, 0:width], src_ap, ss[:, 2:3], None, ALU.mult), reads=srcs + [ss], writes=[xs])
    for c0 in range(0, nch, 8):
        pt = P.psum_tbank()
        for c in range(8):
            P.pe(lambda e, c=c: e.transpose(pt[:, c * 128:(c + 1) * 128], xs[:, (c0 + c) * 128:(c0 + c + 1) * 128], C.ident[:]),
                 reads=[xs, C.ident], writes=[pt])
        P.act(lambda e: e.activation(hnT[:, c0:c0 + 8, col0:col0 + 128], pt[:].rearrange("p (c t) -> p c t", c=8), AF.Copy),
              reads=[pt], writes=[hnT])
    return ss


def mm_acc(P, ps_ap, ps_buf, pairs, reads):
    n = len(pairs)
    for i, (l, r) in enumerate(pairs):
        P.pe(lambda e, l=l, r=r, i=i: e.matmul(ps_ap, l, r, start=(i == 0), stop=(i == n - 1)), reads=reads, writes=[ps_buf])


def h_view(h_dram, t0, nt):
    return h_dram[t0:t0 + nt * 128, :].rearrange("(j p) d -> p j d", p=128)


def phase_xattn(P, L, h_in, h_out, mem_ap, mem_norm_ap, nx_ap, wq_ap, wk_ap, wv_ap, wo_ap):
    with P.phase():
        C = Ctx(P, stage_n=2)
        gq = load_gain(P, nx_ap, 8)
        gm = load_gain(P, mem_norm_ap, 8)
        wq = P.sbuf("wq", [128, 8, 1024], BF16)
        wk = P.sbuf("wk", [128, 8, 1024], BF16)
        wv = P.sbuf("wv", [128, 8, 1024], BF16)
        wo = P.sbuf("wo", [128, 8, 1024], BF16)
        load_w(P, C, wk, wk_ap, gm)
        load_w(P, C, wv, wv_ap, gm)
        load_w(P, C, wq, wq_ap, gq)
        load_w(P, C, wo, wo_ap, None)
        ones = P.sbuf("ones", [128, 128], BF16)
        P.gp(lambda e: e.memset(ones[:], 1.0), writes=[ones])
        memT = P.sbuf("memT", [128, 8, 256], BF16)
        mt = P.sbuf("mt", [128, 2, 1024], F32)
        P.dma(mt[:], h_view(mem_ap, 0, 2), writes=[mt])
        for j in range(2):
            norm_tile(P, C, mt[:, j, :], mt, memT, j * 128)
        kT = P.sbuf("kT", [128, 8, 256], BF16)
        for fc in range(8):
            ps = P.psum_bank()
            mm_acc(P, ps[:, 0:256], ps, [(wk[:, kc, fc * 128:(fc + 1) * 128], memT[:, kc, :]) for kc in range(8)], [wk, memT])
            P.act(lambda e, fc=fc, ps=ps: e.activation(kT[:, fc, :], ps[:, 0:256], AF.Copy), reads=[ps], writes=[kT])
        v = P.sbuf("v", [128, 2, 1024], BF16)
        for j in range(2):
            for hf in range(2):
                ps = P.psum_bank()
                mm_acc(P, ps[:, :], ps, [(memT[:, kc, j * 128:(j + 1) * 128], wv[:, kc, hf * 512:(hf + 1) * 512]) for kc in range(8)], [wv, memT])
                P.act(lambda e, j=j, hf=hf, ps=ps: e.activation(v[:, j, hf * 512:(hf + 1) * 512], ps[:, :], AF.Copy), reads=[ps], writes=[v])
        P.begin_sched()
        NB = L // 512
        hpool = P.pool_of("hblk", [128, 4, 1024], F32, 2)
        hnTp = P.pool_of("hnT", [128, 8, 512], BF16, 2)
        qTp = P.pool_of("qT", [128, 8, 512], BF16, 2)
        oTp = P.pool_of("oT", [128, 8, 512], BF16, 2)
        expp = P.pool_of("expT", [128, 2, 512], BF16, 3)
        rdp = P.pool_of("rden", [128, 512], F32, 3)
        hb_next = hpool.next()
        P.dma(hb_next[:], h_view(h_in.h, 0, 4), reads=[h_in], writes=[hb_next])
        for b in range(NB):
            hb = hb_next
            if b + 1 < NB:
                hb_next = hpool.next()
                P.dma(hb_next[:], h_view(h_in.h, (b + 1) * 512, 4), reads=[h_in], writes=[hb_next])
            hnT = hnTp.next()
            for j in range(4):
                norm_tile(P, C, hb[:, j, :], hb, hnT, j * 128)
            qT = qTp.next()
            for fc in range(8):
                ps = P.psum_bank()
                mm_acc(P, ps[:, :], ps, [(wq[:, kc, fc * 128:(fc + 1) * 128], hnT[:, kc, :]) for kc in range(8)], [wq, hnT])
                P.act(lambda e, fc=fc, ps=ps: e.activation(qT[:, fc, :], ps[:, :], AF.Copy, scale=1.0 / 16.0), reads=[ps], writes=[qT])
            oT = oTp.next()
            for hd in range(4):
                ex = expp.next()
                for j in range(2):
                    ps = P.psum_bank()
                    mm_acc(P, ps[:, :], ps, [(kT[:, fc, j * 128:(j + 1) * 128], qT[:, fc, :]) for fc in (2 * hd, 2 * hd + 1)], [kT, qT])
                    P.act(lambda e, j=j, ps=ps: e.activation(ex[:, j, :], ps[:, :], AF.Exp), reads=[ps], writes=[ex])
                den = P.psum_bank()
                mm_acc(P, den[:, :], den, [(ones[:, :], ex[:, j, :]) for j in range(2)], [ones, ex])
                rd = rdp.next()
                P.dve(lambda e, rd=rd, den=den: e.reciprocal(rd[:], den[:, :]), reads=[den], writes=[rd])
                for fc in (2 * hd, 2 * hd + 1):
                    ps = P.psum_bank()
                    mm_acc(P, ps[:, :], ps, [(v[:, j, fc * 128:(fc + 1) * 128], ex[:, j, :]) for j in range(2)], [v, ex])
                    P.dve(lambda e, fc=fc, ps=ps, rd=rd: e.tensor_tensor(oT[:, fc, :], ps[:, :], rd[:], ALU.mult), reads=[ps, rd], writes=[oT])
            for j in range(4):
                for hf in range(2):
                    ps = P.psum_bank()
                    mm_acc(P, ps[:, :], ps, [(oT[:, fc, j * 128:(j + 1) * 128], wo[:, fc, hf * 512:(hf + 1) * 512]) for fc in range(8)], [oT, wo])
                    P.dve(lambda e, j=j, hf=hf, ps=ps: e.tensor_tensor(hb[:, j, hf * 512:(hf + 1) * 512], ps[:, :], hb[:, j, hf * 512:(hf + 1) * 512], ALU.add),
                          reads=[ps, hb], writes=[hb])
            P.dma(h_view(h_out.h, b * 512, 4), hb[:], reads=[hb], writes=[h_out])


def phase_ffn(P, L, h_in, h_out, norm_ap, w1_aps, w3_aps, w2_aps, FF, router_ap=None,
              final_norm_ap=None, out_ap=None, TS=2048):
    NE = len(w1_aps)
    moe = router_ap is not None
    TS = min(TS, L)
    NT = TS // 128
    NBLK = TS // 512
    groups = [(g0, min(512, FF - g0)) for g0 in range(0, FF, 512)]
    with P.phase():
        C = Ctx(P, stage_w=1024, stage_n=3)
        gn = load_gain(P, norm_ap, 8)
        wpool = [P.pool_of(n, [128, 8, 512], BF16, 2) for n in ("w1g", "w3g")]
        w2pool = P.pool_of("w2g", [128, 4, 1024], BF16, 2)
        hres = P.sbuf("hres", [128, NT, 1024], F32)
        hreg = [[Buf(hres.h, "hres_%d_%d" % (j, hf)) for hf in range(2)] for j in range(NT)]
        hall = [hreg[j][hf] for j in range(NT) for hf in range(2)]
        hnT = P.sbuf("hnT", [128, 8, TS], BF16)
        gTp = P.pool_of("gT", [128, 4, 512], BF16, 2)
        gTreg = {id(bf): [Buf(bf.h, "gT_fc%d" % fc) for fc in range(4)] for bf in gTp.bufs}
        sap = P.pool_of("sa", [128, 512], F32, 2)
        if moe:
            identf = P._identf
            rw = P.sbuf("rw", [128, 8, 8], F32)
            rst = P.sbuf("rst", [128, 8, 8], F32)
            P.dma(rst[:], router_ap.rearrange("(c p) e -> p c e", p=128), writes=[rst])
            for kc in range(8):
                P.gp(lambda e, kc=kc: e.tensor_scalar(rw[:, kc, :], rst[:, kc, :], gn[:, kc:kc + 1], None, ALU.mult),
                     reads=[rst, gn], writes=[rw])
            comb = P.sbuf("comb", [128, NT, 8], F32)
            xsf = P.sbuf("xsf", [128, 1024], F32)
            hnTf = P.sbuf("hnTf", [128, 8, 128], F32)
            rs = P.pool_of("rs", [128, 64], F32, 2)
        if final_norm_ap is not None:
            gfin = P.sbuf("gfin", [128, 1024], F32)
            P.dma(gfin[:], final_norm_ap.partition_broadcast(128), writes=[gfin])
            outp = P.pool_of("outt", [128, 1024], F32, 2)

        def load_group(e, gi):
            g0, gw = groups[gi]
            w1g, w3g = wpool[0].next(), wpool[1].next()
            w2g = w2pool.next()
            load_w(P, C, w1g, w1_aps[e], gn, f0=g0, nf=gw)
            load_w(P, C, w3g, w3_aps[e], gn, f0=g0, nf=gw)
            load_w(P, C, w2g, w2_aps[e], None, kc0=g0 // 128, nkc=gw // 128)
            return w1g, w3g, w2g

        work = [(e, gi) for e in range(NE) for gi in range(len(groups))]
        P.begin_sched()
        for st in range(L // TS):
            t0 = st * TS
            P.dma(hres[:], h_view(h_in.h, t0, NT), reads=[h_in], writes=hall)
            nxt = load_group(*work[0])
            for j in range(NT):
                ss = norm_tile(P, C, hres[:, j, :], hreg[j], hnT, j * 128)
                if moe:
                    P.dve(lambda e, j=j, ss=ss: e.tensor_scalar(xsf[:], hres[:, j, :], ss[:, 2:3], None, ALU.mult), reads=hreg[j] + [ss], writes=[xsf])
                    for half in range(2):
                        pb = P.psum_bank()
                        for c in range(4):
                            P.pe(lambda e, c=c, pb=pb, half=half: e.transpose(pb[:, c * 128:(c + 1) * 128], xsf[:, (half * 4 + c) * 128:(half * 4 + c + 1) * 128], identf[:]),
                                 reads=[xsf, identf], writes=[pb])
                        P.act(lambda e, pb=pb, half=half: e.activation(hnTf[:, half * 4:half * 4 + 4, :], pb[:, :].rearrange("p (c t) -> p c t", c=4), AF.Copy),
                              reads=[pb], writes=[hnTf])
                    pl = P.psum_bank()
                    mm_acc(P, pl[:, 0:8], pl, [(hnTf[:, kc, :], rw[:, kc, :]) for kc in range(8)], [hnTf, rw])
                    r = rs.next()
                    P.dve(lambda e, r=r, pl=pl: e.tensor_copy(r[:, 0:8], pl[:, 0:8]), reads=[pl], writes=[r])
                    P.dve(lambda e, r=r: e.max(r[:, 8:16], r[:, 0:8]), reads=[r], writes=[r])
                    P.dve(lambda e, r=r: e.tensor_scalar(r[:, 16:24], r[:, 0:8], r[:, 9:10], None, ALU.is_ge), reads=[r], writes=[r])
                    P.dve(lambda e, r=r: e.tensor_scalar(r[:, 32:33], r[:, 8:9], -1.0, None, ALU.mult), reads=[r], writes=[r])
                    P.act(lambda e, r=r: e.activation(r[:, 24:32], r[:, 0:8], AF.Exp, bias=r[:, 32:33]), reads=[r], writes=[r])
                    P.dve(lambda e, r=r: e.tensor_tensor(r[:, 24:32], r[:, 24:32], r[:, 16:24], ALU.mult), reads=[r], writes=[r])
                    P.dve(lambda e, r=r: e.reduce_sum(r[:, 33:34], r[:, 24:32], AX.X), reads=[r], writes=[r])
                    P.dve(lambda e, r=r: e.reciprocal(r[:, 34:35], r[:, 33:34]), reads=[r], writes=[r])
                    P.dve(lambda e, r=r: e.tensor_scalar(r[:, 40:48], r[:, 24:32], r[:, 34:35], None, ALU.mult), reads=[r], writes=[r])
                    P.dve(lambda e, r=r, j=j: e.tensor_copy(comb[:, j, :], r[:, 40:48]), reads=[r], writes=[comb])
            units = [(wi, blk) for wi in range(len(work)) for blk in range(NBLK)]
            grp = {0: nxt}
            if len(work) > 1:
                grp[1] = load_group(*work[1])
            gts = {}

            def up(u):
                wi, blk = units[u]
                w1g, w3g, w2g = grp[wi]
                nfc = groups[work[wi][1]][1] // 128
                c0 = blk * 512
                gT = gTp.next()
                gR = gTreg[id(gT)]
                gts[u] = (gT, gR)
                for fc in range(nfc):
                    pa = P.psum_bank()
                    mm_acc(P, pa[:, :], pa, [(w1g[:, kc, fc * 128:(fc + 1) * 128], hnT[:, kc, c0:c0 + 512]) for kc in range(8)], [w1g, hnT])
                    pc_ = P.psum_bank()
                    mm_acc(P, pc_[:, :], pc_, [(w3g[:, kc, fc * 128:(fc + 1) * 128], hnT[:, kc, c0:c0 + 512]) for kc in range(8)], [w3g, hnT])
                    sa = sap.next()
                    P.act(lambda e, sa=sa, pa=pa: e.activation(sa[:], pa[:, :], AF.Silu), reads=[pa], writes=[sa])
                    P.dve(lambda e, sa=sa, pc_=pc_, gT=gT, fc=fc: e.tensor_tensor(gT[:, fc, :], pc_[:, :], sa[:], ALU.mult), reads=[pc_, sa], writes=[gR[fc]])

            def down(u):
                wi, blk = units[u]
                w1g, w3g, w2g = grp[wi]
                e_ = work[wi][0]
                nfc = groups[work[wi][1]][1] // 128
                gT, gR = gts.pop(u)
                for j in range(4):
                    jt = blk * 4 + j
                    for hf in range(2):
                        po = P.psum_bank()
                        mm_acc(P, po[:, :], po, [(gT[:, fc, j * 128:(j + 1) * 128], w2g[:, fc, hf * 512:(hf + 1) * 512]) for fc in range(nfc)], gR[0:nfc] + [w2g])
                        hs = hres[:, jt, hf * 512:(hf + 1) * 512]
                        if moe:
                            P.dve(lambda e, po=po, hs=hs, jt=jt, e_=e_: e.scalar_tensor_tensor(hs, po[:, :], comb[:, jt, e_:e_ + 1], hs, ALU.mult, ALU.add),
                                  reads=[po, comb, hreg[jt][hf]], writes=[hreg[jt][hf]])
                        else:
                            P.dve(lambda e, po=po, hs=hs: e.tensor_tensor(hs, po[:, :], hs, ALU.add), reads=[po, hreg[jt][hf]], writes=[hreg[jt][hf]])

            up(0)
            for u in range(1, len(units)):
                up(u)
                down(u - 1)
                wi_prev, blk_prev = units[u - 1]
                if blk_prev == NBLK - 1 and wi_prev + 2 < len(work):
                    del grp[wi_prev]
                    grp[wi_prev + 2] = load_group(*work[wi_prev + 2])
            down(len(units) - 1)
            if final_norm_ap is None:
                P.dma(h_view(h_out.h, t0, NT), hres[:], reads=hall, writes=[h_out])
            else:
                for j in range(NT):
                    junk = C.junk.next()
                    ss = C.small.next()
                    P.act(lambda e, junk=junk, ss=ss, j=j: e.activation(junk[:, 0:1024], hres[:, j, :], AF.Square, accum_out=ss[:, 0:1]), reads=hreg[j], writes=[junk, ss])
                    P.act(lambda e, ss=ss: e.activation(ss[:, 1:2], ss[:, 0:1], AF.Sqrt, scale=1.0 / 1024, bias=C.eps[:, 0:1]), reads=[ss, C.eps], writes=[ss])
                    P.dve(lambda e, ss=ss: e.reciprocal(ss[:, 2:3], ss[:, 1:2]), reads=[ss], writes=[ss])
                    ot = outp.next()
                    P.dve(lambda e, ot=ot, ss=ss, j=j: e.scalar_tensor_tensor(ot[:], hres[:, j, :], ss[:, 2:3], gfin[:], ALU.mult, ALU.mult),
                          reads=hreg[j] + [ss, gfin], writes=[ot])
                    P.dma(out_ap[t0 + j * 128:t0 + (j + 1) * 128, :], ot[:], reads=[ot])


def sincos(P, T, cyc_ap, cyc_buf, N, sin_ap=None, sin_buf=None, cos_ap=None, cos_buf=None):
    for dst, dbuf, off in ((sin_ap, sin_buf, 0.0), (cos_ap, cos_buf, 0.25)):
        if dst is None:
            continue
        c, ci, r, m = T["c"], T["ci"], T["r"], T["m"]
        P.dve(lambda e: e.tensor_scalar(c[:, 0:N], cyc_ap, off, None, ALU.add), reads=[cyc_buf], writes=[c])
        P.dve(lambda e: e.tensor_copy(ci[:, 0:N], c[:, 0:N]), reads=[c], writes=[ci])
        P.dve(lambda e: e.tensor_copy(r[:, 0:N], ci[:, 0:N]), reads=[ci], writes=[r])
        P.dve(lambda e: e.tensor_tensor(r[:, 0:N], c[:, 0:N], r[:, 0:N], ALU.subtract), reads=[c, r], writes=[r])
        P.dve(lambda e: e.tensor_scalar(m[:, 0:N], r[:, 0:N], 0.5, None, ALU.is_gt), reads=[r], writes=[m])
        P.dve(lambda e: e.tensor_tensor(r[:, 0:N], r[:, 0:N], m[:, 0:N], ALU.subtract), reads=[r, m], writes=[r])
        P.dve(lambda e: e.tensor_scalar(m[:, 0:N], r[:, 0:N], -0.5, None, ALU.is_lt), reads=[r], writes=[m])
        P.dve(lambda e: e.tensor_tensor(r[:, 0:N], r[:, 0:N], m[:, 0:N], ALU.add), reads=[r, m], writes=[r])
        P.act(lambda e, dst=dst: e.activation(dst, r[:, 0:N], AF.Sin, scale=2.0 * math.pi * (1.0 - 1e-6)), reads=[r], writes=[dbuf])


def trig_scratch(P, N):
    return {"c": P.sbuf("tc", [128, N], F32), "ci": P.sbuf("tci", [128, N], I32),
            "r": P.sbuf("tr", [128, N], F32), "m": P.sbuf("tm", [128, N], F32)}


RET_LG = [math.log1p(-(2.0 ** (-5.0 - h))) for h in range(6)]


def phase_even_mixer(P, L, h_in, h_out, pos_ap, norm_ap, w_in_ap, lam_re_ap, lam_im_ap, log_dt_ap, b_re_ap, b_im_ap,
                     c_re_ap, c_im_ap, d_ap, wglu_ap, bglu_ap, w_out_ap, BLK=256):
    NCH = L // 128
    NB = L // BLK
    JB = BLK // 128
    with P.phase():
        C = Ctx(P, stage_w=16, stage_n=1)
        ident = C.ident
        gn = load_gain(P, norm_ap, 8)
        w_in = P.sbuf("w_in", [128, 8, 2560], BF16)
        w_out = P.sbuf("w_out", [128, 8, 1024], BF16)
        wglu = P.sbuf("wglu", [128, 2, 256], BF16)
        rcos = P.sbuf("rcos", [128, NCH, 32], F32)
        rsin = P.sbuf("rsin", [128, NCH, 32], F32)
        dqk = P.sbuf("dqk", [128, 12], F32)
        gS = P.sbuf("gS", [128, 3], F32)
        g128 = P.sbuf("g128", [128, 3], F32)
        g127 = P.sbuf("g127", [128, 3], F32)
        maskT = P.sbuf("maskT", [128, 128], F32)
        S = P.sbuf("Sst", [128, 3, 128], F32)
        Sbfp = P.pool_of("Sbf", [128, 3, 128], BF16, 2)
        BB = [P.sbuf("BB%d" % i, [128, 8, 128], BF16) for i in range(2)]
        sp = P.sbuf("s5p", [128, 16, 8], F32)
        DT, TH, RHO, CB, SB, AR, AI, DEN, FR, FI, T1, T2 = [sp[:, i, :] for i in range(12)]
        cosT = P.sbuf("cosT", [128, 8, BLK], F32); sinT = P.sbuf("sinT", [128, 8, BLK], F32); rhoB = P.sbuf("rhoB", [128, 8, BLK], F32)
        Cm = [P.sbuf("Cm%d" % i, [128, 8, 128], BF16) for i in range(2)]
        dsk = load_gain(P, d_ap, 2)
        bgl = load_gain(P, bglu_ap, 2)
        zl = P.sbuf("zlast", [128, 2, 8], F32)
        zi = P.sbuf("zinit", [128, 2, 8], F32)
        zt = P.sbuf("ztmp", [128, 2, 8], F32)
        with P.phase():
            CS = Ctx(P, stage_w=2048, stage_n=2)
            load_w(P, CS, w_in, w_in_ap, gn)
            load_w(P, CS, w_out, w_out_ap, None)
            load_w(P, CS, wglu, wglu_ap, None)
            T = trig_scratch(P, 1024)
            posi = P.sbuf("posi", [128, NCH], I32)
            P.dma(posi[:], pos_ap.rearrange("(c p) -> p c", p=128), writes=[posi], allow_slow_non_contiguous=True)
            posf = P.sbuf("posf", [128, NCH], F32)
            P.dve(lambda e: e.tensor_copy(posf[:], posi[:]), reads=[posi], writes=[posf])
            ji = P.sbuf("ji", [128, 32], I32)
            P.gp(lambda e: e.iota(ji[:], [[1, 32]], base=0, channel_multiplier=0), writes=[ji])
            invf = P.sbuf("invf", [128, 32], F32)
            P.dve(lambda e: e.tensor_copy(invf[:], ji[:]), reads=[ji], writes=[invf])
            P.act(lambda e: e.activation(invf[:], invf[:], AF.Exp, scale=-math.log(10000.0) / 32.0), reads=[invf], writes=[invf])
            ang = P.sbuf("ang", [128, 32, 32], F32)
            for c0 in range(0, NCH, 32):
                n = min(32, NCH - c0)
                P.dve(lambda e, c0=c0, n=n: e.tensor_tensor(ang[:, 0:n, :], posf[:, c0:c0 + n].unsqueeze(2).broadcast_to([128, n, 32]),
                                                           invf[:, :].unsqueeze(1).broadcast_to([128, n, 32]), ALU.mult), reads=[posf, invf], writes=[ang])
                P.dve(lambda e, n=n: e.tensor_scalar(ang[:, 0:n, :], ang[:, 0:n, :], 1.0 / (2.0 * math.pi), None, ALU.mult), reads=[ang], writes=[ang])
                sincos(P, T, ang[:, 0:n, :].rearrange("p a b -> p (a b)"), ang, n * 32,
                       rsin[:, c0:c0 + n, :].rearrange("p a b -> p (a b)"), rsin, rcos[:, c0:c0 + n, :].rearrange("p a b -> p (a b)"), rcos)
            ti = P.sbuf("ti", [128, 1], I32)
            P.gp(lambda e: e.iota(ti[:], [[0, 1]], base=0, channel_multiplier=1), writes=[ti])
            tf = P.sbuf("tf", [128, 1], F32)
            P.dve(lambda e: e.tensor_copy(tf[:], ti[:]), reads=[ti], writes=[tf])
            for h in range(6):
                P.act(lambda e, h=h: e.activation(dqk[:, h:h + 1], tf[:], AF.Exp, scale=RET_LG[h]), reads=[tf], writes=[dqk])
                P.act(lambda e, h=h: e.activation(dqk[:, 6 + h:7 + h], tf[:], AF.Exp, scale=-RET_LG[h]), reads=[tf], writes=[dqk])
            P.dve(lambda e: e.tensor_scalar(dqk[:, 6:12], dqk[:, 6:12], 0.125, None, ALU.mult), reads=[dqk], writes=[dqk])
            for h in range(6):
                m, hp = h // 2, h % 2
                for tb, val in ((gS, math.exp(RET_LG[h])), (g128, math.exp(128 * RET_LG[h])), (g127, math.exp(127 * RET_LG[h]))):
                    P.gp(lambda e, tb=tb, val=val, m=m, hp=hp: e.memset(tb[hp * 64:(hp + 1) * 64, m:m + 1], val), writes=[tb])
            P.gp(lambda e: e.memset(maskT[:], 1.0), writes=[maskT])
            P.gp(lambda e: e.affine_select(maskT[:], maskT[:], [[1, 128]], ALU.is_ge, 0.0, base=0, channel_multiplier=-1), reads=[maskT], writes=[maskT])
            P.gp(lambda e: e.memset(S[:], 0.0), writes=[S])
            LR = P.sbuf("LR", [128, 8], F32); LI = P.sbuf("LI", [128, 8], F32); LD = P.sbuf("LD", [128, 8], F32)
            for two in range(2):
                P.dma(LR[two * 64:(two + 1) * 64, :], lam_re_ap.rearrange("(st two) p -> two p st", two=2)[two], writes=[LR], allow_slow_non_contiguous=True)
                P.dma(LI[two * 64:(two + 1) * 64, :], lam_im_ap.rearrange("(st two) p -> two p st", two=2)[two], writes=[LI], allow_slow_non_contiguous=True)
                P.dma(LD[two * 64:(two + 1) * 64, :], log_dt_ap.rearrange("(st two) -> two st", two=2)[two].partition_broadcast(64), writes=[LD], allow_slow_non_contiguous=True)
            BBf = [P.sbuf("BBf%d" % i, [128, 8, 128], F32) for i in range(2)]
            Cf = [P.sbuf("Cf%d" % i, [128, 8, 128], F32) for i in range(2)]
            for t_ in BBf + Cf:
                P.gp(lambda e, t_=t_: e.memset(t_[:], 0.0), writes=[t_])
            for g in range(16):
                st, two, r0 = g // 2, g % 2, (g % 8) * 16
                for i, (bap, cap) in enumerate(((b_re_ap, c_re_ap), (b_im_ap, c_im_ap))):
                    P.dma(BBf[i][r0:r0 + 16, st, two * 64:(two + 1) * 64], bap[g].rearrange("p c -> c p"), writes=[BBf[i]], allow_slow_non_contiguous=True)
                    P.dma(Cf[i][two * 64:(two + 1) * 64, st, r0:r0 + 16], cap[g].rearrange("c p -> p c"), writes=[Cf[i]], allow_slow_non_contiguous=True)
            for i in range(2):
                P.dve(lambda e, i=i: e.tensor_copy(BB[i][:], BBf[i][:]), reads=[BBf[i]], writes=[BB[i]])

            def sop(fn, eng="dve"):
                P.op(eng, fn, reads=[sp, LR, LI, LD], writes=[sp])
            sop(lambda e: e.activation(DT, LD[:], AF.Exp), "act")
            sop(lambda e: e.tensor_tensor(TH, LI[:], DT, ALU.mult))
            sop(lambda e: e.tensor_tensor(RHO, LR[:], DT, ALU.mult))
            sop(lambda e: e.activation(RHO, RHO, AF.Exp), "act")
            taui = P.sbuf("taui", [128, BLK], I32)
            P.gp(lambda e: e.iota(taui[:], [[1, BLK]], base=0, channel_multiplier=0), writes=[taui])
            tauc = P.sbuf("tauc", [128, BLK], F32)
            P.dve(lambda e: e.tensor_copy(tauc[:], taui[:]), reads=[taui], writes=[tauc])
            P.dve(lambda e: e.tensor_scalar(tauc[:], tauc[:], 1.0 / (2.0 * math.pi), None, ALU.mult), reads=[tauc], writes=[tauc])
            cyc = P.sbuf("cyc", [128, BLK], F32)
            for st in range(8):
                P.dve(lambda e, st=st: e.tensor_scalar(cyc[:], tauc[:], sp[:, 1, st:st + 1], None, ALU.mult), reads=[tauc, sp], writes=[cyc])
                sincos(P, T, cyc[:, :], cyc, BLK, sinT[:, st, :], sinT, cosT[:, st, :], cosT)
                P.dve(lambda e, st=st: e.tensor_copy(rhoB[:, st, :], sp[:, 2, st:st + 1].broadcast_to([128, BLK])), reads=[sp], writes=[rhoB])
            cyc8 = P.sbuf("cyc8", [128, 8], F32)
            P.dve(lambda e: e.tensor_scalar(cyc8[:], TH, float(BLK) / (2.0 * math.pi), None, ALU.mult), reads=[sp], writes=[cyc8])
            sincos(P, T, cyc8[:, :], cyc8, 8, SB, sp, CB, sp)
            P.dve(lambda e: e.tensor_tensor(AR, RHO, cosT[:, :, 1], ALU.mult), reads=[sp, cosT], writes=[sp])
            P.dve(lambda e: e.tensor_tensor(AI, RHO, sinT[:, :, 1], ALU.mult), reads=[sp, sinT], writes=[sp])
            sop(lambda e: e.tensor_scalar(AR, AR, -1.0, None, ALU.add))
            sop(lambda e: e.tensor_tensor(DEN, LR[:], LR[:], ALU.mult))
            sop(lambda e: e.tensor_tensor(T1, LI[:], LI[:], ALU.mult))
            sop(lambda e: e.tensor_tensor(DEN, DEN, T1, ALU.add))
            sop(lambda e: e.reciprocal(DEN, DEN))
            sop(lambda e: e.tensor_tensor(T1, AR, LR[:], ALU.mult))
            sop(lambda e: e.tensor_tensor(T2, AI, LI[:], ALU.mult))
            sop(lambda e: e.tensor_tensor(FR, T1, T2, ALU.add))
            sop(lambda e: e.tensor_tensor(FR, FR, DEN, ALU.mult))
            sop(lambda e: e.tensor_tensor(T1, AI, LR[:], ALU.mult))
            sop(lambda e: e.tensor_tensor(T2, AR, LI[:], ALU.mult))
            sop(lambda e: e.tensor_tensor(FI, T1, T2, ALU.subtract))
            sop(lambda e: e.tensor_tensor(FI, FI, DEN, ALU.mult))
            ct = P.sbuf("ct", [128, 2, 128], F32)
            for st in range(8):
                fr, fi = sp[:, 8, st:st + 1], sp[:, 9, st:st + 1]
                P.dve(lambda e, st=st, fi=fi: e.tensor_scalar(ct[:, 0, :], Cf[1][:, st, :], fi, None, ALU.mult), reads=[Cf[1], sp], writes=[ct])
                P.dve(lambda e, st=st, fr=fr: e.scalar_tensor_tensor(ct[:, 1, :], Cf[0][:, st, :], fr, ct[:, 0, :], ALU.mult, ALU.subtract), reads=[Cf[0], sp, ct], writes=[ct])
                P.dve(lambda e, st=st: e.tensor_copy(Cm[0][:, st, :], ct[:, 1, :]), reads=[ct], writes=[Cm[0]])
                P.dve(lambda e, st=st, fr=fr: e.tensor_scalar(ct[:, 0, :], Cf[1][:, st, :], fr, None, ALU.mult), reads=[Cf[1], sp], writes=[ct])
                P.dve(lambda e, st=st, fi=fi: e.scalar_tensor_tensor(ct[:, 1, :], Cf[0][:, st, :], fi, ct[:, 0, :], ALU.mult, ALU.add), reads=[Cf[0], sp, ct], writes=[ct])
                P.dve(lambda e, st=st: e.tensor_scalar(Cm[1][:, st, :], ct[:, 1, :], -1.0, None, ALU.mult), reads=[ct], writes=[Cm[1]])
            P.gp(lambda e: e.memset(zl[:], 0.0), writes=[zl])

        P.begin_sched()
        hpool = P.pool_of("hblk", [128, JB, 1024], F32, 2)
        hnTp = P.pool_of("hnT", [128, 8, BLK], BF16, 1)
        ycp = P.pool_of("ycat", [128, 8, BLK], BF16, 1)
        uTp = P.pool_of("uT", [128, 2, BLK], BF16, 1)
        uTfp = P.pool_of("uTf", [128, 2, BLK], F32, 1)
        xtp = P.pool_of("xt", [128, 2, BLK], F32, 2)
        wkp = P.pool_of("wk", [128, 2, BLK], F32, 2)
        zp = P.pool_of("z", [128, 2, BLK], F32, 2)
        sbp = P.pool_of("sbf", [128, 2, 4, BLK], BF16, 1)
        yfp = P.pool_of("yf", [128, BLK], F32, 2)
        y2p = P.pool_of("y2", [128, BLK], F32, 2)
        ybp = P.pool_of("yb", [128, 2, BLK], BF16, 1)
        qkp = P.pool_of("qk", [128, 768], F32, 2)
        r1p = P.pool_of("r1", [128, 768], F32, 1)
        r2p = P.pool_of("r2", [128, 768], F32, 1)
        qksp = P.pool_of("qks", [128, 768], BF16, 2)
        qkTp = P.pool_of("qkT", [128, 6, 128], BF16, 2)
        vsp = P.pool_of("vs", [128, 768], BF16, 2)
        sgp = P.pool_of("sg", [128, 768], F32, 2)
        Mp = P.pool_of("M", [128, 128], BF16, 6)
        ysbp = P.pool_of("ysb", [128, 768], F32, 1)
        ysqp = P.pool_of("ysq", [128, 768], F32, 1)
        ynp = P.pool_of("yn", [128, 768], BF16, 2)
        st6p = P.pool_of("st6", [128, 3, 6], F32, 2)

        hb_next = hpool.next()
        P.dma(hb_next[:], h_view(h_in.h, 0, JB), reads=[h_in], writes=[hb_next])
        for b in range(NB):
            hb = hb_next
            if b + 1 < NB:
                hb_next = hpool.next()
                P.dma(hb_next[:], h_view(h_in.h, (b + 1) * BLK, JB), reads=[h_in], writes=[hb_next])
            hnT = hnTp.next()
            for j in range(JB):
                norm_tile(P, C, hb[:, j, :], hb, hnT, j * 128)
            ycat = ycp.next()
            uT, uTf = uTp.next(), uTfp.next()
            for c in range(2):
                ps = P.psum_bank()
                mm_acc(P, ps[:, 0:BLK], ps, [(w_in[:, kc, 2304 + c * 128:2304 + (c + 1) * 128], hnT[:, kc, :]) for kc in range(8)], [w_in, hnT])
                P.act(lambda e, c=c, ps=ps: e.activation(uT[:, c, :], ps[:, 0:BLK], AF.Copy), reads=[ps], writes=[uT])
                P.act(lambda e, c=c, ps=ps: e.activation(uTf[:, c, :], ps[:, 0:BLK], AF.Copy), reads=[ps], writes=[uTf])
            P.dve(lambda e: e.tensor_tensor(zt[:, 0, :], CB, zl[:, 0, :], ALU.mult), reads=[sp, zl], writes=[zt])
            P.dve(lambda e: e.tensor_tensor(zt[:, 1, :], SB, zl[:, 1, :], ALU.mult), reads=[sp, zl], writes=[zt])
            P.dve(lambda e: e.tensor_tensor(zi[:, 0, :], zt[:, 0, :], zt[:, 1, :], ALU.subtract), reads=[zt], writes=[zi])
            P.dve(lambda e: e.tensor_tensor(zt[:, 0, :], SB, zl[:, 0, :], ALU.mult), reads=[sp, zl], writes=[zt])
            P.dve(lambda e: e.tensor_tensor(zt[:, 1, :], CB, zl[:, 1, :], ALU.mult), reads=[sp, zl], writes=[zt])
            P.dve(lambda e: e.tensor_tensor(zi[:, 1, :], zt[:, 0, :], zt[:, 1, :], ALU.add), reads=[zt], writes=[zi])
            sbf = sbp.next()
            for st in range(8):
                c = st // 4
                pr, pi_ = P.psum_bank(), P.psum_bank()
                P.pe(lambda e, st=st, c=c, pr=pr: e.matmul(pr[:, 0:BLK], BB[0][:, st, :], uT[:, c, :], start=True, stop=True), reads=[BB[0], uT], writes=[pr])
                P.pe(lambda e, st=st, c=c, pi_=pi_: e.matmul(pi_[:, 0:BLK], BB[1][:, st, :], uT[:, c, :], start=True, stop=True), reads=[BB[1], uT], writes=[pi_])
                xt, wk = xtp.next(), wkp.next()
                P.dve(lambda e, st=st, pr=pr, wk=wk: e.tensor_tensor(wk[:, 0, :], pr[:, 0:BLK], cosT[:, st, :], ALU.mult), reads=[pr, cosT], writes=[wk])
                P.dve(lambda e, st=st, pi_=pi_, wk=wk: e.tensor_tensor(wk[:, 1, :], pi_[:, 0:BLK], sinT[:, st, :], ALU.mult), reads=[pi_, sinT], writes=[wk])
                P.gp(lambda e, xt=xt, wk=wk: e.tensor_tensor(xt[:, 0, :], wk[:, 0, :], wk[:, 1, :], ALU.add), reads=[wk], writes=[xt])
                wk2 = wkp.next()
                P.dve(lambda e, st=st, pi_=pi_, wk2=wk2: e.tensor_tensor(wk2[:, 0, :], pi_[:, 0:BLK], cosT[:, st, :], ALU.mult), reads=[pi_, cosT], writes=[wk2])
                P.dve(lambda e, st=st, pr=pr, wk2=wk2: e.tensor_tensor(wk2[:, 1, :], pr[:, 0:BLK], sinT[:, st, :], ALU.mult), reads=[pr, sinT], writes=[wk2])
                P.gp(lambda e, xt=xt, wk2=wk2: e.tensor_tensor(xt[:, 1, :], wk2[:, 0, :], wk2[:, 1, :], ALU.subtract), reads=[wk2], writes=[xt])
                z = zp.next()
                for ri in range(2):
                    P.dve(lambda e, z=z, xt=xt, ri=ri, st=st: e.tensor_tensor_scan(z[:, ri, :], rhoB[:, st, :], xt[:, ri, :], zi[:, ri, st:st + 1], ALU.mult, ALU.add),
                          reads=[rhoB, xt, zi], writes=[z])
                P.dve(lambda e, z=z, st=st: e.tensor_copy(zl[:, :, st:st + 1], z[:, :, BLK - 1:BLK]), reads=[z], writes=[zl])
                w3, w4 = wkp.next(), wkp.next()
                P.gp(lambda e, w3=w3, z=z, st=st: e.tensor_tensor(w3[:, 0, :], z[:, 0, :], cosT[:, st, :], ALU.mult), reads=[z, cosT], writes=[w3])
                P.gp(lambda e, w3=w3, z=z, st=st: e.tensor_tensor(w3[:, 1, :], z[:, 1, :], sinT[:, st, :], ALU.mult), reads=[z, sinT], writes=[w3])
                P.gp(lambda e, w3=w3, st=st: e.tensor_tensor(sbf[:, 0, st % 4, :], w3[:, 0, :], w3[:, 1, :], ALU.subtract), reads=[w3], writes=[sbf])
                P.dve(lambda e, w4=w4, z=z, st=st: e.tensor_tensor(w4[:, 0, :], z[:, 0, :], sinT[:, st, :], ALU.mult), reads=[z, sinT], writes=[w4])
                P.dve(lambda e, w4=w4, z=z, st=st: e.tensor_tensor(w4[:, 1, :], z[:, 1, :], cosT[:, st, :], ALU.mult), reads=[z, cosT], writes=[w4])
                P.dve(lambda e, w4=w4, st=st: e.tensor_tensor(sbf[:, 1, st % 4, :], w4[:, 0, :], w4[:, 1, :], ALU.add), reads=[w4], writes=[sbf])
                if st % 4 == 3:
                    py = P.psum_bank()
                    pairs = []
                    for s4 in range(4):
                        pairs.append((Cm[0][:, c * 4 + s4, :], sbf[:, 0, s4, :]))
                        pairs.append((Cm[1][:, c * 4 + s4, :], sbf[:, 1, s4, :]))
                    mm_acc(P, py[:, 0:BLK], py, pairs, [Cm[0], Cm[1], sbf])
                    yf, y2 = yfp.next(), y2p.next()
                    P.dve(lambda e, yf=yf, py=py, c=c: e.scalar_tensor_tensor(yf[:], uTf[:, c, :], dsk[:, c:c + 1], py[:, 0:BLK], ALU.mult, ALU.add), reads=[uTf, dsk, py], writes=[yf])
                    P.gp(lambda e, yf=yf, y2=y2: e.tensor_tensor(y2[:], yf[:], yf[:], ALU.mult), reads=[yf], writes=[y2])
                    P.dve(lambda e, y2=y2: e.tensor_scalar(y2[:], y2[:], 0.044715, 1.0, ALU.mult, ALU.add), reads=[y2], writes=[y2])
                    P.gp(lambda e, yf=yf, y2=y2: e.tensor_tensor(y2[:], y2[:], yf[:], ALU.mult), reads=[yf, y2], writes=[y2])
                    P.act(lambda e, y2=y2: e.activation(y2[:], y2[:], AF.Sigmoid, scale=1.5957691216057308), reads=[y2], writes=[y2])
                    P.dve(lambda e, yf=yf, y2=y2: e.tensor_tensor(yf[:], yf[:], y2[:], ALU.mult), reads=[yf, y2], writes=[yf])
                    if c == 0:
                        yb = ybp.next()
                        yfs = [yf]
                    else:
                        yfs.append(yf)
                    P.act(lambda e, yb=yb, yf=yf, c=c: e.activation(yb[:, c, :], yf[:], AF.Copy), reads=[yf], writes=[yb])
            for fc in range(2):
                pz = P.psum_bank()
                mm_acc(P, pz[:, 0:BLK], pz, [(wglu[:, kc, fc * 128:(fc + 1) * 128], yb[:, kc, :]) for kc in range(2)], [wglu, yb])
                sg_ = y2p.next()
                P.act(lambda e, pz=pz, sg_=sg_, fc=fc: e.activation(sg_[:], pz[:, 0:BLK], AF.Sigmoid, bias=bgl[:, fc:fc + 1]), reads=[pz, bgl], writes=[sg_])
                P.dve(lambda e, sg_=sg_, fc=fc, yfs=yfs: e.tensor_tensor(ycat[:, 6 + fc, :], yfs[fc][:], sg_[:], ALU.mult), reads=[yfs[fc], sg_], writes=[ycat])
            for j in range(JB):
                ch = b * JB + j
                tsl = slice(j * 128, (j + 1) * 128)
                qk = qkp.next()
                for (c0, c1) in ((0, 512), (512, 768)):
                    ps = P.psum_bank()
                    mm_acc(P, ps[:, 0:c1 - c0], ps, [(hnT[:, kc, tsl], w_in[:, kc, c0:c1]) for kc in range(8)], [w_in, hnT])
                    P.act(lambda e, ps=ps, c0=c0, c1=c1, qk=qk: e.activation(qk[:, c0:c1], ps[:, 0:c1 - c0], AF.Copy), reads=[ps], writes=[qk])
                r1, r2 = r1p.next(), r2p.next()
                X = qk[:, :].rearrange("p (a h j) -> p a h j", a=12, h=2)
                R1 = r1[:, :].rearrange("p (a h j) -> p a h j", a=12, h=2)
                R2 = r2[:, :].rearrange("p (a h j) -> p a h j", a=12, h=2)
                cosb = rcos[:, ch, :].unsqueeze(1).broadcast_to([128, 12, 32])
                sinb = rsin[:, ch, :].unsqueeze(1).broadcast_to([128, 12, 32])
                for hh in range(2):
                    P.gp(lambda e, hh=hh: e.tensor_tensor(R1[:, :, hh, :], X[:, :, hh, :], cosb, ALU.mult), reads=[qk, rcos], writes=[r1])
                    P.dve(lambda e, hh=hh: e.tensor_tensor(R2[:, :, hh, :], X[:, :, 1 - hh, :], sinb, ALU.mult), reads=[qk, rsin], writes=[r2])
                P.gp(lambda e: e.tensor_tensor(R1[:, :, 0, :], R1[:, :, 0, :], R2[:, :, 0, :], ALU.subtract), reads=[r1, r2], writes=[r1])
                P.gp(lambda e: e.tensor_tensor(R1[:, :, 1, :], R1[:, :, 1, :], R2[:, :, 1, :], ALU.add), reads=[r1, r2], writes=[r1])
                qks = qksp.next()
                P.dve(lambda e, qks=qks: e.tensor_tensor(qks[:, :].rearrange("p (a d) -> p a d", a=12), r1[:, :].rearrange("p (a d) -> p a d", a=12),
                                                        dqk[:, :].unsqueeze(2).broadcast_to([128, 12, 64]), ALU.mult), reads=[r1, dqk], writes=[qks])
                pt = P.psum_tbank()
                for m in range(6):
                    P.pe(lambda e, m=m, pt=pt, qks=qks: e.transpose(pt[:, m * 128:(m + 1) * 128], qks[:, m * 128:(m + 1) * 128], ident[:]), reads=[qks, ident], writes=[pt])
                qkT = qkTp.next()
                P.act(lambda e, pt=pt, qkT=qkT: e.activation(qkT[:, :, :], pt[:, 0:768].rearrange("p (m t) -> p m t", m=6), AF.Copy), reads=[pt], writes=[qkT])
                vs, sg = vsp.next(), sgp.next()
                for (dst, base, fn) in ((vs, 768, AF.Copy), (sg, 1536, AF.Silu)):
                    for (c0, c1) in ((0, 512), (512, 768)):
                        ps = P.psum_bank()
                        mm_acc(P, ps[:, 0:c1 - c0], ps, [(hnT[:, kc, tsl], w_in[:, kc, base + c0:base + c1]) for kc in range(8)], [w_in, hnT])
                        P.act(lambda e, ps=ps, c0=c0, c1=c1, dst=dst, fn=fn: e.activation(dst[:, c0:c1], ps[:, 0:c1 - c0], fn), reads=[ps], writes=[dst])
                Sbf = Sbfp.next()
                for m in range(3):
                    P.dve(lambda e, m=m, Sbf=Sbf: e.tensor_scalar(Sbf[:, m, :], S[:, m, :], gS[:, m:m + 1], None, ALU.mult), reads=[S, gS], writes=[Sbf])
                Ms = []
                for h in range(6):
                    m, hp = h // 2, h % 2
                    psl = slice(hp * 64, (hp + 1) * 64)
                    pS = P.psum_bank()
                    P.pe(lambda e, pS=pS, m=m, psl=psl, qkT=qkT: e.matmul(pS[:, 0:128], qkT[psl, 3 + m, :], qkT[psl, m, :], start=True, stop=True), reads=[qkT], writes=[pS])
                    M = Mp.next()
                    P.dve(lambda e, pS=pS, M=M: e.tensor_tensor(M[:], pS[:, 0:128], maskT[:], ALU.mult), reads=[pS, maskT], writes=[M])
                    Ms.append(M)
                ysb = ysbp.next()
                for (h0, h1) in ((0, 4), (4, 6)):
                    py = P.psum_bank()
                    for h in range(h0, h1):
                        m, hp = h // 2, h % 2
                        psl = slice(hp * 64, (hp + 1) * 64)
                        osl = slice((h - h0) * 128, (h - h0 + 1) * 128)
                        P.pe(lambda e, py=py, osl=osl, h=h, vs=vs, M=Ms[h]: e.matmul(py[:, osl], M[:], vs[:, h * 128:(h + 1) * 128], start=True, stop=False), reads=[Ms[h], vs], writes=[py])
                        P.pe(lambda e, py=py, osl=osl, m=m, psl=psl, qkT=qkT, Sbf=Sbf: e.matmul(py[:, osl], qkT[psl, m, :], Sbf[psl, m, :], start=False, stop=True), reads=[qkT, Sbf], writes=[py])
                    P.act(lambda e, py=py, h0=h0, h1=h1, ysb=ysb: e.activation(ysb[:, h0 * 128:h1 * 128], py[:, 0:(h1 - h0) * 128], AF.Copy), reads=[py], writes=[ysb])
                for m in range(3):
                    pU = P.psum_bank()
                    for hp in range(2):
                        h = 2 * m + hp
                        P.pe(lambda e, pU=pU, hp=hp, h=h, qks=qks, vs=vs: e.matmul(pU[hp * 64:(hp + 1) * 64, 0:128], qks[:, 384 + h * 64:384 + (h + 1) * 64], vs[:, h * 128:(h + 1) * 128], start=True, stop=True),
                             reads=[qks, vs], writes=[pU])
                    P.dve(lambda e, m=m: e.tensor_scalar(S[:, m, :], S[:, m, :], g128[:, m:m + 1], None, ALU.mult), reads=[S, g128], writes=[S])
                    P.dve(lambda e, m=m, pU=pU: e.scalar_tensor_tensor(S[:, m, :], pU[:, 0:128], g127[:, m:m + 1], S[:, m, :], ALU.mult, ALU.add), reads=[pU, g127, S], writes=[S])
                ysq, s6 = ysqp.next(), st6p.next()
                P.gp(lambda e, ysq=ysq, ysb=ysb: e.tensor_tensor(ysq[:], ysb[:], ysb[:], ALU.mult), reads=[ysb], writes=[ysq])
                P.dve(lambda e, ysq=ysq, s6=s6: e.tensor_reduce(s6[:, 0, :], ysq[:, :].rearrange("p (h d) -> p h d", h=6), AX.X, ALU.add), reads=[ysq], writes=[s6])
                P.act(lambda e, s6=s6: e.activation(s6[:, 1, :], s6[:, 0, :], AF.Sqrt, scale=1.0 / 128.0, bias=C.eps[:, 0:1]), reads=[s6, C.eps], writes=[s6])
                P.dve(lambda e, s6=s6: e.reciprocal(s6[:, 2, :], s6[:, 1, :]), reads=[s6], writes=[s6])
                P.gp(lambda e, ysq=ysq, ysb=ysb, sg=sg: e.tensor_tensor(ysq[:], ysb[:], sg[:], ALU.mult), reads=[ysb, sg], writes=[ysq])
                yn = ynp.next()
                P.dve(lambda e, yn=yn, ysq=ysq, s6=s6: e.tensor_tensor(yn[:, :].rearrange("p (h d) -> p h d", h=6), ysq[:, :].rearrange("p (h d) -> p h d", h=6),
                                                                      s6[:, 2, :].unsqueeze(2).broadcast_to([128, 6, 128]), ALU.mult), reads=[ysq, s6], writes=[yn])
                pt2 = P.psum_tbank()
                for m in range(6):
                    P.pe(lambda e, m=m, pt2=pt2, yn=yn: e.transpose(pt2[:, m * 128:(m + 1) * 128], yn[:, m * 128:(m + 1) * 128], ident[:]), reads=[yn, ident], writes=[pt2])
                P.act(lambda e, pt2=pt2, tsl=tsl: e.activation(ycat[:, 0:6, tsl], pt2[:, 0:768].rearrange("p (m t) -> p m t", m=6), AF.Copy), reads=[pt2], writes=[ycat])
            for j in range(JB):
                for hf in range(2):
                    po = P.psum_bank()
                    mm_acc(P, po[:, :], po, [(ycat[:, kc, j * 128:(j + 1) * 128], w_out[:, kc, hf * 512:(hf + 1) * 512]) for kc in range(8)], [ycat, w_out])
                    P.dve(lambda e, po=po, j=j, hf=hf: e.tensor_tensor(hb[:, j, hf * 512:(hf + 1) * 512], po[:, :], hb[:, j, hf * 512:(hf + 1) * 512], ALU.add), reads=[po, hb], writes=[hb])
            P.dma(h_view(h_out.h, b * BLK, JB), hb[:], reads=[hb], writes=[h_out])


BCAST_LHST = False


def phase_mamba_a(P, L, h_in, yn_out, norm_ap, w_in_ap, conv_w_ap, conv_b_ap, dt_bias_ap, a_log_ap, d_ap):
    NCH = L // 128
    with P.phase():
        C = Ctx(P, stage_w=16, stage_n=1, junk_w=1024)
        ident, identf = C.ident, P._identf
        gn = load_gain(P, norm_ap, 8)
        cbcol = load_gain(P, conv_b_ap, 24)
        w_in = P.sbuf("w_in", [128, 8, 5152], BF16)
        Dg = P.sbuf("Dg", [128, 24, 4, 128], BF16)
        cbrow = P.sbuf("cbrow", [1, 3072], BF16)
        ones1 = P.sbuf("ones1", [1, 128], BF16)
        dtb = P.sbuf("dtb", [128, 32], F32)
        abc = P.sbuf("abc", [128, 32], F32)
        dskb = P.sbuf("dskb", [128, 32], F32)
        tri = P.sbuf("tri", [128, 128], F32)
        ustr = P.sbuf("ustr", [128, 128], F32)
        onesf = P.sbuf("onesf", [128, 128], F32)
        stT = P.sbuf("stT", [128, 4, 512], F32)
        stTb = P.sbuf("stTb", [128, 4, 512], BF16)
        with P.phase():
            CS = Ctx(P, stage_w=2048, stage_n=2)
            load_w(P, CS, w_in, w_in_ap, gn)
            cw = P.sbuf("cw", [128, 24, 4], F32)
            for k in range(4):
                P.dma(cw[:, :, k], conv_w_ap[k].rearrange("(ct p) -> p ct", p=128), writes=[cw], allow_slow_non_contiguous=True)
            for ct in range(24):
                for k in range(4):
                    P.act(lambda e, ct=ct, k=k: e.activation(Dg[:, ct, k, :], identf[:], AF.Copy, scale=cw[:, ct, k:k + 1]), reads=[identf, cw], writes=[Dg])
            cbr = P.sbuf("cbr", [1, 3072], F32)
            P.dma(cbr[:], conv_b_ap.unsqueeze(0), writes=[cbr])
            P.dve(lambda e: e.tensor_copy(cbrow[:], cbr[:]), reads=[cbr], writes=[cbrow])
            P.gp(lambda e: e.memset(ones1[:], 1.0), writes=[ones1])
            P.dma(dtb[:], dt_bias_ap.partition_broadcast(128), writes=[dtb])
            P.dma(abc[:], a_log_ap.partition_broadcast(128), writes=[abc])
            P.dma(dskb[:], d_ap.partition_broadcast(128), writes=[dskb])
            P.act(lambda e: e.activation(abc[:], abc[:], AF.Exp), reads=[abc], writes=[abc])
            P.dve(lambda e: e.tensor_scalar(abc[:], abc[:], -1.0, None, ALU.mult), reads=[abc], writes=[abc])
            P.gp(lambda e: e.memset(tri[:], 1.0), writes=[tri])
            P.gp(lambda e: e.affine_select(tri[:], tri[:], [[1, 128]], ALU.is_ge, 0.0, base=0, channel_multiplier=-1), reads=[tri], writes=[tri])
            P.gp(lambda e: e.memset(ustr[:], 1.0), writes=[ustr])
            P.gp(lambda e: e.affine_select(ustr[:], ustr[:], [[-1, 128]], ALU.is_gt, 0.0, base=0, channel_multiplier=1), reads=[ustr], writes=[ustr])
            P.gp(lambda e: e.memset(onesf[:], 1.0), writes=[onesf])
            P.gp(lambda e: e.memset(stT[:], 0.0), writes=[stT])
            P.gp(lambda e: e.memset(stTb[:], 0.0), writes=[stTb])
        P.begin_sched()
        hpool = P.pool_of("hblk", [128, 1024], F32, 2)
        hnTp = P.pool_of("hnT", [128, 8, 128], BF16, 2)
        zsp = P.pool_of("zs", [128, 2048], BF16, 1)
        xprep = P.pool_of("xpre", [128, 24, 131], BF16, 2)
        xsp = P.pool_of("xstok", [128, 2048], BF16, 1)
        btp = P.pool_of("btok", [128, 512], BF16, 1)
        BTp = P.pool_of("BT", [128, 4, 128], BF16, 1)
        CTp = P.pool_of("CT", [128, 4, 128], BF16, 1)
        cbtp = P.pool_of("cbt", [128, 4, 128], F32, 1)
        dtp = P.pool_of("dts", [128, 6, 32], F32, 2)
        csp = P.pool_of("cs", [128, 96], F32, 2)
        ecp = P.pool_of("ecs", [128, 96], F32, 2)
        Wp = P.pool_of("Wh", [128, 128], F32, 4)
        eLp = P.pool_of("eL", [128, 4, 128], F32, 2)
        MTp = P.pool_of("MT", [128, 4, 128], BF16, 4)
        xdtp = P.pool_of("xdt", [128, 512], BF16, 2)
        xwp = P.pool_of("xw", [128, 512], BF16, 2)
        yop = P.pool_of("yo", [128, 512], F32, 2)
        ygp = P.pool_of("yg", [128, 2048], F32, 1)
        ynp = P.pool_of("ynb", [128, 2048], BF16, 1)

        hb_next = hpool.next()
        P.dma(hb_next[:], h_in.h[0:128, :], reads=[h_in], writes=[hb_next])
        xprev = None
        for ch in range(NCH):
            hb = hb_next
            if ch + 1 < NCH:
                hb_next = hpool.next()
                P.dma(hb_next[:], h_in.h[(ch + 1) * 128:(ch + 2) * 128, :], reads=[h_in], writes=[hb_next])
            hnT = hnTp.next()
            norm_tile(P, C, hb[:, :], hb, hnT, 0)
            zs = zsp.next()
            for q in range(4):
                ps = P.psum_bank()
                mm_acc(P, ps[:, :], ps, [(hnT[:, kc, :], w_in[:, kc, q * 512:(q + 1) * 512]) for kc in range(8)], [hnT, w_in])
                P.act(lambda e, ps=ps, q=q, zs=zs: e.activation(zs[:, q * 512:(q + 1) * 512], ps[:, :], AF.Silu), reads=[ps], writes=[zs])
            xpre = xprep.next()
            if xprev is None:
                P.gp(lambda e, xpre=xpre: e.memset(xpre[:, :, 0:3], 0.0), writes=[xpre])
            else:
                P.gp(lambda e, xpre=xpre, xprev=xprev: e.tensor_copy(xpre[:, :, 0:3], xprev[:, :, 128:131]), reads=[xprev], writes=[xpre])
            for q in range(6):
                ps = P.psum_bank()
                for i in range(4):
                    ct = q * 4 + i
                    mm_acc(P, ps[:, i * 128:(i + 1) * 128], ps, [(w_in[:, kc, 2048 + ct * 128:2048 + (ct + 1) * 128], hnT[:, kc, :]) for kc in range(8)], [hnT, w_in])
                P.act(lambda e, ps=ps, q=q, xpre=xpre: e.activation(xpre[:, q * 4:(q + 1) * 4, 3:131], ps[:, :].rearrange("p (i t) -> p i t", i=4), AF.Copy), reads=[ps], writes=[xpre])
            xprev = xpre
            dts = dtp.next()
            ps = P.psum_bank()
            mm_acc(P, ps[:, 0:32], ps, [(hnT[:, kc, :], w_in[:, kc, 5120:5152]) for kc in range(8)], [hnT, w_in])
            P.dve(lambda e, ps=ps, dts=dts: e.tensor_tensor(dts[:, 0, :], ps[:, 0:32], dtb[:], ALU.add), reads=[ps, dtb], writes=[dts])
            P.act(lambda e, dts=dts: e.activation(dts[:, 0, :], dts[:, 0, :], AF.Exp), reads=[dts], writes=[dts])
            P.act(lambda e, dts=dts: e.activation(dts[:, 1, :], dts[:, 0, :], AF.Ln, bias=P.const(1.0)[:, 0:1]), reads=[dts, P.const(1.0)], writes=[dts])
            P.dve(lambda e, dts=dts: e.tensor_tensor(dts[:, 2, :], dts[:, 1, :], abc[:], ALU.mult), reads=[dts, abc], writes=[dts])
            pc = P.psum_bank()
            for i, mat in enumerate((tri, ustr, onesf)):
                P.pe(lambda e, pc=pc, i=i, mat=mat, dts=dts: e.matmul(pc[:, i * 32:(i + 1) * 32], mat[:], dts[:, 2, :], start=True, stop=True), reads=[mat, dts], writes=[pc])
            cs, ecs = csp.next(), ecp.next()
            P.act(lambda e, pc=pc, cs=cs: e.activation(cs[:], pc[:, 0:96], AF.Copy), reads=[pc], writes=[cs])
            P.act(lambda e, pc=pc, ecs=ecs: e.activation(ecs[:], pc[:, 0:96], AF.Exp), reads=[pc], writes=[ecs])
            P.dve(lambda e, dts=dts, ecs=ecs: e.tensor_tensor(dts[:, 3, :], dts[:, 1, :], ecs[:, 32:64], ALU.mult), reads=[dts, ecs], writes=[dts])
            xs = xsp.next()
            for q in range(4):
                ps = P.psum_bank()
                P.pe(lambda e, ps=ps, q=q: e.matmul(ps[:, :], ones1[0:1, :], cbrow[0:1, q * 512:(q + 1) * 512], start=True, stop=False), reads=[ones1, cbrow], writes=[ps])
                for i in range(4):
                    ct = q * 4 + i
                    for k in range(4):
                        last = (i == 3 and k == 3)
                        P.pe(lambda e, ps=ps, i=i, ct=ct, k=k, last=last, xpre=xpre: e.matmul(ps[:, i * 128:(i + 1) * 128], xpre[:, ct, k:k + 128], Dg[:, ct, k, :], start=False, stop=last, skip_group_check=True),
                             reads=[xpre, Dg], writes=[ps])
                P.act(lambda e, ps=ps, q=q, xs=xs: e.activation(xs[:, q * 512:(q + 1) * 512], ps[:, :], AF.Silu), reads=[ps], writes=[xs])
            btok = btp.next()
            ps = P.psum_bank()
            P.pe(lambda e, ps=ps: e.matmul(ps[:, :], ones1[0:1, :], cbrow[0:1, 2048:2560], start=True, stop=False), reads=[ones1, cbrow], writes=[ps])
            for i in range(4):
                ct = 16 + i
                for k in range(4):
                    last = (i == 3 and k == 3)
                    P.pe(lambda e, ps=ps, i=i, ct=ct, k=k, last=last, xpre=xpre: e.matmul(ps[:, i * 128:(i + 1) * 128], xpre[:, ct, k:k + 128], Dg[:, ct, k, :], start=False, stop=last, skip_group_check=True),
                         reads=[xpre, Dg], writes=[ps])
            P.act(lambda e, ps=ps, btok=btok: e.activation(btok[:], ps[:, :], AF.Silu), reads=[ps], writes=[btok])
            BT, CT = BTp.next(), CTp.next()
            for dst, base in ((BT, 16), (CT, 20)):
                ps = P.psum_bank()
                for i in range(4):
                    ct = base + i
                    mm_acc(P, ps[:, i * 128:(i + 1) * 128], ps, [(Dg[:, ct, k, :], xpre[:, ct, k:k + 128]) for k in range(4)], [xpre, Dg])
                for i in range(4):
                    ct = base + i
                    P.act(lambda e, ps=ps, i=i, ct=ct, dst=dst: e.activation(dst[:, i, :], ps[:, i * 128:(i + 1) * 128], AF.Silu, bias=cbcol[:, ct:ct + 1]), reads=[ps, cbcol], writes=[dst])
            ps = P.psum_bank()
            for g in range(4):
                P.pe(lambda e, ps=ps, g=g, BT=BT, CT=CT: e.matmul(ps[:, g * 128:(g + 1) * 128], BT[:, g, :], CT[:, g, :], start=True, stop=True), reads=[BT, CT], writes=[ps])
            cbt = cbtp.next()
            P.dve(lambda e, ps=ps, cbt=cbt: e.tensor_tensor(cbt[:, :, :], ps[:, :].rearrange("p (g t) -> p g t", g=4), tri[:, :].unsqueeze(1).broadcast_to([128, 4, 128]), ALU.mult),
                  reads=[ps, tri], writes=[cbt])
            yg = ygp.next()
            ygr = [Buf(yg.h, "yg_g%d" % g_) for g_ in range(4)]
            for g in range(4):
                gs = slice(g * 512, (g + 1) * 512)
                xdt, xw = xdtp.next(), xwp.next()
                P.dve(lambda e, xdt=xdt, xs=xs, dts=dts, g=g, gs=gs: e.tensor_tensor(xdt[:, :].rearrange("p (h d) -> p h d", h=8), xs[:, gs].rearrange("p (h d) -> p h d", h=8),
                                                                          dts[:, 1, g * 8:(g + 1) * 8].unsqueeze(2).broadcast_to([128, 8, 64]), ALU.mult), reads=[xs, dts], writes=[xdt])
                P.dve(lambda e, xw=xw, xs=xs, dts=dts, g=g, gs=gs: e.tensor_tensor(xw[:, :].rearrange("p (h d) -> p h d", h=8), xs[:, gs].rearrange("p (h d) -> p h d", h=8),
                                                                         dts[:, 3, g * 8:(g + 1) * 8].unsqueeze(2).broadcast_to([128, 8, 64]), ALU.mult), reads=[xs, dts], writes=[xw])
                MTs = []
                for half in range(2):
                    pr = P.psum_bank()
                    for i in range(4):
                        h = g * 8 + half * 4 + i
                        Wh = Wp.next()
                        P.act(lambda e, Wh=Wh, h=h, dts=dts: e.activation(Wh[:], ustr[:], AF.Copy, scale=dts[:, 2, h:h + 1]), reads=[ustr, dts], writes=[Wh])
                        P.pe(lambda e, pr=pr, i=i, Wh=Wh: e.matmul(pr[:, i * 128:(i + 1) * 128], Wh[:], tri[:], start=True, stop=True), reads=[tri, Wh], writes=[pr])
                    eL = eLp.next()
                    P.act(lambda e, pr=pr, eL=eL: e.activation(eL[:, :, :], pr[:, :].rearrange("p (i t) -> p i t", i=4), AF.Exp), reads=[pr], writes=[eL])
                    MT = MTp.next()
                    P.dve(lambda e, MT=MT, eL=eL, g=g, cbt=cbt: e.tensor_tensor(MT[:, :, :], eL[:, :, :], cbt[:, g, :].unsqueeze(1).broadcast_to([128, 4, 128]), ALU.mult), reads=[eL, cbt], writes=[MT])
                    MTs.append(MT)
                py = P.psum_bank()
                for hh in range(8):
                    h = g * 8 + hh
                    P.pe(lambda e, py=py, hh=hh, h=h, MT=MTs[hh // 4], xdt=xdt: e.matmul(py[:, hh * 64:(hh + 1) * 64], MT[:, hh % 4, :], xdt[:, hh * 64:(hh + 1) * 64], start=True, stop=True),
                         reads=[MTs[hh // 4], xdt], writes=[py])
                po = P.psum_bank()
                P.pe(lambda e, po=po, g=g, CT=CT: e.matmul(po[:, :], CT[:, g, :], stTb[:, g, :], start=True, stop=True), reads=[CT, stTb], writes=[po])
                yo = yop.next()
                P.dve(lambda e, yo=yo, po=po, g=g, ecs=ecs: e.tensor_tensor(yo[:, :].rearrange("p (h d) -> p h d", h=8), po[:, :].rearrange("p (h d) -> p h d", h=8),
                                                                      ecs[:, g * 8:(g + 1) * 8].unsqueeze(2).broadcast_to([128, 8, 64]), ALU.mult), reads=[po, ecs], writes=[yo])
                P.dve(lambda e, yo=yo, py=py: e.tensor_tensor(yo[:], py[:, :], yo[:], ALU.add), reads=[py, yo], writes=[yo])
                yd = yop.next()
                P.dve(lambda e, yd=yd, xs=xs, g=g, gs=gs: e.tensor_tensor(yd[:, :].rearrange("p (h d) -> p h d", h=8), xs[:, gs].rearrange("p (h d) -> p h d", h=8),
                                                                    dskb[:, g * 8:(g + 1) * 8].unsqueeze(2).broadcast_to([128, 8, 64]), ALU.mult), reads=[xs, dskb], writes=[yd])
                P.dve(lambda e, yd=yd, yo=yo: e.tensor_tensor(yd[:], yd[:], yo[:], ALU.add), reads=[yd, yo], writes=[yd])
                P.dve(lambda e, yd=yd, yg=yg, zs=zs, gs=gs: e.tensor_tensor(yg[:, gs], yd[:], zs[:, gs], ALU.mult), reads=[yd, zs], writes=[ygr[g]])
                pu = P.psum_bank()
                P.pe(lambda e, pu=pu, g=g, btok=btok, xw=xw, gs=gs: e.matmul(pu[:, :], btok[:, g * 128:(g + 1) * 128], xw[:, :], start=True, stop=True), reads=[btok, xw], writes=[pu])
                P.dve(lambda e, g=g, ecs=ecs: e.tensor_tensor(stT[:, g, :].rearrange("p (h d) -> p h d", h=8), stT[:, g, :].rearrange("p (h d) -> p h d", h=8),
                                                            ecs[:, 64 + g * 8:64 + (g + 1) * 8].unsqueeze(2).broadcast_to([128, 8, 64]), ALU.mult), reads=[stT, ecs], writes=[stT])
                P.dve(lambda e, g=g, pu=pu: e.tensor_tensor(stT[:, g, :], pu[:, :], stT[:, g, :], ALU.add), reads=[pu, stT], writes=[stT])
                P.act(lambda e, g=g: e.activation(stTb[:, g, :], stT[:, g, :], AF.Copy), reads=[stT], writes=[stTb])
            ss = C.small.next()
            yn = ynp.next()
            P.act(lambda e, yn=yn, ss=ss, yg=yg: e.activation(yn[:], yg[:], AF.Square, accum_out=ss[:, 0:1]), reads=ygr, writes=[yn, ss])
            P.act(lambda e, ss=ss: e.activation(ss[:, 1:2], ss[:, 0:1], AF.Sqrt, scale=1.0 / 2048.0, bias=C.eps[:, 0:1]), reads=[ss, C.eps], writes=[ss])
            P.dve(lambda e, ss=ss: e.reciprocal(ss[:, 2:3], ss[:, 1:2]), reads=[ss], writes=[ss])
            P.dve(lambda e, yn=yn, yg=yg, ss=ss: e.tensor_scalar(yn[:], yg[:], ss[:, 2:3], None, ALU.mult), reads=ygr + [ss], writes=[yn])
            P.dma(yn_out.h[ch * 128:(ch + 1) * 128, :], yn[:], reads=[yn], writes=[yn_out])


def phase_mamba_b(P, L, h_in, h_out, yn_in, odnorm_ap, w_out_ap):
    NCH = L // 128
    with P.phase():
        C = Ctx(P, stage_w=2048, stage_n=2)
        ident = C.ident
        go = load_gain(P, odnorm_ap, 16)
        w_out = P.sbuf("w_out", [128, 16, 1024], BF16)
        load_w(P, C, w_out, w_out_ap, go)
        hpool = P.pool_of("hblk", [128, 1024], F32, 3)
        ynp = P.pool_of("ynl", [128, 2048], BF16, 3)
        ynTp = P.pool_of("ynT", [128, 16, 128], BF16, 2)
        nxt = None
        P.begin_sched()

        def load(ch):
            hb, yn = hpool.next(), ynp.next()
            P.dma(hb[:], h_in.h[ch * 128:(ch + 1) * 128, :], reads=[h_in], writes=[hb])
            P.dma(yn[:], yn_in.h[ch * 128:(ch + 1) * 128, :], reads=[yn_in], writes=[yn])
            return hb, yn
        nxt = load(0)
        for ch in range(NCH):
            hb, yn = nxt
            if ch + 1 < NCH:
                nxt = load(ch + 1)
            ynT = ynTp.next()
            for c0 in (0, 8):
                pt = P.psum_tbank()
                for c in range(8):
                    P.pe(lambda e, c=c, c0=c0, pt=pt, yn=yn: e.transpose(pt[:, c * 128:(c + 1) * 128], yn[:, (c0 + c) * 128:(c0 + c + 1) * 128], ident[:]), reads=[yn, ident], writes=[pt])
                P.act(lambda e, pt=pt, c0=c0, ynT=ynT: e.activation(ynT[:, c0:c0 + 8, :], pt[:].rearrange("p (c t) -> p c t", c=8), AF.Copy), reads=[pt], writes=[ynT])
            for hf in range(2):
                po = P.psum_bank()
                mm_acc(P, po[:, :], po, [(ynT[:, kc, :], w_out[:, kc, hf * 512:(hf + 1) * 512]) for kc in range(16)], [ynT, w_out])
                P.dve(lambda e, po=po, hf=hf, hb=hb: e.tensor_tensor(hb[:, hf * 512:(hf + 1) * 512], po[:, :], hb[:, hf * 512:(hf + 1) * 512], ALU.add), reads=[po, hb], writes=[hb])
            P.dma(h_out.h[ch * 128:(ch + 1) * 128, :], hb[:], reads=[hb], writes=[h_out])


W_SHAPES = {
    "mem_norm": [1024], "norm_mix": [2, 1024], "norm_xattn": [2, 1024], "norm_ffn": [2, 1024],
    "xa_wq": [2, 1024, 1024], "xa_wk": [2, 1024, 1024], "xa_wv": [2, 1024, 1024], "xa_wo": [2, 1024, 1024],
    "ev_w_in": [1, 1024, 2560], "ev_s5_lam_re": [1, 16, 64], "ev_s5_lam_im": [1, 16, 64], "ev_s5_log_dt": [1, 16],
    "ev_s5_b_re": [1, 16, 64, 16], "ev_s5_b_im": [1, 16, 64, 16], "ev_s5_c_re": [1, 16, 16, 64], "ev_s5_c_im": [1, 16, 16, 64],
    "ev_s5_d": [1, 256], "ev_s5_w_glu": [1, 256, 256], "ev_s5_b_glu": [1, 256], "ev_w_out": [1, 1024, 1024],
    "ev_ffn_w1": [1, 1024, 2816], "ev_ffn_w3": [1, 1024, 2816], "ev_ffn_w2": [1, 2816, 1024],
    "od_w_in": [1, 1024, 5152], "od_conv_w": [1, 4, 3072], "od_conv_b": [1, 3072], "od_dt_bias": [1, 32], "od_a_log": [1, 32],
    "od_d": [1, 32], "od_norm": [1, 2048], "od_w_out": [1, 2048, 1024], "od_router": [1, 1024, 8],
    "od_moe_w1": [1, 8, 1024, 3584], "od_moe_w3": [1, 8, 1024, 3584], "od_moe_w2": [1, 8, 3584, 1024], "final_norm": [1024],
}


def build_program(L, phases=None):
    nc = bass.Bass("TRN2", target_bir_lowering=False)
    P = Prog(nc)
    x = P.dram("x", [L, 1024], F32, kind="ExternalInput")
    mem = nc.dram_tensor("mem", [256, 1024], F32, kind="ExternalInput").ap()
    pos = nc.dram_tensor("positions", [L], I32, kind="ExternalInput").ap()
    out = P.dram("out", [L, 1024], F32, kind="ExternalOutput")
    w = {n: nc.dram_tensor(n, s, F32, kind="ExternalInput").ap() for n, s in W_SHAPES.items()}
    hA = P.dram("hA", [L, 1024], F32)
    hB = P.dram("hB", [L, 1024], F32)
    yn = P.dram("yn", [L, 2048], BF16)
    steps = [
        lambda i, o: phase_even_mixer(P, L, i, o, pos, w["norm_mix"][0], w["ev_w_in"][0], w["ev_s5_lam_re"][0], w["ev_s5_lam_im"][0], w["ev_s5_log_dt"][0],
                                      w["ev_s5_b_re"][0], w["ev_s5_b_im"][0], w["ev_s5_c_re"][0], w["ev_s5_c_im"][0], w["ev_s5_d"][0], w["ev_s5_w_glu"][0],
                                      w["ev_s5_b_glu"][0], w["ev_w_out"][0]),
        lambda i, o: phase_xattn(P, L, i, o, mem, w["mem_norm"], w["norm_xattn"][0], w["xa_wq"][0], w["xa_wk"][0], w["xa_wv"][0], w["xa_wo"][0]),
        lambda i, o: phase_ffn(P, L, i, o, w["norm_ffn"][0], [w["ev_ffn_w1"][0]], [w["ev_ffn_w3"][0]], [w["ev_ffn_w2"][0]], 2816),
        lambda i, o: (phase_mamba_a(P, L, i, yn, w["norm_mix"][1], w["od_w_in"][0], w["od_conv_w"][0], w["od_conv_b"][0], w["od_dt_bias"][0], w["od_a_log"][0], w["od_d"][0]),
                      phase_mamba_b(P, L, i, o, yn, w["od_norm"][0], w["od_w_out"][0])),
        lambda i, o: phase_xattn(P, L, i, o, mem, w["mem_norm"], w["norm_xattn"][1], w["xa_wq"][1], w["xa_wk"][1], w["xa_wv"][1], w["xa_wo"][1]),
        lambda i, o: phase_ffn(P, L, i, None, w["norm_ffn"][1], [w["od_moe_w1"][0][e] for e in range(8)], [w["od_moe_w3"][0][e] for e in range(8)],
                               [w["od_moe_w2"][0][e] for e in range(8)], 3584, router_ap=w["od_router"][0], final_norm_ap=w["final_norm"], out_ap=o.h),
    ]
    if phases is None:
        phases = list(range(len(steps)))
    cur = x
    scratch = [hA, hB]
    for n, pi in enumerate(phases):
        dst = out if n == len(phases) - 1 else scratch[n % 2]
        steps[pi](cur, dst)
        cur = dst
    P.finish()
    return nc, P


def kernel(_phases=None, **inputs):
    x = np.asarray(inputs["x"], dtype=np.float32)
    B, L, _ = x.shape
    nc, P = build_program(L, _phases)
    shared = {n: np.ascontiguousarray(np.asarray(inputs[n], dtype=np.float32)) for n in W_SHAPES}
    mem = np.asarray(inputs["mem"], dtype=np.float32)
    pos = np.asarray(inputs["positions"], dtype=np.int32)
    in_maps = []
    for b in range(B):
        m = dict(shared)
        m["x"] = np.ascontiguousarray(x[b])
        m["mem"] = np.ascontiguousarray(mem[b])
        m["positions"] = np.ascontiguousarray(pos[b])
        in_maps.append(m)
    res = run_bass_kernel_spmd(nc, in_maps, core_ids=list(range(B)))
    return np.stack([np.asarray(r["out"], dtype=np.float32) for r in res.results], axis=0)
```

```python
import math
from contextlib import ExitStack
import numpy as np
import concourse.bass as bass
import concourse.mybir as mybir
from concourse.bass_utils import run_bass_kernel_spmd

F32 = mybir.dt.float32
BF16 = mybir.dt.bfloat16
I32 = mybir.dt.int32
AF = mybir.ActivationFunctionType
ALU = mybir.AluOpType
AX = mybir.AxisListType

SAME_ENGINE_SYNC = {"pe": False, "dve": True, "act": False, "pool": True, "sp": False}
STRICT_ELEMS = 256
N_DMA_SLOTS = 8
SCHEDULE = True


class Buf:
    def __init__(self, h, name, strict=False):
        self.h = h
        self.name = name
        self.writers = {}
        self.readers = {}
        self.strict = strict

    def __getitem__(self, idx):
        return self.h[idx]


class _CallRec:
    def __getattr__(self, name):
        def f(*a, **k):
            self.__dict__["call"] = (name, a, k)
            return self
        return f


class Prog:
    def __init__(self, nc):
        self.nc = nc
        self.es = ExitStack()
        self.es_global = self.es
        self._consts = {}
        self.eng = {"pe": nc.tensor, "dve": nc.vector, "act": nc.scalar, "pool": nc.gpsimd, "sp": nc.sync}
        self.sem = {}
        self.cnt = {}
        self.waited = {}
        for e in self.eng:
            self.sem[e] = self.es.enter_context(nc.semaphore("s_" + e))
            self.cnt[e] = 0
            self.waited[e] = {}
        self.dma_sem = {}
        self.dma_cnt = {}
        for q in ("sp", "act", "pool"):
            self.dma_sem[q] = [self.es.enter_context(nc.semaphore("d_%s%d" % (q, i))) for i in range(N_DMA_SLOTS)]
            self.dma_cnt[q] = 0
        self.semobj = dict(self.sem)
        for q in self.dma_sem:
            for i, s in enumerate(self.dma_sem[q]):
                self.semobj[("dma", q, i)] = s
        self._psum = []
        self._psum_i = 0
        self._ident = None
        self.n_inst = 0
        self.n_wait = 0
        self.uid = 0
        self._rec = None
        self.psum_banks()
        self.identity_bf16()
        for cv in (1e-6, 1.0, 0.0, -math.pi, math.pi, 0.5, 0.25):
            self.const(cv)

    def sbuf(self, name, shape, dtype):
        self.uid += 1
        h = self.es.enter_context(self.nc.sbuf_tensor("%s_%d" % (name, self.uid), list(shape), dtype))
        return Buf(h, name, strict=(int(np.prod(shape[1:])) <= STRICT_ELEMS))

    def psum(self, name, shape, dtype=F32):
        self.uid += 1
        h = self.es.enter_context(self.nc.psum_tensor("%s_%d" % (name, self.uid), list(shape), dtype))
        return Buf(h, name)

    def dram(self, name, shape, dtype, kind="Internal"):
        h = self.nc.dram_tensor(name, list(shape), dtype, kind=kind)
        return Buf(h.ap(), name)

    def pool_of(self, name, shape, dtype, n, space="sbuf"):
        bufs = [(self.sbuf if space == "sbuf" else self.psum)("%s%d" % (name, i), shape, dtype) for i in range(n)]
        return Rot(bufs)

    N_FBANK = 6

    def psum_banks(self):
        if not self._psum:
            g = self.es_global
            self._psum = [Buf(g.enter_context(self.nc.psum_tensor("bank%d" % i, [128, 512], F32)), "bank") for i in range(self.N_FBANK)]
            self._psumt = [Buf(g.enter_context(self.nc.psum_tensor("tbank%d" % i, [128, 1024], BF16)), "tbank") for i in range(8 - self.N_FBANK)]
            self._psumt_i = 0
        return self._psum

    def psum_bank(self):
        banks = self.psum_banks()
        b = banks[self._psum_i % len(banks)]
        self._psum_i += 1
        return b

    def psum_tbank(self):
        self.psum_banks()
        b = self._psumt[self._psumt_i % len(self._psumt)]
        self._psumt_i += 1
        return b

    def _deps(self, reads, writes):
        deps = {}
        self._strict_keys = set()
        for b in reads:
            for k, v in b.writers.items():
                if deps.get(k, 0) < v:
                    deps[k] = v
                if b.strict:
                    self._strict_keys.add(k)
        for b in writes:
            for d in (b.writers, b.readers):
                for k, v in d.items():
                    if deps.get(k, 0) < v:
                        deps[k] = v
                    if b.strict:
                        self._strict_keys.add(k)
        return deps

    def _emit_waits(self, e, deps):
        eng = self.eng[e]
        w = self.waited[e]
        for k, v in deps.items():
            if k == e and not SAME_ENGINE_SYNC[e]:
                continue
            if k == e and SAME_ENGINE_SYNC[e] == "strict" and k not in self._strict_keys:
                continue
            if w.get(k, 0) >= v:
                continue
            eng.wait_ge(self.semobj[k], v)
            w[k] = v
            self.n_wait += 1

    def _record(self, key, val, reads, writes):
        for b in reads:
            if b.readers.get(key, 0) < val:
                b.readers[key] = val
        for b in writes:
            if b.readers:
                b.writers = {key: val}
                b.readers = {}
            else:
                b.writers[key] = val

    def op(self, e, fn, reads=(), writes=(), cost=None):
        if self._rec is not None:
            r = _CallRec()
            fn(r)
            name, a, k = r.call
            self._rec.append((e, (lambda eng, name=name, a=a, k=k: getattr(eng, name)(*a, **k)), tuple(reads), tuple(writes), None, cost))
            return
        deps = self._deps(reads, writes)
        self._emit_waits(e, deps)
        inst = fn(self.eng[e])
        self.cnt[e] += 1
        inst.then_inc(self.sem[e], 1)
        self._record(e, self.cnt[e], reads, writes)
        self.n_inst += 1

    def pe(self, fn, reads=(), writes=()):
        self.op("pe", fn, reads, writes)

    def dve(self, fn, reads=(), writes=()):
        self.op("dve", fn, reads, writes)

    def act(self, fn, reads=(), writes=()):
        self.op("act", fn, reads, writes)

    def gp(self, fn, reads=(), writes=()):
        self.op("pool", fn, reads, writes)

    def dma(self, out, in_, reads=(), writes=(), q="sp", **kw):
        if self._rec is not None:
            self._rec.append((q, None, tuple(reads), tuple(writes), (out, in_, kw), None))
            return
        k = self.dma_cnt[q]
        self.dma_cnt[q] += 1
        slot = k % N_DMA_SLOTS
        key = ("dma", q, slot)
        deps = self._deps(reads, writes)
        prev = 16 * (k // N_DMA_SLOTS)
        if prev > 0:
            deps[key] = max(deps.get(key, 0), prev)
        self._emit_waits(q, deps)
        inst = self.eng[q].dma_start(out=out, in_=in_, **kw)
        inst.then_inc(self.semobj[key], 16)
        self._record(key, prev + 16, reads, writes)
        self.n_inst += 1

    def identity_bf16(self):
        if self._ident is None:
            idf = Buf(self.es_global.enter_context(self.nc.sbuf_tensor("identf", [128, 128], F32)), "identf")
            idb = Buf(self.es_global.enter_context(self.nc.sbuf_tensor("identb", [128, 128], BF16)), "identb")
            self.gp(lambda e: e.memset(idf[:], 1.0), writes=[idf])
            self.gp(lambda e: e.affine_select(idf[:], idf[:], [[-1, 128]], ALU.is_equal, 0.0, base=0, channel_multiplier=1),
                    reads=[idf], writes=[idf])
            self.gp(lambda e: e.tensor_copy(idb[:], idf[:]), reads=[idf], writes=[idb])
            self._ident = idb
            self._identf = idf
        return self._ident

    EST = {"pe": 0.25, "act": 0.55, "dve": 0.65, "pool": 1.2, "sp": 0.15}
    LAT = {"pe": 0.3, "act": 0.5, "dve": 0.5, "pool": 0.8, "sp": 2.5}

    def begin_sched(self):
        if SCHEDULE:
            self._rec = []

    def end_sched(self):
        rec = self._rec
        self._rec = None
        if not rec:
            return
        n = len(rec)
        preds = [None] * n
        lastw = {}
        readers = {}
        for i, (e, fn, reads, writes, dmaargs, cost) in enumerate(rec):
            p = set()
            for b in reads:
                w = lastw.get(id(b))
                if w is not None:
                    p.update(w)
            for b in writes:
                w = lastw.get(id(b))
                if w is not None:
                    p.update(w)
                r = readers.get(id(b))
                if r:
                    p.update(r)
            p.discard(i)
            preds[i] = p
            for b in reads:
                readers.setdefault(id(b), []).append(i)
            for b in writes:
                if readers.get(id(b)):
                    lastw[id(b)] = [i]
                    readers[id(b)] = []
                else:
                    lastw.setdefault(id(b), []).append(i)
        succs = [[] for _ in range(n)]
        npred = [0] * n
        for i in range(n):
            npred[i] = len(preds[i])
            for j in preds[i]:
                succs[j].append(i)
        import heapq
        fin = [0.0] * n
        rdy = [0.0] * n
        efree = {}
        ready = {}
        for i in range(n):
            if npred[i] == 0:
                ready.setdefault(rec[i][0], []).append(i)
        for e in ready:
            heapq.heapify(ready[e])
        done = 0
        while done < n:
            best, beste, bestt = None, None, None
            for e, hq in ready.items():
                if not hq:
                    continue
                i = hq[0]
                t = max(efree.get(e, 0.0), rdy[i])
                if bestt is None or t < bestt or (t == bestt and i < best):
                    best, beste, bestt = i, e, t
            i = heapq.heappop(ready[beste])
            e, fn, reads, writes, dmaargs, cost = rec[i]
            c = cost if cost is not None else self.EST[e]
            efree[e] = bestt + c
            fin[i] = bestt + c + self.LAT[e]
            if dmaargs is None:
                self.op(e, fn, reads, writes)
            else:
                self.dma(dmaargs[0], dmaargs[1], reads=reads, writes=writes, q=e, **dmaargs[2])
            done += 1
            for j in succs[i]:
                npred[j] -= 1
                if rdy[j] < fin[i]:
                    rdy[j] = fin[i]
                if npred[j] == 0:
                    heapq.heappush(ready.setdefault(rec[j][0], []), j)

    def barrier(self):
        final = {}
        for e in self.eng:
            if self.cnt[e] > 0:
                final[e] = self.cnt[e]
        for q in self.dma_cnt:
            k = self.dma_cnt[q]
            for slot in range(N_DMA_SLOTS):
                n = (k - slot + N_DMA_SLOTS - 1) // N_DMA_SLOTS
                if n > 0:
                    final[("dma", q, slot)] = 16 * n
        for e in self.eng:
            self._emit_waits(e, {k: v for k, v in final.items() if k != e})

    def phase(self):
        return _Phase(self)

    def const(self, val):
        key = float(val)
        if key not in self._consts:
            c = Buf(self.es_global.enter_context(self.nc.sbuf_tensor("const_%d" % len(self._consts), [128, 1], F32)), "const")
            self.gp(lambda e: e.memset(c[:], key), writes=[c])
            self._consts[key] = c
        return self._consts[key]

    def finish(self):
        final = {}
        for e in self.eng:
            if self.cnt[e] > 0:
                final[e] = self.cnt[e]
        for q in self.dma_cnt:
            k = self.dma_cnt[q]
            for slot in range(N_DMA_SLOTS):
                n = (k - slot + N_DMA_SLOTS - 1) // N_DMA_SLOTS
                if n > 0:
                    final[("dma", q, slot)] = 16 * n
        self._emit_waits("sp", {k: v for k, v in final.items() if k != "sp"})
        self.es_global.close()


class _Phase:
    def __init__(self, P):
        self.P = P

    def __enter__(self):
        self.saved = self.P.es
        self.stack = ExitStack()
        self.P.es = self.stack
        return self

    def __exit__(self, *a):
        self.P.end_sched()
        self.P.barrier()
        self.P.es = self.saved
        self.stack.close()
        return False


class Rot:
    def __init__(self, bufs):
        self.bufs = bufs
        self.i = 0

    def next(self):
        b = self.bufs[self.i % len(self.bufs)]
        self.i += 1
        return b


EPS = 1e-6
D = 1024
KC = 8


class Ctx:
    def __init__(self, P, stage_w=2048, stage_n=3, junk_w=1024):
        self.P = P
        self.ident = P.identity_bf16()
        self.junk = P.pool_of("junk", [128, junk_w], BF16, 2)
        self.small = P.pool_of("small", [128, 4], F32, 4)
        self.xs = P.pool_of("xs", [128, 1024], BF16, 2)
        self.stage = P.pool_of("stage", [128, stage_w], F32, stage_n)
        self.stage_w = stage_w
        self.eps = P.const(EPS)


def load_gain(P, vec_ap, n):
    g = P.sbuf("gain", [128, n], F32)
    P.dma(g[:], vec_ap.rearrange("(c p) -> p c", p=128), writes=[g], allow_slow_non_contiguous=True)
    return g


def load_w(P, C, dst, src_ap, gain=None, kc0=0, nkc=None, f0=0, nf=None, dst_f0=0, eng="act"):
    K, F = src_ap.shape
    if nkc is None:
        nkc = K // 128
    if nf is None:
        nf = F
    FS = C.stage_w
    for kc in range(nkc):
        for c0 in range(0, nf, FS):
            n = min(FS, nf - c0)
            st = C.stage.next()
            P.dma(st[:, 0:n], src_ap[(kc0 + kc) * 128:(kc0 + kc + 1) * 128, f0 + c0:f0 + c0 + n], writes=[st])
            o = dst[:, kc, dst_f0 + c0:dst_f0 + c0 + n]
            if eng == "act":
                if gain is not None:
                    gap = gain[:, kc0 + kc:kc0 + kc + 1]
                    P.act(lambda e, o=o, st=st, n=n, gap=gap: e.activation(o, st[:, 0:n], AF.Copy, scale=gap), reads=[st, gain], writes=[dst])
                else:
                    P.act(lambda e, o=o, st=st, n=n: e.activation(o, st[:, 0:n], AF.Copy), reads=[st], writes=[dst])
            elif gain is not None:
                gap = gain[:, kc0 + kc:kc0 + kc + 1]
                P.op(eng, lambda e, o=o, st=st, n=n, gap=gap: e.tensor_scalar(o, st[:, 0:n], gap, None, ALU.mult),
                     reads=[st, gain], writes=[dst])
            else:
                P.op(eng, lambda e, o=o, st=st, n=n: e.tensor_copy(o, st[:, 0:n]), reads=[st], writes=[dst])


def norm_tile(P, C, src_ap, src_buf, hnT, col0, width=1024, eps_scale=None, track=None):
    nch = width // 128
    junk = C.junk.next()
    ss = C.small.next()
    srcs = list(src_buf) if isinstance(src_buf, (list, tuple)) else [src_buf]
    P.act(lambda e: e.activation(junk[:, 0:width], src_ap, AF.Square, accum_out=ss[:, 0:1]),
          reads=srcs, writes=[junk, ss])
    P.act(lambda e: e.activation(ss[:, 1:2], ss[:, 0:1], AF.Sqrt, scale=1.0 / width, bias=C.eps[:, 0:1]),
          reads=[ss, C.eps], writes=[ss])
    P.dve(lambda e: e.reciprocal(ss[:, 2:3], ss[:, 1:2]), reads=[ss], writes=[ss])
    xs = C.xs.next() if width <= 1024 else C.xs2.next()
    P.dve(lambda e: e.tensor_scalar(xs[:, 0:width], src_ap, ss[:, 2:3], None, ALU.mult), reads=srcs + [ss], writes=[xs])
    for c0 in range(0, nch, 8):
        pt = P.psum_tbank()
        for c in range(8):
            P.pe(lambda e, c=c: e.transpose(pt[:, c * 128:(c + 1) * 128], xs[:, (c0 + c) * 128:(c0 + c + 1) * 128], C.ident[:]),
                 reads=[xs, C.ident], writes=[pt])
        P.act(lambda e: e.activation(hnT[:, c0:c0 + 8, col0:col0 + 128], pt[:].rearrange("p (c t) -> p c t", c=8), AF.Copy),
              reads=[pt], writes=[track if track is not None else hnT])
    return ss


def mm_acc(P, ps_ap, ps_buf, pairs, reads):
    n = len(pairs)
    for i, (l, r) in enumerate(pairs):
        P.pe(lambda e, l=l, r=r, i=i: e.matmul(ps_ap, l, r, start=(i == 0), stop=(i == n - 1)), reads=reads, writes=[ps_buf])


def h_view(h_dram, t0, nt):
    return h_dram[t0:t0 + nt * 128, :].rearrange("(j p) d -> p j d", p=128)


def phase_xattn(P, L, h_in, h_out, mem_ap, mem_norm_ap, nx_ap, wq_ap, wk_ap, wv_ap, wo_ap):
    with P.phase():
        C = Ctx(P, stage_n=2)
        gq = load_gain(P, nx_ap, 8)
        gm = load_gain(P, mem_norm_ap, 8)
        wq = P.sbuf("wq", [128, 8, 1024], BF16)
        wk = P.sbuf("wk", [128, 8, 1024], BF16)
        wv = P.sbuf("wv", [128, 8, 1024], BF16)
        wo = P.sbuf("wo", [128, 8, 1024], BF16)
        load_w(P, C, wk, wk_ap, gm)
        load_w(P, C, wv, wv_ap, gm)
        load_w(P, C, wq, wq_ap, gq)
        load_w(P, C, wo, wo_ap, None)
        ones = P.sbuf("ones", [128, 128], BF16)
        P.gp(lambda e: e.memset(ones[:], 1.0), writes=[ones])
        memT = P.sbuf("memT", [128, 8, 256], BF16)
        mt = P.sbuf("mt", [128, 2, 1024], F32)
        P.dma(mt[:], h_view(mem_ap, 0, 2), writes=[mt])
        for j in range(2):
            norm_tile(P, C, mt[:, j, :], mt, memT, j * 128)
        kT = P.sbuf("kT", [128, 8, 256], BF16)
        for fc in range(8):
            ps = P.psum_bank()
            mm_acc(P, ps[:, 0:256], ps, [(wk[:, kc, fc * 128:(fc + 1) * 128], memT[:, kc, :]) for kc in range(8)], [wk, memT])
            P.act(lambda e, fc=fc, ps=ps: e.activation(kT[:, fc, :], ps[:, 0:256], AF.Copy), reads=[ps], writes=[kT])
        v = P.sbuf("v", [128, 2, 1024], BF16)
        for j in range(2):
            for hf in range(2):
                ps = P.psum_bank()
                mm_acc(P, ps[:, :], ps, [(memT[:, kc, j * 128:(j + 1) * 128], wv[:, kc, hf * 512:(hf + 1) * 512]) for kc in range(8)], [wv, memT])
                P.act(lambda e, j=j, hf=hf, ps=ps: e.activation(v[:, j, hf * 512:(hf + 1) * 512], ps[:, :], AF.Copy), reads=[ps], writes=[v])
        P.begin_sched()
        NB = L // 512
        hpool = P.pool_of("hblk", [128, 4, 1024], F32, 2)
        hnTp = P.pool_of("hnT", [128, 8, 512], BF16, 2)
        qTp = P.pool_of("qT", [128, 8, 512], BF16, 2)
        oTp = P.pool_of("oT", [128, 8, 512], BF16, 2)
        expp = P.pool_of("expT", [128, 2, 512], BF16, 3)
        rdp = P.pool_of("rden", [128, 512], F32, 3)
        hb_next = hpool.next()
        P.dma(hb_next[:], h_view(h_in.h, 0, 4), reads=[h_in], writes=[hb_next])
        for b in range(NB):
            hb = hb_next
            if b + 1 < NB:
                hb_next = hpool.next()
                P.dma(hb_next[:], h_view(h_in.h, (b + 1) * 512, 4), reads=[h_in], writes=[hb_next])
            hnT = hnTp.next()
            for j in range(4):
                norm_tile(P, C, hb[:, j, :], hb, hnT, j * 128)
            qT = qTp.next()
            for fc in range(8):
                ps = P.psum_bank()
                mm_acc(P, ps[:, :], ps, [(wq[:, kc, fc * 128:(fc + 1) * 128], hnT[:, kc, :]) for kc in range(8)], [wq, hnT])
                P.act(lambda e, fc=fc, ps=ps: e.activation(qT[:, fc, :], ps[:, :], AF.Copy, scale=1.0 / 16.0), reads=[ps], writes=[qT])
            oT = oTp.next()
            for hd in range(4):
                ex = expp.next()
                for j in range(2):
                    ps = P.psum_bank()
                    mm_acc(P, ps[:, :], ps, [(kT[:, fc, j * 128:(j + 1) * 128], qT[:, fc, :]) for fc in (2 * hd, 2 * hd + 1)], [kT, qT])
                    P.act(lambda e, j=j, ps=ps: e.activation(ex[:, j, :], ps[:, :], AF.Exp), reads=[ps], writes=[ex])
                den = P.psum_bank()
                mm_acc(P, den[:, :], den, [(ones[:, :], ex[:, j, :]) for j in range(2)], [ones, ex])
                rd = rdp.next()
                P.dve(lambda e, rd=rd, den=den: e.reciprocal(rd[:], den[:, :]), reads=[den], writes=[rd])
                for fc in (2 * hd, 2 * hd + 1):
                    ps = P.psum_bank()
                    mm_acc(P, ps[:, :], ps, [(v[:, j, fc * 128:(fc + 1) * 128], ex[:, j, :]) for j in range(2)], [v, ex])
                    P.dve(lambda e, fc=fc, ps=ps, rd=rd: e.tensor_tensor(oT[:, fc, :], ps[:, :], rd[:], ALU.mult), reads=[ps, rd], writes=[oT])
            for j in range(4):
                for hf in range(2):
                    ps = P.psum_bank()
                    mm_acc(P, ps[:, :], ps, [(oT[:, fc, j * 128:(j + 1) * 128], wo[:, fc, hf * 512:(hf + 1) * 512]) for fc in range(8)], [oT, wo])
                    P.dve(lambda e, j=j, hf=hf, ps=ps: e.tensor_tensor(hb[:, j, hf * 512:(hf + 1) * 512], ps[:, :], hb[:, j, hf * 512:(hf + 1) * 512], ALU.add),
                          reads=[ps, hb], writes=[hb])
            P.dma(h_view(h_out.h, b * 512, 4), hb[:], reads=[hb], writes=[h_out])


def phase_ffn(P, L, h_in, h_out, norm_ap, w1_aps, w3_aps, w2_aps, FF, router_ap=None,
              final_norm_ap=None, out_ap=None, TS=2048):
    NE = len(w1_aps)
    moe = router_ap is not None
    TS = min(TS, L)
    NT = TS // 128
    NBLK = TS // 512
    groups = [(g0, min(512, FF - g0)) for g0 in range(0, FF, 512)]
    with P.phase():
        C = Ctx(P, stage_w=1024, stage_n=3)
        gn = load_gain(P, norm_ap, 8)
        wpool = [P.pool_of(n, [128, 8, 512], BF16, 2) for n in ("w1g", "w3g")]
        w2pool = P.pool_of("w2g", [128, 4, 1024], BF16, 2)
        hres = P.sbuf("hres", [128, NT, 1024], F32)
        hreg = [[Buf(hres.h, "hres_%d_%d" % (j, hf)) for hf in range(2)] for j in range(NT)]
        hall = [hreg[j][hf] for j in range(NT) for hf in range(2)]
        hnT = P.sbuf("hnT", [128, 8, TS], BF16)
        hnTreg = [Buf(hnT.h, "hnT_blk%d" % i) for i in range(NBLK)]
        gTp = P.pool_of("gT", [128, 4, 512], BF16, 2)
        gTreg = {id(bf): [Buf(bf.h, "gT_fc%d" % fc) for fc in range(4)] for bf in gTp.bufs}
        sap = P.pool_of("sa", [128, 512], F32, 2)
        if moe:
            identf = P._identf
            rw = P.sbuf("rw", [128, 8, 8], F32)
            rst = P.sbuf("rst", [128, 8, 8], F32)
            P.dma(rst[:], router_ap.rearrange("(c p) e -> p c e", p=128), writes=[rst])
            for kc in range(8):
                P.gp(lambda e, kc=kc: e.tensor_scalar(rw[:, kc, :], rst[:, kc, :], gn[:, kc:kc + 1], None, ALU.mult),
                     reads=[rst, gn], writes=[rw])
            comb = P.sbuf("comb", [128, NT, 8], F32)
            xsf = P.sbuf("xsf", [128, 1024], F32)
            hnTf = P.sbuf("hnTf", [128, 8, 128], F32)
            rs = P.pool_of("rs", [128, 64], F32, 2)
        if final_norm_ap is not None:
            gfin = P.sbuf("gfin", [128, 1024], F32)
            P.dma(gfin[:], final_norm_ap.partition_broadcast(128), writes=[gfin])
            outp = P.pool_of("outt", [128, 1024], F32, 2)

        def load_group(e, gi):
            g0, gw = groups[gi]
            w1g, w3g = wpool[0].next(), wpool[1].next()
            w2g = w2pool.next()
            load_w(P, C, w1g, w1_aps[e], gn, f0=g0, nf=gw)
            load_w(P, C, w3g, w3_aps[e], gn, f0=g0, nf=gw)
            load_w(P, C, w2g, w2_aps[e], None, kc0=g0 // 128, nkc=gw // 128)
            return w1g, w3g, w2g

        work = [(e, gi) for e in range(NE) for gi in range(len(groups))]
        P.begin_sched()
        for st in range(L // TS):
            t0 = st * TS
            for j in range(NT):
                P.dma(hres[:, j, :], h_in.h[t0 + j * 128:t0 + (j + 1) * 128, :], reads=[h_in], writes=hreg[j])
            nxt = load_group(*work[0])
            for j in range(NT):
                ss = norm_tile(P, C, hres[:, j, :], hreg[j], hnT, j * 128, track=hnTreg[j // 4])
                if moe:
                    P.dve(lambda e, j=j, ss=ss: e.tensor_scalar(xsf[:], hres[:, j, :], ss[:, 2:3], None, ALU.mult), reads=hreg[j] + [ss], writes=[xsf])
                    for half in range(2):
                        pb = P.psum_bank()
                        for c in range(4):
                            P.pe(lambda e, c=c, pb=pb, half=half: e.transpose(pb[:, c * 128:(c + 1) * 128], xsf[:, (half * 4 + c) * 128:(half * 4 + c + 1) * 128], identf[:]),
                                 reads=[xsf, identf], writes=[pb])
                        P.act(lambda e, pb=pb, half=half: e.activation(hnTf[:, half * 4:half * 4 + 4, :], pb[:, :].rearrange("p (c t) -> p c t", c=4), AF.Copy),
                              reads=[pb], writes=[hnTf])
                    pl = P.psum_bank()
                    mm_acc(P, pl[:, 0:8], pl, [(hnTf[:, kc, :], rw[:, kc, :]) for kc in range(8)], [hnTf, rw])
                    r = rs.next()
                    P.dve(lambda e, r=r, pl=pl: e.tensor_copy(r[:, 0:8], pl[:, 0:8]), reads=[pl], writes=[r])
                    P.dve(lambda e, r=r: e.max(r[:, 8:16], r[:, 0:8]), reads=[r], writes=[r])
                    P.dve(lambda e, r=r: e.tensor_scalar(r[:, 16:24], r[:, 0:8], r[:, 9:10], None, ALU.is_ge), reads=[r], writes=[r])
                    P.dve(lambda e, r=r: e.tensor_scalar(r[:, 32:33], r[:, 8:9], -1.0, None, ALU.mult), reads=[r], writes=[r])
                    P.act(lambda e, r=r: e.activation(r[:, 24:32], r[:, 0:8], AF.Exp, bias=r[:, 32:33]), reads=[r], writes=[r])
                    P.dve(lambda e, r=r: e.tensor_tensor(r[:, 24:32], r[:, 24:32], r[:, 16:24], ALU.mult), reads=[r], writes=[r])
                    P.dve(lambda e, r=r: e.reduce_sum(r[:, 33:34], r[:, 24:32], AX.X), reads=[r], writes=[r])
                    P.dve(lambda e, r=r: e.reciprocal(r[:, 34:35], r[:, 33:34]), reads=[r], writes=[r])
                    P.dve(lambda e, r=r: e.tensor_scalar(r[:, 40:48], r[:, 24:32], r[:, 34:35], None, ALU.mult), reads=[r], writes=[r])
                    P.dve(lambda e, r=r, j=j: e.tensor_copy(comb[:, j, :], r[:, 40:48]), reads=[r], writes=[comb])
            units = [(wi, blk) for wi in range(len(work)) for blk in range(NBLK)]
            grp = {0: nxt}
            if len(work) > 1:
                grp[1] = load_group(*work[1])
            gts = {}

            def up(u):
                wi, blk = units[u]
                w1g, w3g, w2g = grp[wi]
                nfc = groups[work[wi][1]][1] // 128
                c0 = blk * 512
                gT = gTp.next()
                gR = gTreg[id(gT)]
                gts[u] = (gT, gR)
                for fc in range(nfc):
                    pa = P.psum_bank()
                    mm_acc(P, pa[:, :], pa, [(w1g[:, kc, fc * 128:(fc + 1) * 128], hnT[:, kc, c0:c0 + 512]) for kc in range(8)], [w1g, hnTreg[blk]])
                    pc_ = P.psum_bank()
                    mm_acc(P, pc_[:, :], pc_, [(w3g[:, kc, fc * 128:(fc + 1) * 128], hnT[:, kc, c0:c0 + 512]) for kc in range(8)], [w3g, hnTreg[blk]])
                    sa = sap.next()
                    P.act(lambda e, sa=sa, pa=pa: e.activation(sa[:], pa[:, :], AF.Silu), reads=[pa], writes=[sa])
                    P.dve(lambda e, sa=sa, pc_=pc_, gT=gT, fc=fc: e.tensor_tensor(gT[:, fc, :], pc_[:, :], sa[:], ALU.mult), reads=[pc_, sa], writes=[gR[fc]])

            def down(u):
                wi, blk = units[u]
                w1g, w3g, w2g = grp[wi]
                e_ = work[wi][0]
                nfc = groups[work[wi][1]][1] // 128
                gT, gR = gts.pop(u)
                for j in range(4):
                    jt = blk * 4 + j
                    for hf in range(2):
                        po = P.psum_bank()
                        mm_acc(P, po[:, :], po, [(gT[:, fc, j * 128:(j + 1) * 128], w2g[:, fc, hf * 512:(hf + 1) * 512]) for fc in range(nfc)], gR[0:nfc] + [w2g])
                        hs = hres[:, jt, hf * 512:(hf + 1) * 512]
                        if moe:
                            P.dve(lambda e, po=po, hs=hs, jt=jt, e_=e_: e.scalar_tensor_tensor(hs, po[:, :], comb[:, jt, e_:e_ + 1], hs, ALU.mult, ALU.add),
                                  reads=[po, comb, hreg[jt][hf]], writes=[hreg[jt][hf]])
                        else:
                            P.dve(lambda e, po=po, hs=hs: e.tensor_tensor(hs, po[:, :], hs, ALU.add), reads=[po, hreg[jt][hf]], writes=[hreg[jt][hf]])

            up(0)
            for u in range(1, len(units)):
                up(u)
                down(u - 1)
                wi_prev, blk_prev = units[u - 1]
                if blk_prev == NBLK - 1 and wi_prev + 2 < len(work):
                    del grp[wi_prev]
                    grp[wi_prev + 2] = load_group(*work[wi_prev + 2])
            down(len(units) - 1)
            if final_norm_ap is None:
                P.dma(h_view(h_out.h, t0, NT), hres[:], reads=hall, writes=[h_out])
            else:
                for j in range(NT):
                    junk = C.junk.next()
                    ss = C.small.next()
                    P.act(lambda e, junk=junk, ss=ss, j=j: e.activation(junk[:, 0:1024], hres[:, j, :], AF.Square, accum_out=ss[:, 0:1]), reads=hreg[j], writes=[junk, ss])
                    P.act(lambda e, ss=ss: e.activation(ss[:, 1:2], ss[:, 0:1], AF.Sqrt, scale=1.0 / 1024, bias=C.eps[:, 0:1]), reads=[ss, C.eps], writes=[ss])
                    P.dve(lambda e, ss=ss: e.reciprocal(ss[:, 2:3], ss[:, 1:2]), reads=[ss], writes=[ss])
                    ot = outp.next()
                    P.dve(lambda e, ot=ot, ss=ss, j=j: e.scalar_tensor_tensor(ot[:], hres[:, j, :], ss[:, 2:3], gfin[:], ALU.mult, ALU.mult),
                          reads=hreg[j] + [ss, gfin], writes=[ot])
                    P.dma(out_ap[t0 + j * 128:t0 + (j + 1) * 128, :], ot[:], reads=[ot])


def sincos(P, T, cyc_ap, cyc_buf, N, sin_ap=None, sin_buf=None, cos_ap=None, cos_buf=None):
    for dst, dbuf, off in ((sin_ap, sin_buf, 0.0), (cos_ap, cos_buf, 0.25)):
        if dst is None:
            continue
        c, ci, r, m = T["c"], T["ci"], T["r"], T["m"]
        P.dve(lambda e: e.tensor_scalar(c[:, 0:N], cyc_ap, off, None, ALU.add), reads=[cyc_buf], writes=[c])
        P.dve(lambda e: e.tensor_copy(ci[:, 0:N], c[:, 0:N]), reads=[c], writes=[ci])
        P.dve(lambda e: e.tensor_copy(r[:, 0:N], ci[:, 0:N]), reads=[ci], writes=[r])
        P.dve(lambda e: e.tensor_tensor(r[:, 0:N], c[:, 0:N], r[:, 0:N], ALU.subtract), reads=[c, r], writes=[r])
        P.dve(lambda e: e.tensor_scalar(m[:, 0:N], r[:, 0:N], 0.5, None, ALU.is_gt), reads=[r], writes=[m])
        P.dve(lambda e: e.tensor_tensor(r[:, 0:N], r[:, 0:N], m[:, 0:N], ALU.subtract), reads=[r, m], writes=[r])
        P.dve(lambda e: e.tensor_scalar(m[:, 0:N], r[:, 0:N], -0.5, None, ALU.is_lt), reads=[r], writes=[m])
        P.dve(lambda e: e.tensor_tensor(r[:, 0:N], r[:, 0:N], m[:, 0:N], ALU.add), reads=[r, m], writes=[r])
        P.act(lambda e, dst=dst: e.activation(dst, r[:, 0:N], AF.Sin, scale=2.0 * math.pi * (1.0 - 1e-6)), reads=[r], writes=[dbuf])


def trig_scratch(P, N):
    return {"c": P.sbuf("tc", [128, N], F32), "ci": P.sbuf("tci", [128, N], I32),
            "r": P.sbuf("tr", [128, N], F32), "m": P.sbuf("tm", [128, N], F32)}


RET_LG = [math.log1p(-(2.0 ** (-5.0 - h))) for h in range(6)]


def phase_even_mixer(P, L, h_in, h_out, pos_ap, norm_ap, w_in_ap, lam_re_ap, lam_im_ap, log_dt_ap, b_re_ap, b_im_ap,
                     c_re_ap, c_im_ap, d_ap, wglu_ap, bglu_ap, w_out_ap, BLK=256):
    NCH = L // 128
    NB = L // BLK
    JB = BLK // 128
    with P.phase():
        C = Ctx(P, stage_w=16, stage_n=1)
        ident = C.ident
        gn = load_gain(P, norm_ap, 8)
        w_in = P.sbuf("w_in", [128, 8, 2560], BF16)
        w_out = P.sbuf("w_out", [128, 8, 1024], BF16)
        wglu = P.sbuf("wglu", [128, 2, 256], BF16)
        rcos = P.sbuf("rcos", [128, NCH, 32], F32)
        rsin = P.sbuf("rsin", [128, NCH, 32], F32)
        dqk = P.sbuf("dqk", [128, 12], F32)
        gS = P.sbuf("gS", [128, 3], F32)
        g128 = P.sbuf("g128", [128, 3], F32)
        g127 = P.sbuf("g127", [128, 3], F32)
        maskT = P.sbuf("maskT", [128, 128], F32)
        S = P.sbuf("Sst", [128, 3, 128], F32)
        Sbfp = P.pool_of("Sbf", [128, 3, 128], BF16, 2)
        BB = [P.sbuf("BB%d" % i, [128, 8, 128], BF16) for i in range(2)]
        sp = P.sbuf("s5p", [128, 16, 8], F32)
        DT, TH, RHO, CB, SB, AR, AI, DEN, FR, FI, T1, T2 = [sp[:, i, :] for i in range(12)]
        cosT = P.sbuf("cosT", [128, 8, BLK], F32); sinT = P.sbuf("sinT", [128, 8, BLK], F32); rhoB = P.sbuf("rhoB", [128, 8, BLK], F32)
        Cm = [P.sbuf("Cm%d" % i, [128, 8, 128], BF16) for i in range(2)]
        dsk = load_gain(P, d_ap, 2)
        bgl = load_gain(P, bglu_ap, 2)
        zl = P.sbuf("zlast", [128, 2, 8], F32)
        zi = P.sbuf("zinit", [128, 2, 8], F32)
        zt = P.sbuf("ztmp", [128, 2, 8], F32)
        with P.phase():
            CS = Ctx(P, stage_w=2048, stage_n=2)
            load_w(P, CS, w_in, w_in_ap, gn)
            load_w(P, CS, w_out, w_out_ap, None)
            load_w(P, CS, wglu, wglu_ap, None)
            T = trig_scratch(P, 1024)
            posi = P.sbuf("posi", [128, NCH], I32)
            P.dma(posi[:], pos_ap.rearrange("(c p) -> p c", p=128), writes=[posi], allow_slow_non_contiguous=True)
            posf = P.sbuf("posf", [128, NCH], F32)
            P.dve(lambda e: e.tensor_copy(posf[:], posi[:]), reads=[posi], writes=[posf])
            ji = P.sbuf("ji", [128, 32], I32)
            P.gp(lambda e: e.iota(ji[:], [[1, 32]], base=0, channel_multiplier=0), writes=[ji])
            invf = P.sbuf("invf", [128, 32], F32)
            P.dve(lambda e: e.tensor_copy(invf[:], ji[:]), reads=[ji], writes=[invf])
            P.act(lambda e: e.activation(invf[:], invf[:], AF.Exp, scale=-math.log(10000.0) / 32.0), reads=[invf], writes=[invf])
            ang = P.sbuf("ang", [128, 32, 32], F32)
            for c0 in range(0, NCH, 32):
                n = min(32, NCH - c0)
                P.dve(lambda e, c0=c0, n=n: e.tensor_tensor(ang[:, 0:n, :], posf[:, c0:c0 + n].unsqueeze(2).broadcast_to([128, n, 32]),
                                                           invf[:, :].unsqueeze(1).broadcast_to([128, n, 32]), ALU.mult), reads=[posf, invf], writes=[ang])
                P.dve(lambda e, n=n: e.tensor_scalar(ang[:, 0:n, :], ang[:, 0:n, :], 1.0 / (2.0 * math.pi), None, ALU.mult), reads=[ang], writes=[ang])
                sincos(P, T, ang[:, 0:n, :].rearrange("p a b -> p (a b)"), ang, n * 32,
                       rsin[:, c0:c0 + n, :].rearrange("p a b -> p (a b)"), rsin, rcos[:, c0:c0 + n, :].rearrange("p a b -> p (a b)"), rcos)
            ti = P.sbuf("ti", [128, 1], I32)
            P.gp(lambda e: e.iota(ti[:], [[0, 1]], base=0, channel_multiplier=1), writes=[ti])
            tf = P.sbuf("tf", [128, 1], F32)
            P.dve(lambda e: e.tensor_copy(tf[:], ti[:]), reads=[ti], writes=[tf])
            for h in range(6):
                P.act(lambda e, h=h: e.activation(dqk[:, h:h + 1], tf[:], AF.Exp, scale=RET_LG[h]), reads=[tf], writes=[dqk])
                P.act(lambda e, h=h: e.activation(dqk[:, 6 + h:7 + h], tf[:], AF.Exp, scale=-RET_LG[h]), reads=[tf], writes=[dqk])
            P.dve(lambda e: e.tensor_scalar(dqk[:, 6:12], dqk[:, 6:12], 0.125, None, ALU.mult), reads=[dqk], writes=[dqk])
            for h in range(6):
                m, hp = h // 2, h % 2
                for tb, val in ((gS, math.exp(RET_LG[h])), (g128, math.exp(128 * RET_LG[h])), (g127, math.exp(127 * RET_LG[h]))):
                    P.gp(lambda e, tb=tb, val=val, m=m, hp=hp: e.memset(tb[hp * 64:(hp + 1) * 64, m:m + 1], val), writes=[tb])
            P.gp(lambda e: e.memset(maskT[:], 1.0), writes=[maskT])
            P.gp(lambda e: e.affine_select(maskT[:], maskT[:], [[1, 128]], ALU.is_ge, 0.0, base=0, channel_multiplier=-1), reads=[maskT], writes=[maskT])
            P.gp(lambda e: e.memset(S[:], 0.0), writes=[S])
            LR = P.sbuf("LR", [128, 8], F32); LI = P.sbuf("LI", [128, 8], F32); LD = P.sbuf("LD", [128, 8], F32)
            for two in range(2):
                P.dma(LR[two * 64:(two + 1) * 64, :], lam_re_ap.rearrange("(st two) p -> two p st", two=2)[two], writes=[LR], allow_slow_non_contiguous=True)
                P.dma(LI[two * 64:(two + 1) * 64, :], lam_im_ap.rearrange("(st two) p -> two p st", two=2)[two], writes=[LI], allow_slow_non_contiguous=True)
                P.dma(LD[two * 64:(two + 1) * 64, :], log_dt_ap.rearrange("(st two) -> two st", two=2)[two].partition_broadcast(64), writes=[LD], allow_slow_non_contiguous=True)
            BBf = [P.sbuf("BBf%d" % i, [128, 8, 128], F32) for i in range(2)]
            Cf = [P.sbuf("Cf%d" % i, [128, 8, 128], F32) for i in range(2)]
            for t_ in BBf + Cf:
                P.gp(lambda e, t_=t_: e.memset(t_[:], 0.0), writes=[t_])
            for g in range(16):
                st, two, r0 = g // 2, g % 2, (g % 8) * 16
                for i, (bap, cap) in enumerate(((b_re_ap, c_re_ap), (b_im_ap, c_im_ap))):
                    P.dma(BBf[i][r0:r0 + 16, st, two * 64:(two + 1) * 64], bap[g].rearrange("p c -> c p"), writes=[BBf[i]], allow_slow_non_contiguous=True)
                    P.dma(Cf[i][two * 64:(two + 1) * 64, st, r0:r0 + 16], cap[g].rearrange("c p -> p c"), writes=[Cf[i]], allow_slow_non_contiguous=True)
            for i in range(2):
                P.dve(lambda e, i=i: e.tensor_copy(BB[i][:], BBf[i][:]), reads=[BBf[i]], writes=[BB[i]])

            def sop(fn, eng="dve"):
                P.op(eng, fn, reads=[sp, LR, LI, LD], writes=[sp])
            sop(lambda e: e.activation(DT, LD[:], AF.Exp), "act")
            sop(lambda e: e.tensor_tensor(TH, LI[:], DT, ALU.mult))
            sop(lambda e: e.tensor_tensor(RHO, LR[:], DT, ALU.mult))
            sop(lambda e: e.activation(RHO, RHO, AF.Exp), "act")
            taui = P.sbuf("taui", [128, BLK], I32)
            P.gp(lambda e: e.iota(taui[:], [[1, BLK]], base=0, channel_multiplier=0), writes=[taui])
            tauc = P.sbuf("tauc", [128, BLK], F32)
            P.dve(lambda e: e.tensor_copy(tauc[:], taui[:]), reads=[taui], writes=[tauc])
            P.dve(lambda e: e.tensor_scalar(tauc[:], tauc[:], 1.0 / (2.0 * math.pi), None, ALU.mult), reads=[tauc], writes=[tauc])
            cyc = P.sbuf("cyc", [128, BLK], F32)
            for st in range(8):
                P.dve(lambda e, st=st: e.tensor_scalar(cyc[:], tauc[:], sp[:, 1, st:st + 1], None, ALU.mult), reads=[tauc, sp], writes=[cyc])
                sincos(P, T, cyc[:, :], cyc, BLK, sinT[:, st, :], sinT, cosT[:, st, :], cosT)
                P.dve(lambda e, st=st: e.tensor_copy(rhoB[:, st, :], sp[:, 2, st:st + 1].broadcast_to([128, BLK])), reads=[sp], writes=[rhoB])
            cyc8 = P.sbuf("cyc8", [128, 8], F32)
            P.dve(lambda e: e.tensor_scalar(cyc8[:], TH, float(BLK) / (2.0 * math.pi), None, ALU.mult), reads=[sp], writes=[cyc8])
            sincos(P, T, cyc8[:, :], cyc8, 8, SB, sp, CB, sp)
            P.dve(lambda e: e.tensor_tensor(AR, RHO, cosT[:, :, 1], ALU.mult), reads=[sp, cosT], writes=[sp])
            P.dve(lambda e: e.tensor_tensor(AI, RHO, sinT[:, :, 1], ALU.mult), reads=[sp, sinT], writes=[sp])
            sop(lambda e: e.tensor_scalar(AR, AR, -1.0, None, ALU.add))
            sop(lambda e: e.tensor_tensor(DEN, LR[:], LR[:], ALU.mult))
            sop(lambda e: e.tensor_tensor(T1, LI[:], LI[:], ALU.mult))
            sop(lambda e: e.tensor_tensor(DEN, DEN, T1, ALU.add))
            sop(lambda e: e.reciprocal(DEN, DEN))
            sop(lambda e: e.tensor_tensor(T1, AR, LR[:], ALU.mult))
            sop(lambda e: e.tensor_tensor(T2, AI, LI[:], ALU.mult))
            sop(lambda e: e.tensor_tensor(FR, T1, T2, ALU.add))
            sop(lambda e: e.tensor_tensor(FR, FR, DEN, ALU.mult))
            sop(lambda e: e.tensor_tensor(T1, AI, LR[:], ALU.mult))
            sop(lambda e: e.tensor_tensor(T2, AR, LI[:], ALU.mult))
            sop(lambda e: e.tensor_tensor(FI, T1, T2, ALU.subtract))
            sop(lambda e: e.tensor_tensor(FI, FI, DEN, ALU.mult))
            ct = P.sbuf("ct", [128, 2, 128], F32)
            for st in range(8):
                fr, fi = sp[:, 8, st:st + 1], sp[:, 9, st:st + 1]
                P.dve(lambda e, st=st, fi=fi: e.tensor_scalar(ct[:, 0, :], Cf[1][:, st, :], fi, None, ALU.mult), reads=[Cf[1], sp], writes=[ct])
                P.dve(lambda e, st=st, fr=fr: e.scalar_tensor_tensor(ct[:, 1, :], Cf[0][:, st, :], fr, ct[:, 0, :], ALU.mult, ALU.subtract), reads=[Cf[0], sp, ct], writes=[ct])
                P.dve(lambda e, st=st: e.tensor_copy(Cm[0][:, st, :], ct[:, 1, :]), reads=[ct], writes=[Cm[0]])
                P.dve(lambda e, st=st, fr=fr: e.tensor_scalar(ct[:, 0, :], Cf[1][:, st, :], fr, None, ALU.mult), reads=[Cf[1], sp], writes=[ct])
                P.dve(lambda e, st=st, fi=fi: e.scalar_tensor_tensor(ct[:, 1, :], Cf[0][:, st, :], fi, ct[:, 0, :], ALU.mult, ALU.add), reads=[Cf[0], sp, ct], writes=[ct])
                P.dve(lambda e, st=st: e.tensor_scalar(Cm[1][:, st, :], ct[:, 1, :], -1.0, None, ALU.mult), reads=[ct], writes=[Cm[1]])
            P.gp(lambda e: e.memset(zl[:], 0.0), writes=[zl])

        P.begin_sched()
        hpool = P.pool_of("hblk", [128, JB, 1024], F32, 2)
        hnTp = P.pool_of("hnT", [128, 8, BLK], BF16, 1)
        ycp = P.pool_of("ycat", [128, 8, BLK], BF16, 1)
        uTp = P.pool_of("uT", [128, 2, BLK], BF16, 1)
        uTfp = P.pool_of("uTf", [128, 2, BLK], F32, 1)
        xtp = P.pool_of("xt", [128, 2, BLK], F32, 2)
        wkp = P.pool_of("wk", [128, 2, BLK], F32, 2)
        zp = P.pool_of("z", [128, 2, BLK], F32, 2)
        sbp = P.pool_of("sbf", [128, 2, 4, BLK], BF16, 1)
        yfp = P.pool_of("yf", [128, BLK], F32, 2)
        y2p = P.pool_of("y2", [128, BLK], F32, 2)
        ybp = P.pool_of("yb", [128, 2, BLK], BF16, 1)
        qkp = P.pool_of("qk", [128, 768], F32, 2)
        r1p = P.pool_of("r1", [128, 768], F32, 1)
        r2p = P.pool_of("r2", [128, 768], F32, 1)
        qksp = P.pool_of("qks", [128, 768], BF16, 2)
        qkTp = P.pool_of("qkT", [128, 6, 128], BF16, 2)
        vsp = P.pool_of("vs", [128, 768], BF16, 2)
        sgp = P.pool_of("sg", [128, 768], F32, 2)
        Mp = P.pool_of("M", [128, 128], BF16, 6)
        ysbp = P.pool_of("ysb", [128, 768], F32, 1)
        ysqp = P.pool_of("ysq", [128, 768], F32, 1)
        ynp = P.pool_of("yn", [128, 768], BF16, 2)
        st6p = P.pool_of("st6", [128, 3, 6], F32, 2)

        hb_next = hpool.next()
        P.dma(hb_next[:], h_view(h_in.h, 0, JB), reads=[h_in], writes=[hb_next])
        for b in range(NB):
            hb = hb_next
            if b + 1 < NB:
                hb_next = hpool.next()
                P.dma(hb_next[:], h_view(h_in.h, (b + 1) * BLK, JB), reads=[h_in], writes=[hb_next])
            hnT = hnTp.next()
            for j in range(JB):
                norm_tile(P, C, hb[:, j, :], hb, hnT, j * 128)
            ycat = ycp.next()
            uT, uTf = uTp.next(), uTfp.next()
            for c in range(2):
                ps = P.psum_bank()
                mm_acc(P, ps[:, 0:BLK], ps, [(w_in[:, kc, 2304 + c * 128:2304 + (c + 1) * 128], hnT[:, kc, :]) for kc in range(8)], [w_in, hnT])
                P.act(lambda e, c=c, ps=ps: e.activation(uT[:, c, :], ps[:, 0:BLK], AF.Copy), reads=[ps], writes=[uT])
                P.act(lambda e, c=c, ps=ps: e.activation(uTf[:, c, :], ps[:, 0:BLK], AF.Copy), reads=[ps], writes=[uTf])
            P.dve(lambda e: e.tensor_tensor(zt[:, 0, :], CB, zl[:, 0, :], ALU.mult), reads=[sp, zl], writes=[zt])
            P.dve(lambda e: e.tensor_tensor(zt[:, 1, :], SB, zl[:, 1, :], ALU.mult), reads=[sp, zl], writes=[zt])
            P.dve(lambda e: e.tensor_tensor(zi[:, 0, :], zt[:, 0, :], zt[:, 1, :], ALU.subtract), reads=[zt], writes=[zi])
            P.dve(lambda e: e.tensor_tensor(zt[:, 0, :], SB, zl[:, 0, :], ALU.mult), reads=[sp, zl], writes=[zt])
            P.dve(lambda e: e.tensor_tensor(zt[:, 1, :], CB, zl[:, 1, :], ALU.mult), reads=[sp, zl], writes=[zt])
            P.dve(lambda e: e.tensor_tensor(zi[:, 1, :], zt[:, 0, :], zt[:, 1, :], ALU.add), reads=[zt], writes=[zi])
            sbf = sbp.next()
            for st in range(8):
                c = st // 4
                pr, pi_ = P.psum_bank(), P.psum_bank()
                P.pe(lambda e, st=st, c=c, pr=pr: e.matmul(pr[:, 0:BLK], BB[0][:, st, :], uT[:, c, :], start=True, stop=True), reads=[BB[0], uT], writes=[pr])
                P.pe(lambda e, st=st, c=c, pi_=pi_: e.matmul(pi_[:, 0:BLK], BB[1][:, st, :], uT[:, c, :], start=True, stop=True), reads=[BB[1], uT], writes=[pi_])
                xt, wk = xtp.next(), wkp.next()
                P.dve(lambda e, st=st, pr=pr, wk=wk: e.tensor_tensor(wk[:, 0, :], pr[:, 0:BLK], cosT[:, st, :], ALU.mult), reads=[pr, cosT], writes=[wk])
                P.dve(lambda e, st=st, pi_=pi_, wk=wk: e.tensor_tensor(wk[:, 1, :], pi_[:, 0:BLK], sinT[:, st, :], ALU.mult), reads=[pi_, sinT], writes=[wk])
                P.gp(lambda e, xt=xt, wk=wk: e.tensor_tensor(xt[:, 0, :], wk[:, 0, :], wk[:, 1, :], ALU.add), reads=[wk], writes=[xt])
                wk2 = wkp.next()
                P.dve(lambda e, st=st, pi_=pi_, wk2=wk2: e.tensor_tensor(wk2[:, 0, :], pi_[:, 0:BLK], cosT[:, st, :], ALU.mult), reads=[pi_, cosT], writes=[wk2])
                P.dve(lambda e, st=st, pr=pr, wk2=wk2: e.tensor_tensor(wk2[:, 1, :], pr[:, 0:BLK], sinT[:, st, :], ALU.mult), reads=[pr, sinT], writes=[wk2])
                P.gp(lambda e, xt=xt, wk2=wk2: e.tensor_tensor(xt[:, 1, :], wk2[:, 0, :], wk2[:, 1, :], ALU.subtract), reads=[wk2], writes=[xt])
                z = zp.next()
                for ri in range(2):
                    P.dve(lambda e, z=z, xt=xt, ri=ri, st=st: e.tensor_tensor_scan(z[:, ri, :], rhoB[:, st, :], xt[:, ri, :], zi[:, ri, st:st + 1], ALU.mult, ALU.add),
                          reads=[rhoB, xt, zi], writes=[z])
                P.dve(lambda e, z=z, st=st: e.tensor_copy(zl[:, :, st:st + 1], z[:, :, BLK - 1:BLK]), reads=[z], writes=[zl])
                w3, w4 = wkp.next(), wkp.next()
                P.gp(lambda e, w3=w3, z=z, st=st: e.tensor_tensor(w3[:, 0, :], z[:, 0, :], cosT[:, st, :], ALU.mult), reads=[z, cosT], writes=[w3])
                P.gp(lambda e, w3=w3, z=z, st=st: e.tensor_tensor(w3[:, 1, :], z[:, 1, :], sinT[:, st, :], ALU.mult), reads=[z, sinT], writes=[w3])
                P.gp(lambda e, w3=w3, st=st: e.tensor_tensor(sbf[:, 0, st % 4, :], w3[:, 0, :], w3[:, 1, :], ALU.subtract), reads=[w3], writes=[sbf])
                P.dve(lambda e, w4=w4, z=z, st=st: e.tensor_tensor(w4[:, 0, :], z[:, 0, :], sinT[:, st, :], ALU.mult), reads=[z, sinT], writes=[w4])
                P.dve(lambda e, w4=w4, z=z, st=st: e.tensor_tensor(w4[:, 1, :], z[:, 1, :], cosT[:, st, :], ALU.mult), reads=[z, cosT], writes=[w4])
                P.dve(lambda e, w4=w4, st=st: e.tensor_tensor(sbf[:, 1, st % 4, :], w4[:, 0, :], w4[:, 1, :], ALU.add), reads=[w4], writes=[sbf])
                if st % 4 == 3:
                    py = P.psum_bank()
                    pairs = []
                    for s4 in range(4):
                        pairs.append((Cm[0][:, c * 4 + s4, :], sbf[:, 0, s4, :]))
                        pairs.append((Cm[1][:, c * 4 + s4, :], sbf[:, 1, s4, :]))
                    mm_acc(P, py[:, 0:BLK], py, pairs, [Cm[0], Cm[1], sbf])
                    yf, y2 = yfp.next(), y2p.next()
                    P.dve(lambda e, yf=yf, py=py, c=c: e.scalar_tensor_tensor(yf[:], uTf[:, c, :], dsk[:, c:c + 1], py[:, 0:BLK], ALU.mult, ALU.add), reads=[uTf, dsk, py], writes=[yf])
                    P.gp(lambda e, yf=yf, y2=y2: e.tensor_tensor(y2[:], yf[:], yf[:], ALU.mult), reads=[yf], writes=[y2])
                    P.dve(lambda e, y2=y2: e.tensor_scalar(y2[:], y2[:], 0.044715, 1.0, ALU.mult, ALU.add), reads=[y2], writes=[y2])
                    P.gp(lambda e, yf=yf, y2=y2: e.tensor_tensor(y2[:], y2[:], yf[:], ALU.mult), reads=[yf, y2], writes=[y2])
                    P.act(lambda e, y2=y2: e.activation(y2[:], y2[:], AF.Sigmoid, scale=1.5957691216057308), reads=[y2], writes=[y2])
                    P.dve(lambda e, yf=yf, y2=y2: e.tensor_tensor(yf[:], yf[:], y2[:], ALU.mult), reads=[yf, y2], writes=[yf])
                    if c == 0:
                        yb = ybp.next()
                        yfs = [yf]
                    else:
                        yfs.append(yf)
                    P.act(lambda e, yb=yb, yf=yf, c=c: e.activation(yb[:, c, :], yf[:], AF.Copy), reads=[yf], writes=[yb])
            for fc in range(2):
                pz = P.psum_bank()
                mm_acc(P, pz[:, 0:BLK], pz, [(wglu[:, kc, fc * 128:(fc + 1) * 128], yb[:, kc, :]) for kc in range(2)], [wglu, yb])
                sg_ = y2p.next()
                P.act(lambda e, pz=pz, sg_=sg_, fc=fc: e.activation(sg_[:], pz[:, 0:BLK], AF.Sigmoid, bias=bgl[:, fc:fc + 1]), reads=[pz, bgl], writes=[sg_])
                P.dve(lambda e, sg_=sg_, fc=fc, yfs=yfs: e.tensor_tensor(ycat[:, 6 + fc, :], yfs[fc][:], sg_[:], ALU.mult), reads=[yfs[fc], sg_], writes=[ycat])
            for j in range(JB):
                ch = b * JB + j
                tsl = slice(j * 128, (j + 1) * 128)
                qk = qkp.next()
                for (c0, c1) in ((0, 512), (512, 768)):
                    ps = P.psum_bank()
                    mm_acc(P, ps[:, 0:c1 - c0], ps, [(hnT[:, kc, tsl], w_in[:, kc, c0:c1]) for kc in range(8)], [w_in, hnT])
                    P.act(lambda e, ps=ps, c0=c0, c1=c1, qk=qk: e.activation(qk[:, c0:c1], ps[:, 0:c1 - c0], AF.Copy), reads=[ps], writes=[qk])
                r1, r2 = r1p.next(), r2p.next()
                X = qk[:, :].rearrange("p (a h j) -> p a h j", a=12, h=2)
                R1 = r1[:, :].rearrange("p (a h j) -> p a h j", a=12, h=2)
                R2 = r2[:, :].rearrange("p (a h j) -> p a h j", a=12, h=2)
                cosb = rcos[:, ch, :].unsqueeze(1).broadcast_to([128, 12, 32])
                sinb = rsin[:, ch, :].unsqueeze(1).broadcast_to([128, 12, 32])
                for hh in range(2):
                    P.gp(lambda e, hh=hh: e.tensor_tensor(R1[:, :, hh, :], X[:, :, hh, :], cosb, ALU.mult), reads=[qk, rcos], writes=[r1])
                    P.dve(lambda e, hh=hh: e.tensor_tensor(R2[:, :, hh, :], X[:, :, 1 - hh, :], sinb, ALU.mult), reads=[qk, rsin], writes=[r2])
                P.gp(lambda e: e.tensor_tensor(R1[:, :, 0, :], R1[:, :, 0, :], R2[:, :, 0, :], ALU.subtract), reads=[r1, r2], writes=[r1])
                P.gp(lambda e: e.tensor_tensor(R1[:, :, 1, :], R1[:, :, 1, :], R2[:, :, 1, :], ALU.add), reads=[r1, r2], writes=[r1])
                qks = qksp.next()
                P.dve(lambda e, qks=qks: e.tensor_tensor(qks[:, :].rearrange("p (a d) -> p a d", a=12), r1[:, :].rearrange("p (a d) -> p a d", a=12),
                                                        dqk[:, :].unsqueeze(2).broadcast_to([128, 12, 64]), ALU.mult), reads=[r1, dqk], writes=[qks])
                pt = P.psum_tbank()
                for m in range(6):
                    P.pe(lambda e, m=m, pt=pt, qks=qks: e.transpose(pt[:, m * 128:(m + 1) * 128], qks[:, m * 128:(m + 1) * 128], ident[:]), reads=[qks, ident], writes=[pt])
                qkT = qkTp.next()
                P.act(lambda e, pt=pt, qkT=qkT: e.activation(qkT[:, :, :], pt[:, 0:768].rearrange("p (m t) -> p m t", m=6), AF.Copy), reads=[pt], writes=[qkT])
                vs, sg = vsp.next(), sgp.next()
                for (dst, base, fn) in ((vs, 768, AF.Copy), (sg, 1536, AF.Silu)):
                    for (c0, c1) in ((0, 512), (512, 768)):
                        ps = P.psum_bank()
                        mm_acc(P, ps[:, 0:c1 - c0], ps, [(hnT[:, kc, tsl], w_in[:, kc, base + c0:base + c1]) for kc in range(8)], [w_in, hnT])
                        P.act(lambda e, ps=ps, c0=c0, c1=c1, dst=dst, fn=fn: e.activation(dst[:, c0:c1], ps[:, 0:c1 - c0], fn), reads=[ps], writes=[dst])
                Sbf = Sbfp.next()
                for m in range(3):
                    P.dve(lambda e, m=m, Sbf=Sbf: e.tensor_scalar(Sbf[:, m, :], S[:, m, :], gS[:, m:m + 1], None, ALU.mult), reads=[S, gS], writes=[Sbf])
                Ms = []
                for h in range(6):
                    m, hp = h // 2, h % 2
                    psl = slice(hp * 64, (hp + 1) * 64)
                    pS = P.psum_bank()
                    P.pe(lambda e, pS=pS, m=m, psl=psl, qkT=qkT: e.matmul(pS[:, 0:128], qkT[psl, 3 + m, :], qkT[psl, m, :], start=True, stop=True), reads=[qkT], writes=[pS])
                    M = Mp.next()
                    P.dve(lambda e, pS=pS, M=M: e.tensor_tensor(M[:], pS[:, 0:128], maskT[:], ALU.mult), reads=[pS, maskT], writes=[M])
                    Ms.append(M)
                ysb = ysbp.next()
                for (h0, h1) in ((0, 4), (4, 6)):
                    py = P.psum_bank()
                    for h in range(h0, h1):
                        m, hp = h // 2, h % 2
                        psl = slice(hp * 64, (hp + 1) * 64)
                        osl = slice((h - h0) * 128, (h - h0 + 1) * 128)
                        P.pe(lambda e, py=py, osl=osl, h=h, vs=vs, M=Ms[h]: e.matmul(py[:, osl], M[:], vs[:, h * 128:(h + 1) * 128], start=True, stop=False), reads=[Ms[h], vs], writes=[py])
                        P.pe(lambda e, py=py, osl=osl, m=m, psl=psl, qkT=qkT, Sbf=Sbf: e.matmul(py[:, osl], qkT[psl, m, :], Sbf[psl, m, :], start=False, stop=True), reads=[qkT, Sbf], writes=[py])
                    P.act(lambda e, py=py, h0=h0, h1=h1, ysb=ysb: e.activation(ysb[:, h0 * 128:h1 * 128], py[:, 0:(h1 - h0) * 128], AF.Copy), reads=[py], writes=[ysb])
                for m in range(3):
                    pU = P.psum_bank()
                    for hp in range(2):
                        h = 2 * m + hp
                        P.pe(lambda e, pU=pU, hp=hp, h=h, qks=qks, vs=vs: e.matmul(pU[hp * 64:(hp + 1) * 64, 0:128], qks[:, 384 + h * 64:384 + (h + 1) * 64], vs[:, h * 128:(h + 1) * 128], start=True, stop=True),
                             reads=[qks, vs], writes=[pU])
                    P.dve(lambda e, m=m: e.tensor_scalar(S[:, m, :], S[:, m, :], g128[:, m:m + 1], None, ALU.mult), reads=[S, g128], writes=[S])
                    P.dve(lambda e, m=m, pU=pU: e.scalar_tensor_tensor(S[:, m, :], pU[:, 0:128], g127[:, m:m + 1], S[:, m, :], ALU.mult, ALU.add), reads=[pU, g127, S], writes=[S])
                ysq, s6 = ysqp.next(), st6p.next()
                P.gp(lambda e, ysq=ysq, ysb=ysb: e.tensor_tensor(ysq[:], ysb[:], ysb[:], ALU.mult), reads=[ysb], writes=[ysq])
                P.dve(lambda e, ysq=ysq, s6=s6: e.tensor_reduce(s6[:, 0, :], ysq[:, :].rearrange("p (h d) -> p h d", h=6), AX.X, ALU.add), reads=[ysq], writes=[s6])
                P.act(lambda e, s6=s6: e.activation(s6[:, 1, :], s6[:, 0, :], AF.Sqrt, scale=1.0 / 128.0, bias=C.eps[:, 0:1]), reads=[s6, C.eps], writes=[s6])
                P.dve(lambda e, s6=s6: e.reciprocal(s6[:, 2, :], s6[:, 1, :]), reads=[s6], writes=[s6])
                P.gp(lambda e, ysq=ysq, ysb=ysb, sg=sg: e.tensor_tensor(ysq[:], ysb[:], sg[:], ALU.mult), reads=[ysb, sg], writes=[ysq])
                yn = ynp.next()
                P.dve(lambda e, yn=yn, ysq=ysq, s6=s6: e.tensor_tensor(yn[:, :].rearrange("p (h d) -> p h d", h=6), ysq[:, :].rearrange("p (h d) -> p h d", h=6),
                                                                      s6[:, 2, :].unsqueeze(2).broadcast_to([128, 6, 128]), ALU.mult), reads=[ysq, s6], writes=[yn])
                pt2 = P.psum_tbank()
                for m in range(6):
                    P.pe(lambda e, m=m, pt2=pt2, yn=yn: e.transpose(pt2[:, m * 128:(m + 1) * 128], yn[:, m * 128:(m + 1) * 128], ident[:]), reads=[yn, ident], writes=[pt2])
                P.act(lambda e, pt2=pt2, tsl=tsl: e.activation(ycat[:, 0:6, tsl], pt2[:, 0:768].rearrange("p (m t) -> p m t", m=6), AF.Copy), reads=[pt2], writes=[ycat])
            for j in range(JB):
                for hf in range(2):
                    po = P.psum_bank()
                    mm_acc(P, po[:, :], po, [(ycat[:, kc, j * 128:(j + 1) * 128], w_out[:, kc, hf * 512:(hf + 1) * 512]) for kc in range(8)], [ycat, w_out])
                    P.dve(lambda e, po=po, j=j, hf=hf: e.tensor_tensor(hb[:, j, hf * 512:(hf + 1) * 512], po[:, :], hb[:, j, hf * 512:(hf + 1) * 512], ALU.add), reads=[po, hb], writes=[hb])
            P.dma(h_view(h_out.h, b * BLK, JB), hb[:], reads=[hb], writes=[h_out])


BCAST_LHST = False


def phase_mamba_a(P, L, h_in, yn_out, norm_ap, w_in_ap, conv_w_ap, conv_b_ap, dt_bias_ap, a_log_ap, d_ap):
    NCH = L // 128
    with P.phase():
        C = Ctx(P, stage_w=16, stage_n=1, junk_w=1024)
        ident, identf = C.ident, P._identf
        gn = load_gain(P, norm_ap, 8)
        cbcol = load_gain(P, conv_b_ap, 24)
        w_in = P.sbuf("w_in", [128, 8, 5152], BF16)
        Dg = P.sbuf("Dg", [128, 24, 4, 128], BF16)
        cbrow = P.sbuf("cbrow", [1, 3072], BF16)
        ones1 = P.sbuf("ones1", [1, 128], BF16)
        dtb = P.sbuf("dtb", [128, 32], F32)
        abc = P.sbuf("abc", [128, 32], F32)
        dskb = P.sbuf("dskb", [128, 32], F32)
        tri = P.sbuf("tri", [128, 128], F32)
        ustr = P.sbuf("ustr", [128, 128], F32)
        onesf = P.sbuf("onesf", [128, 128], F32)
        stT = P.sbuf("stT", [128, 4, 512], F32)
        stTb = P.sbuf("stTb", [128, 4, 512], BF16)
        with P.phase():
            CS = Ctx(P, stage_w=2048, stage_n=2)
            load_w(P, CS, w_in, w_in_ap, gn)
            cw = P.sbuf("cw", [128, 24, 4], F32)
            for k in range(4):
                P.dma(cw[:, :, k], conv_w_ap[k].rearrange("(ct p) -> p ct", p=128), writes=[cw], allow_slow_non_contiguous=True)
            for ct in range(24):
                for k in range(4):
                    P.act(lambda e, ct=ct, k=k: e.activation(Dg[:, ct, k, :], identf[:], AF.Copy, scale=cw[:, ct, k:k + 1]), reads=[identf, cw], writes=[Dg])
            cbr = P.sbuf("cbr", [1, 3072], F32)
            P.dma(cbr[:], conv_b_ap.unsqueeze(0), writes=[cbr])
            P.dve(lambda e: e.tensor_copy(cbrow[:], cbr[:]), reads=[cbr], writes=[cbrow])
            P.gp(lambda e: e.memset(ones1[:], 1.0), writes=[ones1])
            P.dma(dtb[:], dt_bias_ap.partition_broadcast(128), writes=[dtb])
            P.dma(abc[:], a_log_ap.partition_broadcast(128), writes=[abc])
            P.dma(dskb[:], d_ap.partition_broadcast(128), writes=[dskb])
            P.act(lambda e: e.activation(abc[:], abc[:], AF.Exp), reads=[abc], writes=[abc])
            P.dve(lambda e: e.tensor_scalar(abc[:], abc[:], -1.0, None, ALU.mult), reads=[abc], writes=[abc])
            P.gp(lambda e: e.memset(tri[:], 1.0), writes=[tri])
            P.gp(lambda e: e.affine_select(tri[:], tri[:], [[1, 128]], ALU.is_ge, 0.0, base=0, channel_multiplier=-1), reads=[tri], writes=[tri])
            P.gp(lambda e: e.memset(ustr[:], 1.0), writes=[ustr])
            P.gp(lambda e: e.affine_select(ustr[:], ustr[:], [[-1, 128]], ALU.is_gt, 0.0, base=0, channel_multiplier=1), reads=[ustr], writes=[ustr])
            P.gp(lambda e: e.memset(onesf[:], 1.0), writes=[onesf])
            P.gp(lambda e: e.memset(stT[:], 0.0), writes=[stT])
            P.gp(lambda e: e.memset(stTb[:], 0.0), writes=[stTb])
        P.begin_sched()
        hpool = P.pool_of("hblk", [128, 1024], F32, 2)
        hnTp = P.pool_of("hnT", [128, 8, 128], BF16, 2)
        zsp = P.pool_of("zs", [128, 2048], BF16, 1)
        xprep = P.pool_of("xpre", [128, 24, 131], BF16, 2)
        xsp = P.pool_of("xstok", [128, 2048], BF16, 1)
        btp = P.pool_of("btok", [128, 512], BF16, 1)
        BTp = P.pool_of("BT", [128, 4, 128], BF16, 1)
        CTp = P.pool_of("CT", [128, 4, 128], BF16, 1)
        cbtp = P.pool_of("cbt", [128, 4, 128], F32, 1)
        dtp = P.pool_of("dts", [128, 6, 32], F32, 2)
        csp = P.pool_of("cs", [128, 96], F32, 2)
        ecp = P.pool_of("ecs", [128, 96], F32, 2)
        Wp = P.pool_of("Wh", [128, 128], F32, 4)
        eLp = P.pool_of("eL", [128, 4, 128], F32, 2)
        MTp = P.pool_of("MT", [128, 4, 128], BF16, 4)
        xdtp = P.pool_of("xdt", [128, 512], BF16, 2)
        xwp = P.pool_of("xw", [128, 512], BF16, 2)
        yop = P.pool_of("yo", [128, 512], F32, 2)
        ygp = P.pool_of("yg", [128, 2048], F32, 1)
        ynp = P.pool_of("ynb", [128, 2048], BF16, 1)

        hb_next = hpool.next()
        P.dma(hb_next[:], h_in.h[0:128, :], reads=[h_in], writes=[hb_next])
        xprev = None
        for ch in range(NCH):
            hb = hb_next
            if ch + 1 < NCH:
                hb_next = hpool.next()
                P.dma(hb_next[:], h_in.h[(ch + 1) * 128:(ch + 2) * 128, :], reads=[h_in], writes=[hb_next])
            hnT = hnTp.next()
            norm_tile(P, C, hb[:, :], hb, hnT, 0)
            zs = zsp.next()
            for q in range(4):
                ps = P.psum_bank()
                mm_acc(P, ps[:, :], ps, [(hnT[:, kc, :], w_in[:, kc, q * 512:(q + 1) * 512]) for kc in range(8)], [hnT, w_in])
                P.act(lambda e, ps=ps, q=q, zs=zs: e.activation(zs[:, q * 512:(q + 1) * 512], ps[:, :], AF.Silu), reads=[ps], writes=[zs])
            xpre = xprep.next()
            if xprev is None:
                P.gp(lambda e, xpre=xpre: e.memset(xpre[:, :, 0:3], 0.0), writes=[xpre])
            else:
                P.gp(lambda e, xpre=xpre, xprev=xprev: e.tensor_copy(xpre[:, :, 0:3], xprev[:, :, 128:131]), reads=[xprev], writes=[xpre])
            for q in range(6):
                ps = P.psum_bank()
                for i in range(4):
                    ct = q * 4 + i
                    mm_acc(P, ps[:, i * 128:(i + 1) * 128], ps, [(w_in[:, kc, 2048 + ct * 128:2048 + (ct + 1) * 128], hnT[:, kc, :]) for kc in range(8)], [hnT, w_in])
                P.act(lambda e, ps=ps, q=q, xpre=xpre: e.activation(xpre[:, q * 4:(q + 1) * 4, 3:131], ps[:, :].rearrange("p (i t) -> p i t", i=4), AF.Copy), reads=[ps], writes=[xpre])
            xprev = xpre
            dts = dtp.next()
            ps = P.psum_bank()
            mm_acc(P, ps[:, 0:32], ps, [(hnT[:, kc, :], w_in[:, kc, 5120:5152]) for kc in range(8)], [hnT, w_in])
            P.dve(lambda e, ps=ps, dts=dts: e.tensor_tensor(dts[:, 0, :], ps[:, 0:32], dtb[:], ALU.add), reads=[ps, dtb], writes=[dts])
            P.act(lambda e, dts=dts: e.activation(dts[:, 0, :], dts[:, 0, :], AF.Exp), reads=[dts], writes=[dts])
            P.act(lambda e, dts=dts: e.activation(dts[:, 1, :], dts[:, 0, :], AF.Ln, bias=P.const(1.0)[:, 0:1]), reads=[dts, P.const(1.0)], writes=[dts])
            P.dve(lambda e, dts=dts: e.tensor_tensor(dts[:, 2, :], dts[:, 1, :], abc[:], ALU.mult), reads=[dts, abc], writes=[dts])
            pc = P.psum_bank()
            for i, mat in enumerate((tri, ustr, onesf)):
                P.pe(lambda e, pc=pc, i=i, mat=mat, dts=dts: e.matmul(pc[:, i * 32:(i + 1) * 32], mat[:], dts[:, 2, :], start=True, stop=True), reads=[mat, dts], writes=[pc])
            cs, ecs = csp.next(), ecp.next()
            P.act(lambda e, pc=pc, cs=cs: e.activation(cs[:], pc[:, 0:96], AF.Copy), reads=[pc], writes=[cs])
            P.act(lambda e, pc=pc, ecs=ecs: e.activation(ecs[:], pc[:, 0:96], AF.Exp), reads=[pc], writes=[ecs])
            P.dve(lambda e, dts=dts, ecs=ecs: e.tensor_tensor(dts[:, 3, :], dts[:, 1, :], ecs[:, 32:64], ALU.mult), reads=[dts, ecs], writes=[dts])
            xs = xsp.next()
            for q in range(4):
                ps = P.psum_bank()
                P.pe(lambda e, ps=ps, q=q: e.matmul(ps[:, :], ones1[0:1, :], cbrow[0:1, q * 512:(q + 1) * 512], start=True, stop=False), reads=[ones1, cbrow], writes=[ps])
                for i in range(4):
                    ct = q * 4 + i
                    for k in range(4):
                        last = (i == 3 and k == 3)
                        P.pe(lambda e, ps=ps, i=i, ct=ct, k=k, last=last, xpre=xpre: e.matmul(ps[:, i * 128:(i + 1) * 128], xpre[:, ct, k:k + 128], Dg[:, ct, k, :], start=False, stop=last, skip_group_check=True),
                             reads=[xpre, Dg], writes=[ps])
                P.act(lambda e, ps=ps, q=q, xs=xs: e.activation(xs[:, q * 512:(q + 1) * 512], ps[:, :], AF.Silu), reads=[ps], writes=[xs])
            btok = btp.next()
            ps = P.psum_bank()
            P.pe(lambda e, ps=ps: e.matmul(ps[:, :], ones1[0:1, :], cbrow[0:1, 2048:2560], start=True, stop=False), reads=[ones1, cbrow], writes=[ps])
            for i in range(4):
                ct = 16 + i
                for k in range(4):
                    last = (i == 3 and k == 3)
                    P.pe(lambda e, ps=ps, i=i, ct=ct, k=k, last=last, xpre=xpre: e.matmul(ps[:, i * 128:(i + 1) * 128], xpre[:, ct, k:k + 128], Dg[:, ct, k, :], start=False, stop=last, skip_group_check=True),
                         reads=[xpre, Dg], writes=[ps])
            P.act(lambda e, ps=ps, btok=btok: e.activation(btok[:], ps[:, :], AF.Silu), reads=[ps], writes=[btok])
            BT, CT = BTp.next(), CTp.next()
            for dst, base in ((BT, 16), (CT, 20)):
                ps = P.psum_bank()
                for i in range(4):
                    ct = base + i
                    mm_acc(P, ps[:, i * 128:(i + 1) * 128], ps, [(Dg[:, ct, k, :], xpre[:, ct, k:k + 128]) for k in range(4)], [xpre, Dg])
                for i in range(4):
                    ct = base + i
                    P.act(lambda e, ps=ps, i=i, ct=ct, dst=dst: e.activation(dst[:, i, :], ps[:, i * 128:(i + 1) * 128], AF.Silu, bias=cbcol[:, ct:ct + 1]), reads=[ps, cbcol], writes=[dst])
            ps = P.psum_bank()
            for g in range(4):
                P.pe(lambda e, ps=ps, g=g, BT=BT, CT=CT: e.matmul(ps[:, g * 128:(g + 1) * 128], BT[:, g, :], CT[:, g, :], start=True, stop=True), reads=[BT, CT], writes=[ps])
            cbt = cbtp.next()
            P.dve(lambda e, ps=ps, cbt=cbt: e.tensor_tensor(cbt[:, :, :], ps[:, :].rearrange("p (g t) -> p g t", g=4), tri[:, :].unsqueeze(1).broadcast_to([128, 4, 128]), ALU.mult),
                  reads=[ps, tri], writes=[cbt])
            yg = ygp.next()
            ygr = [Buf(yg.h, "yg_g%d" % g_) for g_ in range(4)]
            for g in range(4):
                gs = slice(g * 512, (g + 1) * 512)
                xdt, xw = xdtp.next(), xwp.next()
                P.dve(lambda e, xdt=xdt, xs=xs, dts=dts, g=g, gs=gs: e.tensor_tensor(xdt[:, :].rearrange("p (h d) -> p h d", h=8), xs[:, gs].rearrange("p (h d) -> p h d", h=8),
                                                                          dts[:, 1, g * 8:(g + 1) * 8].unsqueeze(2).broadcast_to([128, 8, 64]), ALU.mult), reads=[xs, dts], writes=[xdt])
                P.dve(lambda e, xw=xw, xs=xs, dts=dts, g=g, gs=gs: e.tensor_tensor(xw[:, :].rearrange("p (h d) -> p h d", h=8), xs[:, gs].rearrange("p (h d) -> p h d", h=8),
                                                                         dts[:, 3, g * 8:(g + 1) * 8].unsqueeze(2).broadcast_to([128, 8, 64]), ALU.mult), reads=[xs, dts], writes=[xw])
                MTs = []
                for half in range(2):
                    pr = P.psum_bank()
                    for i in range(4):
                        h = g * 8 + half * 4 + i
                        Wh = Wp.next()
                        P.act(lambda e, Wh=Wh, h=h, dts=dts: e.activation(Wh[:], ustr[:], AF.Copy, scale=dts[:, 2, h:h + 1]), reads=[ustr, dts], writes=[Wh])
                        P.pe(lambda e, pr=pr, i=i, Wh=Wh: e.matmul(pr[:, i * 128:(i + 1) * 128], Wh[:], tri[:], start=True, stop=True), reads=[tri, Wh], writes=[pr])
                    eL = eLp.next()
                    P.act(lambda e, pr=pr, eL=eL: e.activation(eL[:, :, :], pr[:, :].rearrange("p (i t) -> p i t", i=4), AF.Exp), reads=[pr], writes=[eL])
                    MT = MTp.next()
                    P.dve(lambda e, MT=MT, eL=eL, g=g, cbt=cbt: e.tensor_tensor(MT[:, :, :], eL[:, :, :], cbt[:, g, :].unsqueeze(1).broadcast_to([128, 4, 128]), ALU.mult), reads=[eL, cbt], writes=[MT])
                    MTs.append(MT)
                py = P.psum_bank()
                for hh in range(8):
                    h = g * 8 + hh
                    P.pe(lambda e, py=py, hh=hh, h=h, MT=MTs[hh // 4], xdt=xdt: e.matmul(py[:, hh * 64:(hh + 1) * 64], MT[:, hh % 4, :], xdt[:, hh * 64:(hh + 1) * 64], start=True, stop=True),
                         reads=[MTs[hh // 4], xdt], writes=[py])
                po = P.psum_bank()
                P.pe(lambda e, po=po, g=g, CT=CT: e.matmul(po[:, :], CT[:, g, :], stTb[:, g, :], start=True, stop=True), reads=[CT, stTb], writes=[po])
                yo = yop.next()
                P.dve(lambda e, yo=yo, po=po, g=g, ecs=ecs: e.tensor_tensor(yo[:, :].rearrange("p (h d) -> p h d", h=8), po[:, :].rearrange("p (h d) -> p h d", h=8),
                                                                      ecs[:, g * 8:(g + 1) * 8].unsqueeze(2).broadcast_to([128, 8, 64]), ALU.mult), reads=[po, ecs], writes=[yo])
                P.dve(lambda e, yo=yo, py=py: e.tensor_tensor(yo[:], py[:, :], yo[:], ALU.add), reads=[py, yo], writes=[yo])
                yd = yop.next()
                P.dve(lambda e, yd=yd, xs=xs, g=g, gs=gs: e.tensor_tensor(yd[:, :].rearrange("p (h d) -> p h d", h=8), xs[:, gs].rearrange("p (h d) -> p h d", h=8),
                                                                    dskb[:, g * 8:(g + 1) * 8].unsqueeze(2).broadcast_to([128, 8, 64]), ALU.mult), reads=[xs, dskb], writes=[yd])
                P.dve(lambda e, yd=yd, yo=yo: e.tensor_tensor(yd[:], yd[:], yo[:], ALU.add), reads=[yd, yo], writes=[yd])
                P.dve(lambda e, yd=yd, yg=yg, zs=zs, gs=gs: e.tensor_tensor(yg[:, gs], yd[:], zs[:, gs], ALU.mult), reads=[yd, zs], writes=[ygr[g]])
                pu = P.psum_bank()
                P.pe(lambda e, pu=pu, g=g, btok=btok, xw=xw, gs=gs: e.matmul(pu[:, :], btok[:, g * 128:(g + 1) * 128], xw[:, :], start=True, stop=True), reads=[btok, xw], writes=[pu])
                P.dve(lambda e, g=g, ecs=ecs: e.tensor_tensor(stT[:, g, :].rearrange("p (h d) -> p h d", h=8), stT[:, g, :].rearrange("p (h d) -> p h d", h=8),
                                                            ecs[:, 64 + g * 8:64 + (g + 1) * 8].unsqueeze(2).broadcast_to([128, 8, 64]), ALU.mult), reads=[stT, ecs], writes=[stT])
                P.dve(lambda e, g=g, pu=pu: e.tensor_tensor(stT[:, g, :], pu[:, :], stT[:, g, :], ALU.add), reads=[pu, stT], writes=[stT])
                P.act(lambda e, g=g: e.activation(stTb[:, g, :], stT[:, g, :], AF.Copy), reads=[stT], writes=[stTb])
            ss = C.small.next()
            yn = ynp.next()
            P.act(lambda e, yn=yn, ss=ss, yg=yg: e.activation(yn[:], yg[:], AF.Square, accum_out=ss[:, 0:1]), reads=ygr, writes=[yn, ss])
            P.act(lambda e, ss=ss: e.activation(ss[:, 1:2], ss[:, 0:1], AF.Sqrt, scale=1.0 / 2048.0, bias=C.eps[:, 0:1]), reads=[ss, C.eps], writes=[ss])
            P.dve(lambda e, ss=ss: e.reciprocal(ss[:, 2:3], ss[:, 1:2]), reads=[ss], writes=[ss])
            P.dve(lambda e, yn=yn, yg=yg, ss=ss: e.tensor_scalar(yn[:], yg[:], ss[:, 2:3], None, ALU.mult), reads=ygr + [ss], writes=[yn])
            P.dma(yn_out.h[ch * 128:(ch + 1) * 128, :], yn[:], reads=[yn], writes=[yn_out])


def phase_mamba_b(P, L, h_in, h_out, yn_in, odnorm_ap, w_out_ap):
    NCH = L // 128
    with P.phase():
        C = Ctx(P, stage_w=2048, stage_n=2)
        ident = C.ident
        go = load_gain(P, odnorm_ap, 16)
        w_out = P.sbuf("w_out", [128, 16, 1024], BF16)
        load_w(P, C, w_out, w_out_ap, go)
        hpool = P.pool_of("hblk", [128, 1024], F32, 3)
        ynp = P.pool_of("ynl", [128, 2048], BF16, 3)
        ynTp = P.pool_of("ynT", [128, 16, 128], BF16, 2)
        nxt = None
        P.begin_sched()

        def load(ch):
            hb, yn = hpool.next(), ynp.next()
            P.dma(hb[:], h_in.h[ch * 128:(ch + 1) * 128, :], reads=[h_in], writes=[hb])
            P.dma(yn[:], yn_in.h[ch * 128:(ch + 1) * 128, :], reads=[yn_in], writes=[yn])
            return hb, yn
        nxt = load(0)
        for ch in range(NCH):
            hb, yn = nxt
            if ch + 1 < NCH:
                nxt = load(ch + 1)
            ynT = ynTp.next()
            for c0 in (0, 8):
                pt = P.psum_tbank()
                for c in range(8):
                    P.pe(lambda e, c=c, c0=c0, pt=pt, yn=yn: e.transpose(pt[:, c * 128:(c + 1) * 128], yn[:, (c0 + c) * 128:(c0 + c + 1) * 128], ident[:]), reads=[yn, ident], writes=[pt])
                P.act(lambda e, pt=pt, c0=c0, ynT=ynT: e.activation(ynT[:, c0:c0 + 8, :], pt[:].rearrange("p (c t) -> p c t", c=8), AF.Copy), reads=[pt], writes=[ynT])
            for hf in range(2):
                po = P.psum_bank()
                mm_acc(P, po[:, :], po, [(ynT[:, kc, :], w_out[:, kc, hf * 512:(hf + 1) * 512]) for kc in range(16)], [ynT, w_out])
                P.dve(lambda e, po=po, hf=hf, hb=hb: e.tensor_tensor(hb[:, hf * 512:(hf + 1) * 512], po[:, :], hb[:, hf * 512:(hf + 1) * 512], ALU.add), reads=[po, hb], writes=[hb])
            P.dma(h_out.h[ch * 128:(ch + 1) * 128, :], hb[:], reads=[hb], writes=[h_out])


W_SHAPES = {
    "mem_norm": [1024], "norm_mix": [2, 1024], "norm_xattn": [2, 1024], "norm_ffn": [2, 1024],
    "xa_wq": [2, 1024, 1024], "xa_wk": [2, 1024, 1024], "xa_wv": [2, 1024, 1024], "xa_wo": [2, 1024, 1024],
    "ev_w_in": [1, 1024, 2560], "ev_s5_lam_re": [1, 16, 64], "ev_s5_lam_im": [1, 16, 64], "ev_s5_log_dt": [1, 16],
    "ev_s5_b_re": [1, 16, 64, 16], "ev_s5_b_im": [1, 16, 64, 16], "ev_s5_c_re": [1, 16, 16, 64], "ev_s5_c_im": [1, 16, 16, 64],
    "ev_s5_d": [1, 256], "ev_s5_w_glu": [1, 256, 256], "ev_s5_b_glu": [1, 256], "ev_w_out": [1, 1024, 1024],
    "ev_ffn_w1": [1, 1024, 2816], "ev_ffn_w3": [1, 1024, 2816], "ev_ffn_w2": [1, 2816, 1024],
    "od_w_in": [1, 1024, 5152], "od_conv_w": [1, 4, 3072], "od_conv_b": [1, 3072], "od_dt_bias": [1, 32], "od_a_log": [1, 32],
    "od_d": [1, 32], "od_norm": [1, 2048], "od_w_out": [1, 2048, 1024], "od_router": [1, 1024, 8],
    "od_moe_w1": [1, 8, 1024, 3584], "od_moe_w3": [1, 8, 1024, 3584], "od_moe_w2": [1, 8, 3584, 1024], "final_norm": [1024],
}


def build_program(L, phases=None):
    nc = bass.Bass("TRN2", target_bir_lowering=False)
    P = Prog(nc)
    x = P.dram("x", [L, 1024], F32, kind="ExternalInput")
    mem = nc.dram_tensor("mem", [256, 1024], F32, kind="ExternalInput").ap()
    pos = nc.dram_tensor("positions", [L], I32, kind="ExternalInput").ap()
    out = P.dram("out", [L, 1024], F32, kind="ExternalOutput")
    w = {n: nc.dram_tensor(n, s, F32, kind="ExternalInput").ap() for n, s in W_SHAPES.items()}
    hA = P.dram("hA", [L, 1024], F32)
    hB = P.dram("hB", [L, 1024], F32)
    yn = P.dram("yn", [L, 2048], BF16)
    steps = [
        lambda i, o: phase_even_mixer(P, L, i, o, pos, w["norm_mix"][0], w["ev_w_in"][0], w["ev_s5_lam_re"][0], w["ev_s5_lam_im"][0], w["ev_s5_log_dt"][0],
                                      w["ev_s5_b_re"][0], w["ev_s5_b_im"][0], w["ev_s5_c_re"][0], w["ev_s5_c_im"][0], w["ev_s5_d"][0], w["ev_s5_w_glu"][0],
                                      w["ev_s5_b_glu"][0], w["ev_w_out"][0]),
        lambda i, o: phase_xattn(P, L, i, o, mem, w["mem_norm"], w["norm_xattn"][0], w["xa_wq"][0], w["xa_wk"][0], w["xa_wv"][0], w["xa_wo"][0]),
        lambda i, o: phase_ffn(P, L, i, o, w["norm_ffn"][0], [w["ev_ffn_w1"][0]], [w["ev_ffn_w3"][0]], [w["ev_ffn_w2"][0]], 2816),
        lambda i, o: (phase_mamba_a(P, L, i, yn, w["norm_mix"][1], w["od_w_in"][0], w["od_conv_w"][0], w["od_conv_b"][0], w["od_dt_bias"][0], w["od_a_log"][0], w["od_d"][0]),
                      phase_mamba_b(P, L, i, o, yn, w["od_norm"][0], w["od_w_out"][0])),
        lambda i, o: phase_xattn(P, L, i, o, mem, w["mem_norm"], w["norm_xattn"][1], w["xa_wq"][1], w["xa_wk"][1], w["xa_wv"][1], w["xa_wo"][1]),
        lambda i, o: phase_ffn(P, L, i, None, w["norm_ffn"][1], [w["od_moe_w1"][0][e] for e in range(8)], [w["od_moe_w3"][0][e] for e in range(8)],
                               [w["od_moe_w2"][0][e] for e in range(8)], 3584, router_ap=w["od_router"][0], final_norm_ap=w["final_norm"], out_ap=o.h),
    ]
    if phases is None:
        phases = list(range(len(steps)))
    cur = x
    scratch = [hA, hB]
    for n, pi in enumerate(phases):
        dst = out if n == len(phases) - 1 else scratch[n % 2]
        steps[pi](cur, dst)
        cur = dst
    P.finish()
    return nc, P


def kernel(_phases=None, **inputs):
    x = np.asarray(inputs["x"], dtype=np.float32)
    B, L, _ = x.shape
    nc, P = build_program(L, _phases)
    shared = {n: np.ascontiguousarray(np.asarray(inputs[n], dtype=np.float32)) for n in W_SHAPES}
    mem = np.asarray(inputs["mem"], dtype=np.float32)
    pos = np.asarray(inputs["positions"], dtype=np.int32)
    in_maps = []
    for b in range(B):
        m = dict(shared)
        m["x"] = np.ascontiguousarray(x[b])
        m["mem"] = np.ascontiguousarray(mem[b])
        m["positions"] = np.ascontiguousarray(pos[b])
        in_maps.append(m)
    res = run_bass_kernel_spmd(nc, in_maps, core_ids=list(range(B)))
    return np.stack([np.asarray(r["out"], dtype=np.float32) for r in res.results], axis=0)
```

```python
import math
from contextlib import ExitStack
import numpy as np
import concourse.bass as bass
import concourse.mybir as mybir
from concourse.bass_utils import run_bass_kernel_spmd

F32 = mybir.dt.float32
BF16 = mybir.dt.bfloat16
I32 = mybir.dt.int32
AF = mybir.ActivationFunctionType
ALU = mybir.AluOpType
AX = mybir.AxisListType

SAME_ENGINE_SYNC = {"pe": False, "dve": True, "act": False, "pool": True, "sp": False}
STRICT_ELEMS = 256
N_DMA_SLOTS = 8
SCHEDULE = True
ACT_SWITCH = 1.3
ACT_SET = {AF.Exp: "explog", AF.Ln: "explog", AF.Silu: "silu", AF.Sqrt: "sqrt", AF.Sigmoid: "sig", AF.Sin: "sin"}


class Buf:
    def __init__(self, h, name, strict=False):
        self.h = h
        self.name = name
        self.writers = {}
        self.readers = {}
        self.strict = strict

    def __getitem__(self, idx):
        return self.h[idx]


class _CallRec:
    def __getattr__(self, name):
        def f(*a, **k):
            self.__dict__["call"] = (name, a, k)
            return self
        return f


class Prog:
    def __init__(self, nc):
        self.nc = nc
        self.es = ExitStack()
        self.es_global = self.es
        self._consts = {}
        self.eng = {"pe": nc.tensor, "dve": nc.vector, "act": nc.scalar, "pool": nc.gpsimd, "sp": nc.sync}
        self.sem = {}
        self.cnt = {}
        self.waited = {}
        for e in self.eng:
            self.sem[e] = self.es.enter_context(nc.semaphore("s_" + e))
            self.cnt[e] = 0
            self.waited[e] = {}
        self.dma_sem = {}
        self.dma_cnt = {}
        for q in ("sp", "act", "pool"):
            self.dma_sem[q] = [self.es.enter_context(nc.semaphore("d_%s%d" % (q, i))) for i in range(N_DMA_SLOTS)]
            self.dma_cnt[q] = 0
        self.semobj = dict(self.sem)
        for q in self.dma_sem:
            for i, s in enumerate(self.dma_sem[q]):
                self.semobj[("dma", q, i)] = s
        self._psum = []
        self._psum_i = 0
        self._ident = None
        self.n_inst = 0
        self.n_wait = 0
        self.uid = 0
        self._rec = None
        self.psum_banks()
        self.identity_bf16()
        for cv in (1e-6, 1.0, 0.0, -math.pi, math.pi, 0.5, 0.25):
            self.const(cv)

    def sbuf(self, name, shape, dtype):
        self.uid += 1
        h = self.es.enter_context(self.nc.sbuf_tensor("%s_%d" % (name, self.uid), list(shape), dtype))
        return Buf(h, name, strict=(int(np.prod(shape[1:])) <= STRICT_ELEMS))

    def psum(self, name, shape, dtype=F32):
        self.uid += 1
        h = self.es.enter_context(self.nc.psum_tensor("%s_%d" % (name, self.uid), list(shape), dtype))
        return Buf(h, name)

    def dram(self, name, shape, dtype, kind="Internal"):
        h = self.nc.dram_tensor(name, list(shape), dtype, kind=kind)
        return Buf(h.ap(), name)

    def pool_of(self, name, shape, dtype, n, space="sbuf"):
        bufs = [(self.sbuf if space == "sbuf" else self.psum)("%s%d" % (name, i), shape, dtype) for i in range(n)]
        return Rot(bufs)

    N_FBANK = 7

    def psum_banks(self):
        if not self._psum:
            g = self.es_global
            self._psum = [Buf(g.enter_context(self.nc.psum_tensor("bank%d" % i, [128, 512], F32)), "bank") for i in range(self.N_FBANK)]
            self._psumt = [Buf(g.enter_context(self.nc.psum_tensor("tbank%d" % i, [128, 1024], BF16)), "tbank") for i in range(8 - self.N_FBANK)]
            self._psumt_i = 0
        return self._psum

    def psum_bank(self):
        banks = self.psum_banks()
        b = banks[self._psum_i % len(banks)]
        self._psum_i += 1
        return b

    def psum_tbank(self):
        self.psum_banks()
        b = self._psumt[self._psumt_i % len(self._psumt)]
        self._psumt_i += 1
        return b

    def _deps(self, reads, writes):
        deps = {}
        self._strict_keys = set()
        for b in reads:
            for k, v in b.writers.items():
                if deps.get(k, 0) < v:
                    deps[k] = v
                if b.strict:
                    self._strict_keys.add(k)
        for b in writes:
            for d in (b.writers, b.readers):
                for k, v in d.items():
                    if deps.get(k, 0) < v:
                        deps[k] = v
                    if b.strict:
                        self._strict_keys.add(k)
        return deps

    def _emit_waits(self, e, deps):
        eng = self.eng[e]
        w = self.waited[e]
        for k, v in deps.items():
            if k == e and not SAME_ENGINE_SYNC[e]:
                continue
            if k == e and SAME_ENGINE_SYNC[e] == "strict" and k not in self._strict_keys:
                continue
            if w.get(k, 0) >= v:
                continue
            eng.wait_ge(self.semobj[k], v)
            w[k] = v
            self.n_wait += 1

    def _record(self, key, val, reads, writes):
        for b in reads:
            if b.readers.get(key, 0) < val:
                b.readers[key] = val
        for b in writes:
            if b.readers:
                b.writers = {key: val}
                b.readers = {}
            else:
                b.writers[key] = val

    def op(self, e, fn, reads=(), writes=(), cost=None):
        if self._rec is not None:
            r = _CallRec()
            fn(r)
            name, a, k = r.call
            tset = None
            if e == "act" and name == "activation":
                f_ = a[2] if len(a) > 2 else k.get("func")
                tset = ACT_SET.get(f_)
            self._rec.append((e, (lambda eng, name=name, a=a, k=k: getattr(eng, name)(*a, **k)), tuple(reads), tuple(writes), None, cost, tset))
            return
        deps = self._deps(reads, writes)
        self._emit_waits(e, deps)
        inst = fn(self.eng[e])
        self.cnt[e] += 1
        inst.then_inc(self.sem[e], 1)
        self._record(e, self.cnt[e], reads, writes)
        self.n_inst += 1

    def pe(self, fn, reads=(), writes=()):
        self.op("pe", fn, reads, writes)

    def dve(self, fn, reads=(), writes=()):
        self.op("dve", fn, reads, writes)

    def act(self, fn, reads=(), writes=()):
        self.op("act", fn, reads, writes)

    def gp(self, fn, reads=(), writes=()):
        self.op("pool", fn, reads, writes)

    def dma(self, out, in_, reads=(), writes=(), q="sp", **kw):
        if self._rec is not None:
            self._rec.append((q, None, tuple(reads), tuple(writes), (out, in_, kw), None, None))
            return
        k = self.dma_cnt[q]
        self.dma_cnt[q] += 1
        slot = k % N_DMA_SLOTS
        key = ("dma", q, slot)
        deps = self._deps(reads, writes)
        prev = 16 * (k // N_DMA_SLOTS)
        if prev > 0:
            deps[key] = max(deps.get(key, 0), prev)
        self._emit_waits(q, deps)
        inst = self.eng[q].dma_start(out=out, in_=in_, **kw)
        inst.then_inc(self.semobj[key], 16)
        self._record(key, prev + 16, reads, writes)
        self.n_inst += 1

    def identity_bf16(self):
        if self._ident is None:
            idf = Buf(self.es_global.enter_context(self.nc.sbuf_tensor("identf", [128, 128], F32)), "identf")
            idb = Buf(self.es_global.enter_context(self.nc.sbuf_tensor("identb", [128, 128], BF16)), "identb")
            self.gp(lambda e: e.memset(idf[:], 1.0), writes=[idf])
            self.gp(lambda e: e.affine_select(idf[:], idf[:], [[-1, 128]], ALU.is_equal, 0.0, base=0, channel_multiplier=1),
                    reads=[idf], writes=[idf])
            self.gp(lambda e: e.tensor_copy(idb[:], idf[:]), reads=[idf], writes=[idb])
            self._ident = idb
            self._identf = idf
        return self._ident

    EST = {"pe": 0.25, "act": 0.55, "dve": 0.65, "pool": 1.2, "sp": 0.15}
    LAT = {"pe": 0.3, "act": 0.5, "dve": 0.5, "pool": 0.8, "sp": 2.5}

    def begin_sched(self):
        if SCHEDULE:
            self._rec = []

    def end_sched(self):
        rec = self._rec
        self._rec = None
        if not rec:
            return
        n = len(rec)
        preds = [None] * n
        lastw = {}
        readers = {}
        for i, (e, fn, reads, writes, dmaargs, cost, tset) in enumerate(rec):
            p = set()
            for b in reads:
                w = lastw.get(id(b))
                if w is not None:
                    p.update(w)
            for b in writes:
                w = lastw.get(id(b))
                if w is not None:
                    p.update(w)
                r = readers.get(id(b))
                if r:
                    p.update(r)
            p.discard(i)
            preds[i] = p
            for b in reads:
                readers.setdefault(id(b), []).append(i)
            for b in writes:
                if readers.get(id(b)):
                    lastw[id(b)] = [i]
                    readers[id(b)] = []
                else:
                    lastw.setdefault(id(b), []).append(i)
        succs = [[] for _ in range(n)]
        npred = [0] * n
        for i in range(n):
            npred[i] = len(preds[i])
            for j in preds[i]:
                succs[j].append(i)
        import heapq
        fin = [0.0] * n
        rdy = [0.0] * n
        efree = {}
        ready = {}
        for i in range(n):
            if npred[i] == 0:
                ready.setdefault(rec[i][0], []).append(i)
        for e in ready:
            heapq.heapify(ready[e])
        done = 0
        cur_set = None
        while done < n:
            best, beste, bestt = None, None, None
            for e, hq in ready.items():
                if not hq:
                    continue
                i = hq[0]
                if e == "act" and len(hq) > 1:
                    ef = efree.get(e, 0.0)
                    bk = None
                    for j in hq:
                        ts_ = rec[j][6]
                        pen = ACT_SWITCH if (ts_ is not None and ts_ != cur_set) else 0.0
                        key = (max(ef, rdy[j]) + pen, j)
                        if bk is None or key < bk:
                            bk, i = key, j
                t = max(efree.get(e, 0.0), rdy[i])
                if bestt is None or t < bestt or (t == bestt and i < best):
                    best, beste, bestt = i, e, t
            i = best
            hq = ready[beste]
            if hq[0] == i:
                heapq.heappop(hq)
            else:
                hq.remove(i)
                heapq.heapify(hq)
            e, fn, reads, writes, dmaargs, cost, tset = rec[i]
            c = cost if cost is not None else self.EST[e]
            if tset is not None:
                if tset != cur_set:
                    c += ACT_SWITCH
                cur_set = tset
            efree[e] = bestt + c
            fin[i] = bestt + c + self.LAT[e]
            if dmaargs is None:
                self.op(e, fn, reads, writes)
            else:
                self.dma(dmaargs[0], dmaargs[1], reads=reads, writes=writes, q=e, **dmaargs[2])
            done += 1
            for j in succs[i]:
                npred[j] -= 1
                if rdy[j] < fin[i]:
                    rdy[j] = fin[i]
                if npred[j] == 0:
                    heapq.heappush(ready.setdefault(rec[j][0], []), j)

    def barrier(self):
        final = {}
        for e in self.eng:
            if self.cnt[e] > 0:
                final[e] = self.cnt[e]
        for q in self.dma_cnt:
            k = self.dma_cnt[q]
            for slot in range(N_DMA_SLOTS):
                n = (k - slot + N_DMA_SLOTS - 1) // N_DMA_SLOTS
                if n > 0:
                    final[("dma", q, slot)] = 16 * n
        for e in self.eng:
            self._emit_waits(e, {k: v for k, v in final.items() if k != e})

    def phase(self):
        return _Phase(self)

    def const(self, val):
        key = float(val)
        if key not in self._consts:
            c = Buf(self.es_global.enter_context(self.nc.sbuf_tensor("const_%d" % len(self._consts), [128, 1], F32)), "const")
            self.gp(lambda e: e.memset(c[:], key), writes=[c])
            self._consts[key] = c
        return self._consts[key]

    def finish(self):
        final = {}
        for e in self.eng:
            if self.cnt[e] > 0:
                final[e] = self.cnt[e]
        for q in self.dma_cnt:
            k = self.dma_cnt[q]
            for slot in range(N_DMA_SLOTS):
                n = (k - slot + N_DMA_SLOTS - 1) // N_DMA_SLOTS
                if n > 0:
                    final[("dma", q, slot)] = 16 * n
        self._emit_waits("sp", {k: v for k, v in final.items() if k != "sp"})
        self.es_global.close()


class _Phase:
    def __init__(self, P):
        self.P = P

    def __enter__(self):
        self.saved = self.P.es
        self.stack = ExitStack()
        self.P.es = self.stack
        return self

    def __exit__(self, *a):
        self.P.end_sched()
        self.P.barrier()
        self.P.es = self.saved
        self.stack.close()
        return False


class Rot:
    def __init__(self, bufs):
        self.bufs = bufs
        self.i = 0

    def next(self):
        b = self.bufs[self.i % len(self.bufs)]
        self.i += 1
        return b


EPS = 1e-6
D = 1024
KC = 8


class Ctx:
    def __init__(self, P, stage_w=2048, stage_n=3, junk_w=1024):
        self.P = P
        self.ident = P.identity_bf16()
        self.junk = P.pool_of("junk", [128, junk_w], BF16, 2)
        self.small = P.pool_of("small", [128, 4], F32, 4)
        self.xs = P.pool_of("xs", [128, 1024], BF16, 2)
        self.stage = P.pool_of("stage", [128, stage_w], F32, stage_n)
        self.stage_w = stage_w
        self.eps = P.const(EPS)


def load_gain(P, vec_ap, n):
    g = P.sbuf("gain", [128, n], F32)
    P.dma(g[:], vec_ap.rearrange("(c p) -> p c", p=128), writes=[g], allow_slow_non_contiguous=True)
    return g


def load_w(P, C, dst, src_ap, gain=None, kc0=0, nkc=None, f0=0, nf=None, dst_f0=0, eng="act"):
    K, F = src_ap.shape
    if nkc is None:
        nkc = K // 128
    if nf is None:
        nf = F
    FS = C.stage_w
    for kc in range(nkc):
        for c0 in range(0, nf, FS):
            n = min(FS, nf - c0)
            st = C.stage.next()
            P.dma(st[:, 0:n], src_ap[(kc0 + kc) * 128:(kc0 + kc + 1) * 128, f0 + c0:f0 + c0 + n], writes=[st])
            o = dst[:, kc, dst_f0 + c0:dst_f0 + c0 + n]
            if eng == "act":
                if gain is not None:
                    gap = gain[:, kc0 + kc:kc0 + kc + 1]
                    P.act(lambda e, o=o, st=st, n=n, gap=gap: e.activation(o, st[:, 0:n], AF.Copy, scale=gap), reads=[st, gain], writes=[dst])
                else:
                    P.act(lambda e, o=o, st=st, n=n: e.activation(o, st[:, 0:n], AF.Copy), reads=[st], writes=[dst])
            elif gain is not None:
                gap = gain[:, kc0 + kc:kc0 + kc + 1]
                P.op(eng, lambda e, o=o, st=st, n=n, gap=gap: e.tensor_scalar(o, st[:, 0:n], gap, None, ALU.mult),
                     reads=[st, gain], writes=[dst])
            else:
                P.op(eng, lambda e, o=o, st=st, n=n: e.tensor_copy(o, st[:, 0:n]), reads=[st], writes=[dst])


def norm_tile(P, C, src_ap, src_buf, hnT, col0, width=1024, eps_scale=None, track=None):
    nch = width // 128
    junk = C.junk.next()
    ss = C.small.next()
    srcs = list(src_buf) if isinstance(src_buf, (list, tuple)) else [src_buf]
    P.act(lambda e: e.activation(junk[:, 0:width], src_ap, AF.Square, accum_out=ss[:, 0:1]),
          reads=srcs, writes=[junk, ss])
    P.act(lambda e: e.activation(ss[:, 1:2], ss[:, 0:1], AF.Sqrt, scale=1.0 / width, bias=C.eps[:, 0:1]),
          reads=[ss, C.eps], writes=[ss])
    P.dve(lambda e: e.reciprocal(ss[:, 2:3], ss[:, 1:2]), reads=[ss], writes=[ss])
    xs = C.xs.next() if width <= 1024 else C.xs2.next()
    P.dve(lambda e: e.tensor_scalar(xs[:, 0:width], src_ap, ss[:, 2:3], None, ALU.mult), reads=srcs + [ss], writes=[xs])
    for c0 in range(0, nch, 8):
        pt = P.psum_tbank()
        for c in range(8):
            P.pe(lambda e, c=c: e.transpose(pt[:, c * 128:(c + 1) * 128], xs[:, (c0 + c) * 128:(c0 + c + 1) * 128], C.ident[:]),
                 reads=[xs, C.ident], writes=[pt])
        P.act(lambda e: e.activation(hnT[:, c0:c0 + 8, col0:col0 + 128], pt[:].rearrange("p (c t) -> p c t", c=8), AF.Copy),
              reads=[pt], writes=[track if track is not None else hnT])
    return ss


def mm_acc(P, ps_ap, ps_buf, pairs, reads):
    n = len(pairs)
    for i, (l, r) in enumerate(pairs):
        P.pe(lambda e, l=l, r=r, i=i: e.matmul(ps_ap, l, r, start=(i == 0), stop=(i == n - 1)), reads=reads, writes=[ps_buf])


def h_view(h_dram, t0, nt):
    return h_dram[t0:t0 + nt * 128, :].rearrange("(j p) d -> p j d", p=128)


def phase_xattn(P, L, h_in, h_out, mem_ap, mem_norm_ap, nx_ap, wq_ap, wk_ap, wv_ap, wo_ap):
    with P.phase():
        C = Ctx(P, stage_n=2)
        gq = load_gain(P, nx_ap, 8)
        gm = load_gain(P, mem_norm_ap, 8)
        wq = P.sbuf("wq", [128, 8, 1024], BF16)
        wk = P.sbuf("wk", [128, 8, 1024], BF16)
        wv = P.sbuf("wv", [128, 8, 1024], BF16)
        wo = P.sbuf("wo", [128, 8, 1024], BF16)
        load_w(P, C, wk, wk_ap, gm)
        load_w(P, C, wv, wv_ap, gm)
        load_w(P, C, wq, wq_ap, gq)
        load_w(P, C, wo, wo_ap, None)
        ones = P.sbuf("ones", [128, 128], BF16)
        P.gp(lambda e: e.memset(ones[:], 1.0), writes=[ones])
        memT = P.sbuf("memT", [128, 8, 256], BF16)
        mt = P.sbuf("mt", [128, 2, 1024], F32)
        P.dma(mt[:], h_view(mem_ap, 0, 2), writes=[mt])
        for j in range(2):
            norm_tile(P, C, mt[:, j, :], mt, memT, j * 128)
        kT = P.sbuf("kT", [128, 8, 256], BF16)
        for fc in range(8):
            ps = P.psum_bank()
            mm_acc(P, ps[:, 0:256], ps, [(wk[:, kc, fc * 128:(fc + 1) * 128], memT[:, kc, :]) for kc in range(8)], [wk, memT])
            P.act(lambda e, fc=fc, ps=ps: e.activation(kT[:, fc, :], ps[:, 0:256], AF.Copy), reads=[ps], writes=[kT])
        v = P.sbuf("v", [128, 2, 1024], BF16)
        for j in range(2):
            for hf in range(2):
                ps = P.psum_bank()
                mm_acc(P, ps[:, :], ps, [(memT[:, kc, j * 128:(j + 1) * 128], wv[:, kc, hf * 512:(hf + 1) * 512]) for kc in range(8)], [wv, memT])
                P.act(lambda e, j=j, hf=hf, ps=ps: e.activation(v[:, j, hf * 512:(hf + 1) * 512], ps[:, :], AF.Copy), reads=[ps], writes=[v])
        P.begin_sched()
        NB = L // 512
        hpool = P.pool_of("hblk", [128, 4, 1024], F32, 2)
        hnTp = P.pool_of("hnT", [128, 8, 512], BF16, 2)
        qTp = P.pool_of("qT", [128, 8, 512], BF16, 2)
        oTp = P.pool_of("oT", [128, 8, 512], BF16, 2)
        expp = P.pool_of("expT", [128, 2, 512], BF16, 3)
        rdp = P.pool_of("rden", [128, 512], F32, 3)
        hb_next = hpool.next()
        P.dma(hb_next[:], h_view(h_in.h, 0, 4), reads=[h_in], writes=[hb_next])
        for b in range(NB):
            hb = hb_next
            if b + 1 < NB:
                hb_next = hpool.next()
                P.dma(hb_next[:], h_view(h_in.h, (b + 1) * 512, 4), reads=[h_in], writes=[hb_next])
            hnT = hnTp.next()
            for j in range(4):
                norm_tile(P, C, hb[:, j, :], hb, hnT, j * 128)
            qT = qTp.next()
            for fc in range(8):
                ps = P.psum_bank()
                mm_acc(P, ps[:, :], ps, [(wq[:, kc, fc * 128:(fc + 1) * 128], hnT[:, kc, :]) for kc in range(8)], [wq, hnT])
                P.act(lambda e, fc=fc, ps=ps: e.activation(qT[:, fc, :], ps[:, :], AF.Copy, scale=1.0 / 16.0), reads=[ps], writes=[qT])
            oT = oTp.next()
            for hd in range(4):
                ex = expp.next()
                for j in range(2):
                    ps = P.psum_bank()
                    mm_acc(P, ps[:, :], ps, [(kT[:, fc, j * 128:(j + 1) * 128], qT[:, fc, :]) for fc in (2 * hd, 2 * hd + 1)], [kT, qT])
                    P.act(lambda e, j=j, ps=ps: e.activation(ex[:, j, :], ps[:, :], AF.Exp), reads=[ps], writes=[ex])
                den = P.psum_bank()
                mm_acc(P, den[:, :], den, [(ones[:, :], ex[:, j, :]) for j in range(2)], [ones, ex])
                rd = rdp.next()
                P.dve(lambda e, rd=rd, den=den: e.reciprocal(rd[:], den[:, :]), reads=[den], writes=[rd])
                for fc in (2 * hd, 2 * hd + 1):
                    ps = P.psum_bank()
                    mm_acc(P, ps[:, :], ps, [(v[:, j, fc * 128:(fc + 1) * 128], ex[:, j, :]) for j in range(2)], [v, ex])
                    P.dve(lambda e, fc=fc, ps=ps, rd=rd: e.tensor_tensor(oT[:, fc, :], ps[:, :], rd[:], ALU.mult), reads=[ps, rd], writes=[oT])
            for j in range(4):
                for hf in range(2):
                    ps = P.psum_bank()
                    mm_acc(P, ps[:, :], ps, [(oT[:, fc, j * 128:(j + 1) * 128], wo[:, fc, hf * 512:(hf + 1) * 512]) for fc in range(8)], [oT, wo])
                    P.dve(lambda e, j=j, hf=hf, ps=ps: e.tensor_tensor(hb[:, j, hf * 512:(hf + 1) * 512], ps[:, :], hb[:, j, hf * 512:(hf + 1) * 512], ALU.add),
                          reads=[ps, hb], writes=[hb])
            P.dma(h_view(h_out.h, b * 512, 4), hb[:], reads=[hb], writes=[h_out])


def phase_ffn(P, L, h_in, h_out, norm_ap, w1_aps, w3_aps, w2_aps, FF, router_ap=None,
              final_norm_ap=None, out_ap=None, TS=2048):
    NE = len(w1_aps)
    moe = router_ap is not None
    TS = min(TS, L)
    NT = TS // 128
    NBLK = TS // 512
    groups = [(g0, min(512, FF - g0)) for g0 in range(0, FF, 512)]
    with P.phase():
        C = Ctx(P, stage_w=1024, stage_n=3)
        gn = load_gain(P, norm_ap, 8)
        wpool = [P.pool_of(n, [128, 8, 512], BF16, 2) for n in ("w1g", "w3g")]
        w2pool = P.pool_of("w2g", [128, 4, 1024], BF16, 2)
        hres = P.sbuf("hres", [128, NT, 1024], F32)
        hreg = [[Buf(hres.h, "hres_%d_%d" % (j, hf)) for hf in range(2)] for j in range(NT)]
        hall = [hreg[j][hf] for j in range(NT) for hf in range(2)]
        hnT = P.sbuf("hnT", [128, 8, TS], BF16)
        hnTreg = [Buf(hnT.h, "hnT_blk%d" % i) for i in range(NBLK)]
        gTp = P.pool_of("gT", [128, 4, 512], BF16, 2)
        gTreg = {id(bf): [Buf(bf.h, "gT_fc%d" % fc) for fc in range(4)] for bf in gTp.bufs}
        sap = P.pool_of("sa", [128, 512], F32, 2)
        if moe:
            identf = P._identf
            rw = P.sbuf("rw", [128, 8, 8], F32)
            rst = P.sbuf("rst", [128, 8, 8], F32)
            P.dma(rst[:], router_ap.rearrange("(c p) e -> p c e", p=128), writes=[rst])
            for kc in range(8):
                P.gp(lambda e, kc=kc: e.tensor_scalar(rw[:, kc, :], rst[:, kc, :], gn[:, kc:kc + 1], None, ALU.mult),
                     reads=[rst, gn], writes=[rw])
            comb = P.sbuf("comb", [128, NT, 8], F32)
            xsf = P.sbuf("xsf", [128, 1024], F32)
            hnTf = P.sbuf("hnTf", [128, 8, 128], F32)
            rs = P.pool_of("rs", [128, 64], F32, 2)
        if final_norm_ap is not None:
            gfin = P.sbuf("gfin", [128, 1024], F32)
            P.dma(gfin[:], final_norm_ap.partition_broadcast(128), writes=[gfin])
            outp = P.pool_of("outt", [128, 1024], F32, 2)

        def load_group(e, gi):
            g0, gw = groups[gi]
            w1g, w3g = wpool[0].next(), wpool[1].next()
            w2g = w2pool.next()
            load_w(P, C, w1g, w1_aps[e], gn, f0=g0, nf=gw)
            load_w(P, C, w3g, w3_aps[e], gn, f0=g0, nf=gw)
            load_w(P, C, w2g, w2_aps[e], None, kc0=g0 // 128, nkc=gw // 128)
            return w1g, w3g, w2g

        work = [(e, gi) for e in range(NE) for gi in range(len(groups))]
        P.begin_sched()
        for st in range(L // TS):
            t0 = st * TS
            for j in range(NT):
                P.dma(hres[:, j, :], h_in.h[t0 + j * 128:t0 + (j + 1) * 128, :], reads=[h_in], writes=hreg[j])
            nxt = load_group(*work[0])
            for j in range(NT):
                ss = norm_tile(P, C, hres[:, j, :], hreg[j], hnT, j * 128, track=hnTreg[j // 4])
                if moe:
                    P.dve(lambda e, j=j, ss=ss: e.tensor_scalar(xsf[:], hres[:, j, :], ss[:, 2:3], None, ALU.mult), reads=hreg[j] + [ss], writes=[xsf])
                    for half in range(2):
                        pb = P.psum_bank()
                        for c in range(4):
                            P.pe(lambda e, c=c, pb=pb, half=half: e.transpose(pb[:, c * 128:(c + 1) * 128], xsf[:, (half * 4 + c) * 128:(half * 4 + c + 1) * 128], identf[:]),
                                 reads=[xsf, identf], writes=[pb])
                        P.act(lambda e, pb=pb, half=half: e.activation(hnTf[:, half * 4:half * 4 + 4, :], pb[:, :].rearrange("p (c t) -> p c t", c=4), AF.Copy),
                              reads=[pb], writes=[hnTf])
                    pl = P.psum_bank()
                    mm_acc(P, pl[:, 0:8], pl, [(hnTf[:, kc, :], rw[:, kc, :]) for kc in range(8)], [hnTf, rw])
                    r = rs.next()
                    P.dve(lambda e, r=r, pl=pl: e.tensor_copy(r[:, 0:8], pl[:, 0:8]), reads=[pl], writes=[r])
                    P.dve(lambda e, r=r: e.max(r[:, 8:16], r[:, 0:8]), reads=[r], writes=[r])
                    P.dve(lambda e, r=r: e.tensor_scalar(r[:, 16:24], r[:, 0:8], r[:, 9:10], None, ALU.is_ge), reads=[r], writes=[r])
                    P.dve(lambda e, r=r: e.tensor_scalar(r[:, 32:33], r[:, 8:9], -1.0, None, ALU.mult), reads=[r], writes=[r])
                    P.act(lambda e, r=r: e.activation(r[:, 24:32], r[:, 0:8], AF.Exp, bias=r[:, 32:33]), reads=[r], writes=[r])
                    P.dve(lambda e, r=r: e.tensor_tensor(r[:, 24:32], r[:, 24:32], r[:, 16:24], ALU.mult), reads=[r], writes=[r])
                    P.dve(lambda e, r=r: e.reduce_sum(r[:, 33:34], r[:, 24:32], AX.X), reads=[r], writes=[r])
                    P.dve(lambda e, r=r: e.reciprocal(r[:, 34:35], r[:, 33:34]), reads=[r], writes=[r])
                    P.dve(lambda e, r=r: e.tensor_scalar(r[:, 40:48], r[:, 24:32], r[:, 34:35], None, ALU.mult), reads=[r], writes=[r])
                    P.dve(lambda e, r=r, j=j: e.tensor_copy(comb[:, j, :], r[:, 40:48]), reads=[r], writes=[comb])
            units = [(wi, blk) for wi in range(len(work)) for blk in range(NBLK)]
            grp = {0: nxt}
            if len(work) > 1:
                grp[1] = load_group(*work[1])
            gts = {}

            def up(u):
                wi, blk = units[u]
                w1g, w3g, w2g = grp[wi]
                nfc = groups[work[wi][1]][1] // 128
                c0 = blk * 512
                gT = gTp.next()
                gR = gTreg[id(gT)]
                gts[u] = (gT, gR)
                for fc in range(nfc):
                    pa = P.psum_bank()
                    mm_acc(P, pa[:, :], pa, [(w1g[:, kc, fc * 128:(fc + 1) * 128], hnT[:, kc, c0:c0 + 512]) for kc in range(8)], [w1g, hnTreg[blk]])
                    pc_ = P.psum_bank()
                    mm_acc(P, pc_[:, :], pc_, [(w3g[:, kc, fc * 128:(fc + 1) * 128], hnT[:, kc, c0:c0 + 512]) for kc in range(8)], [w3g, hnTreg[blk]])
                    sa = sap.next()
                    P.act(lambda e, sa=sa, pa=pa: e.activation(sa[:], pa[:, :], AF.Silu), reads=[pa], writes=[sa])
                    P.dve(lambda e, sa=sa, pc_=pc_, gT=gT, fc=fc: e.tensor_tensor(gT[:, fc, :], pc_[:, :], sa[:], ALU.mult), reads=[pc_, sa], writes=[gR[fc]])

            def down(u):
                wi, blk = units[u]
                w1g, w3g, w2g = grp[wi]
                e_ = work[wi][0]
                nfc = groups[work[wi][1]][1] // 128
                gT, gR = gts.pop(u)
                for j in range(4):
                    jt = blk * 4 + j
                    for hf in range(2):
                        po = P.psum_bank()
                        mm_acc(P, po[:, :], po, [(gT[:, fc, j * 128:(j + 1) * 128], w2g[:, fc, hf * 512:(hf + 1) * 512]) for fc in range(nfc)], gR[0:nfc] + [w2g])
                        hs = hres[:, jt, hf * 512:(hf + 1) * 512]
                        if moe:
                            P.dve(lambda e, po=po, hs=hs, jt=jt, e_=e_: e.scalar_tensor_tensor(hs, po[:, :], comb[:, jt, e_:e_ + 1], hs, ALU.mult, ALU.add),
                                  reads=[po, comb, hreg[jt][hf]], writes=[hreg[jt][hf]])
                        else:
                            P.dve(lambda e, po=po, hs=hs: e.tensor_tensor(hs, po[:, :], hs, ALU.add), reads=[po, hreg[jt][hf]], writes=[hreg[jt][hf]])

            up(0)
            for u in range(1, len(units)):
                up(u)
                down(u - 1)
                wi_prev, blk_prev = units[u - 1]
                if blk_prev == NBLK - 1 and wi_prev + 2 < len(work):
                    del grp[wi_prev]
                    grp[wi_prev + 2] = load_group(*work[wi_prev + 2])
            down(len(units) - 1)
            if final_norm_ap is None:
                P.dma(h_view(h_out.h, t0, NT), hres[:], reads=hall, writes=[h_out])
            else:
                for j in range(NT):
                    junk = C.junk.next()
                    ss = C.small.next()
                    P.act(lambda e, junk=junk, ss=ss, j=j: e.activation(junk[:, 0:1024], hres[:, j, :], AF.Square, accum_out=ss[:, 0:1]), reads=hreg[j], writes=[junk, ss])
                    P.act(lambda e, ss=ss: e.activation(ss[:, 1:2], ss[:, 0:1], AF.Sqrt, scale=1.0 / 1024, bias=C.eps[:, 0:1]), reads=[ss, C.eps], writes=[ss])
                    P.dve(lambda e, ss=ss: e.reciprocal(ss[:, 2:3], ss[:, 1:2]), reads=[ss], writes=[ss])
                    ot = outp.next()
                    P.dve(lambda e, ot=ot, ss=ss, j=j: e.scalar_tensor_tensor(ot[:], hres[:, j, :], ss[:, 2:3], gfin[:], ALU.mult, ALU.mult),
                          reads=hreg[j] + [ss, gfin], writes=[ot])
                    P.dma(out_ap[t0 + j * 128:t0 + (j + 1) * 128, :], ot[:], reads=[ot])


def sincos(P, T, cyc_ap, cyc_buf, N, sin_ap=None, sin_buf=None, cos_ap=None, cos_buf=None):
    for dst, dbuf, off in ((sin_ap, sin_buf, 0.0), (cos_ap, cos_buf, 0.25)):
        if dst is None:
            continue
        c, ci, r, m = T["c"], T["ci"], T["r"], T["m"]
        P.dve(lambda e: e.tensor_scalar(c[:, 0:N], cyc_ap, off, None, ALU.add), reads=[cyc_buf], writes=[c])
        P.dve(lambda e: e.tensor_copy(ci[:, 0:N], c[:, 0:N]), reads=[c], writes=[ci])
        P.dve(lambda e: e.tensor_copy(r[:, 0:N], ci[:, 0:N]), reads=[ci], writes=[r])
        P.dve(lambda e: e.tensor_tensor(r[:, 0:N], c[:, 0:N], r[:, 0:N], ALU.subtract), reads=[c, r], writes=[r])
        P.dve(lambda e: e.tensor_scalar(m[:, 0:N], r[:, 0:N], 0.5, None, ALU.is_gt), reads=[r], writes=[m])
        P.dve(lambda e: e.tensor_tensor(r[:, 0:N], r[:, 0:N], m[:, 0:N], ALU.subtract), reads=[r, m], writes=[r])
        P.dve(lambda e: e.tensor_scalar(m[:, 0:N], r[:, 0:N], -0.5, None, ALU.is_lt), reads=[r], writes=[m])
        P.dve(lambda e: e.tensor_tensor(r[:, 0:N], r[:, 0:N], m[:, 0:N], ALU.add), reads=[r, m], writes=[r])
        P.act(lambda e, dst=dst: e.activation(dst, r[:, 0:N], AF.Sin, scale=2.0 * math.pi * (1.0 - 1e-6)), reads=[r], writes=[dbuf])


def trig_scratch(P, N):
    return {"c": P.sbuf("tc", [128, N], F32), "ci": P.sbuf("tci", [128, N], I32),
            "r": P.sbuf("tr", [128, N], F32), "m": P.sbuf("tm", [128, N], F32)}


RET_LG = [math.log1p(-(2.0 ** (-5.0 - h))) for h in range(6)]


def phase_even_mixer(P, L, h_in, h_out, pos_ap, norm_ap, w_in_ap, lam_re_ap, lam_im_ap, log_dt_ap, b_re_ap, b_im_ap,
                     c_re_ap, c_im_ap, d_ap, wglu_ap, bglu_ap, w_out_ap, BLK=256):
    NCH = L // 128
    NB = L // BLK
    JB = BLK // 128
    with P.phase():
        C = Ctx(P, stage_w=16, stage_n=1)
        ident = C.ident
        gn = load_gain(P, norm_ap, 8)
        w_in = P.sbuf("w_in", [128, 8, 2560], BF16)
        w_out = P.sbuf("w_out", [128, 8, 1024], BF16)
        wglu = P.sbuf("wglu", [128, 2, 256], BF16)
        rcos = P.sbuf("rcos", [128, NCH, 32], F32)
        rsin = P.sbuf("rsin", [128, NCH, 32], F32)
        dqk = P.sbuf("dqk", [128, 12], F32)
        gS = P.sbuf("gS", [128, 3], F32)
        g128 = P.sbuf("g128", [128, 3], F32)
        g127 = P.sbuf("g127", [128, 3], F32)
        maskT = P.sbuf("maskT", [128, 128], F32)
        S = P.sbuf("Sst", [128, 3, 128], F32)
        Sbfp = P.pool_of("Sbf", [128, 3, 128], BF16, 2)
        BB = [P.sbuf("BB%d" % i, [128, 8, 128], BF16) for i in range(2)]
        sp = P.sbuf("s5p", [128, 16, 8], F32)
        DT, TH, RHO, CB, SB, AR, AI, DEN, FR, FI, T1, T2 = [sp[:, i, :] for i in range(12)]
        cosT = P.sbuf("cosT", [128, 8, BLK], F32); sinT = P.sbuf("sinT", [128, 8, BLK], F32); rhoB = P.sbuf("rhoB", [128, 8, BLK], F32)
        Cm = [P.sbuf("Cm%d" % i, [128, 8, 128], BF16) for i in range(2)]
        dsk = load_gain(P, d_ap, 2)
        bgl = load_gain(P, bglu_ap, 2)
        zl = P.sbuf("zlast", [128, 2, 8], F32)
        zi = P.sbuf("zinit", [128, 2, 8], F32)
        zt = P.sbuf("ztmp", [128, 2, 8], F32)
        with P.phase():
            CS = Ctx(P, stage_w=2048, stage_n=2)
            load_w(P, CS, w_in, w_in_ap, gn)
            load_w(P, CS, w_out, w_out_ap, None)
            load_w(P, CS, wglu, wglu_ap, None)
            T = trig_scratch(P, 1024)
            posi = P.sbuf("posi", [128, NCH], I32)
            P.dma(posi[:], pos_ap.rearrange("(c p) -> p c", p=128), writes=[posi], allow_slow_non_contiguous=True)
            posf = P.sbuf("posf", [128, NCH], F32)
            P.dve(lambda e: e.tensor_copy(posf[:], posi[:]), reads=[posi], writes=[posf])
            ji = P.sbuf("ji", [128, 32], I32)
            P.gp(lambda e: e.iota(ji[:], [[1, 32]], base=0, channel_multiplier=0), writes=[ji])
            invf = P.sbuf("invf", [128, 32], F32)
            P.dve(lambda e: e.tensor_copy(invf[:], ji[:]), reads=[ji], writes=[invf])
            P.act(lambda e: e.activation(invf[:], invf[:], AF.Exp, scale=-math.log(10000.0) / 32.0), reads=[invf], writes=[invf])
            ang = P.sbuf("ang", [128, 32, 32], F32)
            for c0 in range(0, NCH, 32):
                n = min(32, NCH - c0)
                P.dve(lambda e, c0=c0, n=n: e.tensor_tensor(ang[:, 0:n, :], posf[:, c0:c0 + n].unsqueeze(2).broadcast_to([128, n, 32]),
                                                           invf[:, :].unsqueeze(1).broadcast_to([128, n, 32]), ALU.mult), reads=[posf, invf], writes=[ang])
                P.dve(lambda e, n=n: e.tensor_scalar(ang[:, 0:n, :], ang[:, 0:n, :], 1.0 / (2.0 * math.pi), None, ALU.mult), reads=[ang], writes=[ang])
                sincos(P, T, ang[:, 0:n, :].rearrange("p a b -> p (a b)"), ang, n * 32,
                       rsin[:, c0:c0 + n, :].rearrange("p a b -> p (a b)"), rsin, rcos[:, c0:c0 + n, :].rearrange("p a b -> p (a b)"), rcos)
            ti = P.sbuf("ti", [128, 1], I32)
            P.gp(lambda e: e.iota(ti[:], [[0, 1]], base=0, channel_multiplier=1), writes=[ti])
            tf = P.sbuf("tf", [128, 1], F32)
            P.dve(lambda e: e.tensor_copy(tf[:], ti[:]), reads=[ti], writes=[tf])
            for h in range(6):
                P.act(lambda e, h=h: e.activation(dqk[:, h:h + 1], tf[:], AF.Exp, scale=RET_LG[h]), reads=[tf], writes=[dqk])
                P.act(lambda e, h=h: e.activation(dqk[:, 6 + h:7 + h], tf[:], AF.Exp, scale=-RET_LG[h]), reads=[tf], writes=[dqk])
            P.dve(lambda e: e.tensor_scalar(dqk[:, 6:12], dqk[:, 6:12], 0.125, None, ALU.mult), reads=[dqk], writes=[dqk])
            for h in range(6):
                m, hp = h // 2, h % 2
                for tb, val in ((gS, math.exp(RET_LG[h])), (g128, math.exp(128 * RET_LG[h])), (g127, math.exp(127 * RET_LG[h]))):
                    P.gp(lambda e, tb=tb, val=val, m=m, hp=hp: e.memset(tb[hp * 64:(hp + 1) * 64, m:m + 1], val), writes=[tb])
            P.gp(lambda e: e.memset(maskT[:], 1.0), writes=[maskT])
            P.gp(lambda e: e.affine_select(maskT[:], maskT[:], [[1, 128]], ALU.is_ge, 0.0, base=0, channel_multiplier=-1), reads=[maskT], writes=[maskT])
            P.gp(lambda e: e.memset(S[:], 0.0), writes=[S])
            LR = P.sbuf("LR", [128, 8], F32); LI = P.sbuf("LI", [128, 8], F32); LD = P.sbuf("LD", [128, 8], F32)
            for two in range(2):
                P.dma(LR[two * 64:(two + 1) * 64, :], lam_re_ap.rearrange("(st two) p -> two p st", two=2)[two], writes=[LR], allow_slow_non_contiguous=True)
                P.dma(LI[two * 64:(two + 1) * 64, :], lam_im_ap.rearrange("(st two) p -> two p st", two=2)[two], writes=[LI], allow_slow_non_contiguous=True)
                P.dma(LD[two * 64:(two + 1) * 64, :], log_dt_ap.rearrange("(st two) -> two st", two=2)[two].partition_broadcast(64), writes=[LD], allow_slow_non_contiguous=True)
            BBf = [P.sbuf("BBf%d" % i, [128, 8, 128], F32) for i in range(2)]
            Cf = [P.sbuf("Cf%d" % i, [128, 8, 128], F32) for i in range(2)]
            for t_ in BBf + Cf:
                P.gp(lambda e, t_=t_: e.memset(t_[:], 0.0), writes=[t_])
            for g in range(16):
                st, two, r0 = g // 2, g % 2, (g % 8) * 16
                for i, (bap, cap) in enumerate(((b_re_ap, c_re_ap), (b_im_ap, c_im_ap))):
                    P.dma(BBf[i][r0:r0 + 16, st, two * 64:(two + 1) * 64], bap[g].rearrange("p c -> c p"), writes=[BBf[i]], allow_slow_non_contiguous=True)
                    P.dma(Cf[i][two * 64:(two + 1) * 64, st, r0:r0 + 16], cap[g].rearrange("c p -> p c"), writes=[Cf[i]], allow_slow_non_contiguous=True)
            for i in range(2):
                P.dve(lambda e, i=i: e.tensor_copy(BB[i][:], BBf[i][:]), reads=[BBf[i]], writes=[BB[i]])

            def sop(fn, eng="dve"):
                P.op(eng, fn, reads=[sp, LR, LI, LD], writes=[sp])
            sop(lambda e: e.activation(DT, LD[:], AF.Exp), "act")
            sop(lambda e: e.tensor_tensor(TH, LI[:], DT, ALU.mult))
            sop(lambda e: e.tensor_tensor(RHO, LR[:], DT, ALU.mult))
            sop(lambda e: e.activation(RHO, RHO, AF.Exp), "act")
            taui = P.sbuf("taui", [128, BLK], I32)
            P.gp(lambda e: e.iota(taui[:], [[1, BLK]], base=0, channel_multiplier=0), writes=[taui])
            tauc = P.sbuf("tauc", [128, BLK], F32)
            P.dve(lambda e: e.tensor_copy(tauc[:], taui[:]), reads=[taui], writes=[tauc])
            P.dve(lambda e: e.tensor_scalar(tauc[:], tauc[:], 1.0 / (2.0 * math.pi), None, ALU.mult), reads=[tauc], writes=[tauc])
            cyc = P.sbuf("cyc", [128, BLK], F32)
            for st in range(8):
                P.dve(lambda e, st=st: e.tensor_scalar(cyc[:], tauc[:], sp[:, 1, st:st + 1], None, ALU.mult), reads=[tauc, sp], writes=[cyc])
                sincos(P, T, cyc[:, :], cyc, BLK, sinT[:, st, :], sinT, cosT[:, st, :], cosT)
                P.dve(lambda e, st=st: e.tensor_copy(rhoB[:, st, :], sp[:, 2, st:st + 1].broadcast_to([128, BLK])), reads=[sp], writes=[rhoB])
            cyc8 = P.sbuf("cyc8", [128, 8], F32)
            P.dve(lambda e: e.tensor_scalar(cyc8[:], TH, float(BLK) / (2.0 * math.pi), None, ALU.mult), reads=[sp], writes=[cyc8])
            sincos(P, T, cyc8[:, :], cyc8, 8, SB, sp, CB, sp)
            P.dve(lambda e: e.tensor_tensor(AR, RHO, cosT[:, :, 1], ALU.mult), reads=[sp, cosT], writes=[sp])
            P.dve(lambda e: e.tensor_tensor(AI, RHO, sinT[:, :, 1], ALU.mult), reads=[sp, sinT], writes=[sp])
            sop(lambda e: e.tensor_scalar(AR, AR, -1.0, None, ALU.add))
            sop(lambda e: e.tensor_tensor(DEN, LR[:], LR[:], ALU.mult))
            sop(lambda e: e.tensor_tensor(T1, LI[:], LI[:], ALU.mult))
            sop(lambda e: e.tensor_tensor(DEN, DEN, T1, ALU.add))
            sop(lambda e: e.reciprocal(DEN, DEN))
            sop(lambda e: e.tensor_tensor(T1, AR, LR[:], ALU.mult))
            sop(lambda e: e.tensor_tensor(T2, AI, LI[:], ALU.mult))
            sop(lambda e: e.tensor_tensor(FR, T1, T2, ALU.add))
            sop(lambda e: e.tensor_tensor(FR, FR, DEN, ALU.mult))
            sop(lambda e: e.tensor_tensor(T1, AI, LR[:], ALU.mult))
            sop(lambda e: e.tensor_tensor(T2, AR, LI[:], ALU.mult))
            sop(lambda e: e.tensor_tensor(FI, T1, T2, ALU.subtract))
            sop(lambda e: e.tensor_tensor(FI, FI, DEN, ALU.mult))
            ct = P.sbuf("ct", [128, 2, 128], F32)
            for st in range(8):
                fr, fi = sp[:, 8, st:st + 1], sp[:, 9, st:st + 1]
                P.dve(lambda e, st=st, fi=fi: e.tensor_scalar(ct[:, 0, :], Cf[1][:, st, :], fi, None, ALU.mult), reads=[Cf[1], sp], writes=[ct])
                P.dve(lambda e, st=st, fr=fr: e.scalar_tensor_tensor(ct[:, 1, :], Cf[0][:, st, :], fr, ct[:, 0, :], ALU.mult, ALU.subtract), reads=[Cf[0], sp, ct], writes=[ct])
                P.dve(lambda e, st=st: e.tensor_copy(Cm[0][:, st, :], ct[:, 1, :]), reads=[ct], writes=[Cm[0]])
                P.dve(lambda e, st=st, fr=fr: e.tensor_scalar(ct[:, 0, :], Cf[1][:, st, :], fr, None, ALU.mult), reads=[Cf[1], sp], writes=[ct])
                P.dve(lambda e, st=st, fi=fi: e.scalar_tensor_tensor(ct[:, 1, :], Cf[0][:, st, :], fi, ct[:, 0, :], ALU.mult, ALU.add), reads=[Cf[0], sp, ct], writes=[ct])
                P.dve(lambda e, st=st: e.tensor_scalar(Cm[1][:, st, :], ct[:, 1, :], -1.0, None, ALU.mult), reads=[ct], writes=[Cm[1]])
            P.gp(lambda e: e.memset(zl[:], 0.0), writes=[zl])

        P.begin_sched()
        hpool = P.pool_of("hblk", [128, JB, 1024], F32, 2)
        hnTp = P.pool_of("hnT", [128, 8, BLK], BF16, 1)
        ycp = P.pool_of("ycat", [128, 8, BLK], BF16, 1)
        uTp = P.pool_of("uT", [128, 2, BLK], BF16, 1)
        uTfp = P.pool_of("uTf", [128, 2, BLK], F32, 1)
        xtp = P.pool_of("xt", [128, 2, BLK], F32, 2)
        wkp = P.pool_of("wk", [128, 2, BLK], F32, 2)
        zp = P.pool_of("z", [128, 2, BLK], F32, 2)
        sbp = P.pool_of("sbf", [128, 2, 4, BLK], BF16, 1)
        yfp = P.pool_of("yf", [128, BLK], F32, 2)
        y2p = P.pool_of("y2", [128, BLK], F32, 2)
        ybp = P.pool_of("yb", [128, 2, BLK], BF16, 1)
        qkp = P.pool_of("qk", [128, 768], F32, 2)
        r1p = P.pool_of("r1", [128, 768], F32, 1)
        r2p = P.pool_of("r2", [128, 768], F32, 1)
        qksp = P.pool_of("qks", [128, 768], BF16, 2)
        qkTp = P.pool_of("qkT", [128, 6, 128], BF16, 2)
        vsp = P.pool_of("vs", [128, 768], BF16, 2)
        sgp = P.pool_of("sg", [128, 768], F32, 2)
        Mp = P.pool_of("M", [128, 128], BF16, 6)
        ysbp = P.pool_of("ysb", [128, 768], F32, 1)
        ysqp = P.pool_of("ysq", [128, 768], F32, 1)
        ynp = P.pool_of("yn", [128, 768], BF16, 2)
        st6p = P.pool_of("st6", [128, 3, 6], F32, 2)

        hb_next = hpool.next()
        P.dma(hb_next[:], h_view(h_in.h, 0, JB), reads=[h_in], writes=[hb_next])
        for b in range(NB):
            hb = hb_next
            if b + 1 < NB:
                hb_next = hpool.next()
                P.dma(hb_next[:], h_view(h_in.h, (b + 1) * BLK, JB), reads=[h_in], writes=[hb_next])
            hnT = hnTp.next()
            for j in range(JB):
                norm_tile(P, C, hb[:, j, :], hb, hnT, j * 128)
            ycat = ycp.next()
            uT, uTf = uTp.next(), uTfp.next()
            for c in range(2):
                ps = P.psum_bank()
                mm_acc(P, ps[:, 0:BLK], ps, [(w_in[:, kc, 2304 + c * 128:2304 + (c + 1) * 128], hnT[:, kc, :]) for kc in range(8)], [w_in, hnT])
                P.act(lambda e, c=c, ps=ps: e.activation(uT[:, c, :], ps[:, 0:BLK], AF.Copy), reads=[ps], writes=[uT])
                P.act(lambda e, c=c, ps=ps: e.activation(uTf[:, c, :], ps[:, 0:BLK], AF.Copy), reads=[ps], writes=[uTf])
            P.dve(lambda e: e.tensor_tensor(zt[:, 0, :], CB, zl[:, 0, :], ALU.mult), reads=[sp, zl], writes=[zt])
            P.dve(lambda e: e.tensor_tensor(zt[:, 1, :], SB, zl[:, 1, :], ALU.mult), reads=[sp, zl], writes=[zt])
            P.dve(lambda e: e.tensor_tensor(zi[:, 0, :], zt[:, 0, :], zt[:, 1, :], ALU.subtract), reads=[zt], writes=[zi])
            P.dve(lambda e: e.tensor_tensor(zt[:, 0, :], SB, zl[:, 0, :], ALU.mult), reads=[sp, zl], writes=[zt])
            P.dve(lambda e: e.tensor_tensor(zt[:, 1, :], CB, zl[:, 1, :], ALU.mult), reads=[sp, zl], writes=[zt])
            P.dve(lambda e: e.tensor_tensor(zi[:, 1, :], zt[:, 0, :], zt[:, 1, :], ALU.add), reads=[zt], writes=[zi])
            sbf = sbp.next()
            for st in range(8):
                c = st // 4
                pr, pi_ = P.psum_bank(), P.psum_bank()
                P.pe(lambda e, st=st, c=c, pr=pr: e.matmul(pr[:, 0:BLK], BB[0][:, st, :], uT[:, c, :], start=True, stop=True), reads=[BB[0], uT], writes=[pr])
                P.pe(lambda e, st=st, c=c, pi_=pi_: e.matmul(pi_[:, 0:BLK], BB[1][:, st, :], uT[:, c, :], start=True, stop=True), reads=[BB[1], uT], writes=[pi_])
                xt, wk = xtp.next(), wkp.next()
                P.dve(lambda e, st=st, pr=pr, wk=wk: e.tensor_tensor(wk[:, 0, :], pr[:, 0:BLK], cosT[:, st, :], ALU.mult), reads=[pr, cosT], writes=[wk])
                P.dve(lambda e, st=st, pi_=pi_, wk=wk: e.tensor_tensor(wk[:, 1, :], pi_[:, 0:BLK], sinT[:, st, :], ALU.mult), reads=[pi_, sinT], writes=[wk])
                P.gp(lambda e, xt=xt, wk=wk: e.tensor_tensor(xt[:, 0, :], wk[:, 0, :], wk[:, 1, :], ALU.add), reads=[wk], writes=[xt])
                wk2 = wkp.next()
                P.dve(lambda e, st=st, pi_=pi_, wk2=wk2: e.tensor_tensor(wk2[:, 0, :], pi_[:, 0:BLK], cosT[:, st, :], ALU.mult), reads=[pi_, cosT], writes=[wk2])
                P.dve(lambda e, st=st, pr=pr, wk2=wk2: e.tensor_tensor(wk2[:, 1, :], pr[:, 0:BLK], sinT[:, st, :], ALU.mult), reads=[pr, sinT], writes=[wk2])
                P.gp(lambda e, xt=xt, wk2=wk2: e.tensor_tensor(xt[:, 1, :], wk2[:, 0, :], wk2[:, 1, :], ALU.subtract), reads=[wk2], writes=[xt])
                z = zp.next()
                for ri in range(2):
                    P.dve(lambda e, z=z, xt=xt, ri=ri, st=st: e.tensor_tensor_scan(z[:, ri, :], rhoB[:, st, :], xt[:, ri, :], zi[:, ri, st:st + 1], ALU.mult, ALU.add),
                          reads=[rhoB, xt, zi], writes=[z])
                P.dve(lambda e, z=z, st=st: e.tensor_copy(zl[:, :, st:st + 1], z[:, :, BLK - 1:BLK]), reads=[z], writes=[zl])
                w3, w4 = wkp.next(), wkp.next()
                P.gp(lambda e, w3=w3, z=z, st=st: e.tensor_tensor(w3[:, 0, :], z[:, 0, :], cosT[:, st, :], ALU.mult), reads=[z, cosT], writes=[w3])
                P.gp(lambda e, w3=w3, z=z, st=st: e.tensor_tensor(w3[:, 1, :], z[:, 1, :], sinT[:, st, :], ALU.mult), reads=[z, sinT], writes=[w3])
                P.gp(lambda e, w3=w3, st=st: e.tensor_tensor(sbf[:, 0, st % 4, :], w3[:, 0, :], w3[:, 1, :], ALU.subtract), reads=[w3], writes=[sbf])
                P.dve(lambda e, w4=w4, z=z, st=st: e.tensor_tensor(w4[:, 0, :], z[:, 0, :], sinT[:, st, :], ALU.mult), reads=[z, sinT], writes=[w4])
                P.dve(lambda e, w4=w4, z=z, st=st: e.tensor_tensor(w4[:, 1, :], z[:, 1, :], cosT[:, st, :], ALU.mult), reads=[z, cosT], writes=[w4])
                P.dve(lambda e, w4=w4, st=st: e.tensor_tensor(sbf[:, 1, st % 4, :], w4[:, 0, :], w4[:, 1, :], ALU.add), reads=[w4], writes=[sbf])
                if st % 4 == 3:
                    py = P.psum_bank()
                    pairs = []
                    for s4 in range(4):
                        pairs.append((Cm[0][:, c * 4 + s4, :], sbf[:, 0, s4, :]))
                        pairs.append((Cm[1][:, c * 4 + s4, :], sbf[:, 1, s4, :]))
                    mm_acc(P, py[:, 0:BLK], py, pairs, [Cm[0], Cm[1], sbf])
                    yf, y2 = yfp.next(), y2p.next()
                    P.dve(lambda e, yf=yf, py=py, c=c: e.scalar_tensor_tensor(yf[:], uTf[:, c, :], dsk[:, c:c + 1], py[:, 0:BLK], ALU.mult, ALU.add), reads=[uTf, dsk, py], writes=[yf])
                    P.gp(lambda e, yf=yf, y2=y2: e.tensor_tensor(y2[:], yf[:], yf[:], ALU.mult), reads=[yf], writes=[y2])
                    P.dve(lambda e, y2=y2: e.tensor_scalar(y2[:], y2[:], 0.044715, 1.0, ALU.mult, ALU.add), reads=[y2], writes=[y2])
                    P.gp(lambda e, yf=yf, y2=y2: e.tensor_tensor(y2[:], y2[:], yf[:], ALU.mult), reads=[yf, y2], writes=[y2])
                    P.act(lambda e, y2=y2: e.activation(y2[:], y2[:], AF.Sigmoid, scale=1.5957691216057308), reads=[y2], writes=[y2])
                    P.dve(lambda e, yf=yf, y2=y2: e.tensor_tensor(yf[:], yf[:], y2[:], ALU.mult), reads=[yf, y2], writes=[yf])
                    if c == 0:
                        yb = ybp.next()
                        yfs = [yf]
                    else:
                        yfs.append(yf)
                    P.act(lambda e, yb=yb, yf=yf, c=c: e.activation(yb[:, c, :], yf[:], AF.Copy), reads=[yf], writes=[yb])
            for fc in range(2):
                pz = P.psum_bank()
                mm_acc(P, pz[:, 0:BLK], pz, [(wglu[:, kc, fc * 128:(fc + 1) * 128], yb[:, kc, :]) for kc in range(2)], [wglu, yb])
                sg_ = y2p.next()
                P.act(lambda e, pz=pz, sg_=sg_, fc=fc: e.activation(sg_[:], pz[:, 0:BLK], AF.Sigmoid, bias=bgl[:, fc:fc + 1]), reads=[pz, bgl], writes=[sg_])
                P.dve(lambda e, sg_=sg_, fc=fc, yfs=yfs: e.tensor_tensor(ycat[:, 6 + fc, :], yfs[fc][:], sg_[:], ALU.mult), reads=[yfs[fc], sg_], writes=[ycat])
            for j in range(JB):
                ch = b * JB + j
                tsl = slice(j * 128, (j + 1) * 128)
                qk = qkp.next()
                for (c0, c1) in ((0, 512), (512, 768)):
                    ps = P.psum_bank()
                    mm_acc(P, ps[:, 0:c1 - c0], ps, [(hnT[:, kc, tsl], w_in[:, kc, c0:c1]) for kc in range(8)], [w_in, hnT])
                    P.act(lambda e, ps=ps, c0=c0, c1=c1, qk=qk: e.activation(qk[:, c0:c1], ps[:, 0:c1 - c0], AF.Copy), reads=[ps], writes=[qk])
                r1, r2 = r1p.next(), r2p.next()
                X = qk[:, :].rearrange("p (a h j) -> p a h j", a=12, h=2)
                R1 = r1[:, :].rearrange("p (a h j) -> p a h j", a=12, h=2)
                R2 = r2[:, :].rearrange("p (a h j) -> p a h j", a=12, h=2)
                cosb = rcos[:, ch, :].unsqueeze(1).broadcast_to([128, 12, 32])
                sinb = rsin[:, ch, :].unsqueeze(1).broadcast_to([128, 12, 32])
                for hh in range(2):
                    P.gp(lambda e, hh=hh: e.tensor_tensor(R1[:, :, hh, :], X[:, :, hh, :], cosb, ALU.mult), reads=[qk, rcos], writes=[r1])
                    P.dve(lambda e, hh=hh: e.tensor_tensor(R2[:, :, hh, :], X[:, :, 1 - hh, :], sinb, ALU.mult), reads=[qk, rsin], writes=[r2])
                P.gp(lambda e: e.tensor_tensor(R1[:, :, 0, :], R1[:, :, 0, :], R2[:, :, 0, :], ALU.subtract), reads=[r1, r2], writes=[r1])
                P.gp(lambda e: e.tensor_tensor(R1[:, :, 1, :], R1[:, :, 1, :], R2[:, :, 1, :], ALU.add), reads=[r1, r2], writes=[r1])
                qks = qksp.next()
                P.dve(lambda e, qks=qks: e.tensor_tensor(qks[:, :].rearrange("p (a d) -> p a d", a=12), r1[:, :].rearrange("p (a d) -> p a d", a=12),
                                                        dqk[:, :].unsqueeze(2).broadcast_to([128, 12, 64]), ALU.mult), reads=[r1, dqk], writes=[qks])
                pt = P.psum_tbank()
                for m in range(6):
                    P.pe(lambda e, m=m, pt=pt, qks=qks: e.transpose(pt[:, m * 128:(m + 1) * 128], qks[:, m * 128:(m + 1) * 128], ident[:]), reads=[qks, ident], writes=[pt])
                qkT = qkTp.next()
                P.act(lambda e, pt=pt, qkT=qkT: e.activation(qkT[:, :, :], pt[:, 0:768].rearrange("p (m t) -> p m t", m=6), AF.Copy), reads=[pt], writes=[qkT])
                vs, sg = vsp.next(), sgp.next()
                for (dst, base, fn) in ((vs, 768, AF.Copy), (sg, 1536, AF.Silu)):
                    for (c0, c1) in ((0, 512), (512, 768)):
                        ps = P.psum_bank()
                        mm_acc(P, ps[:, 0:c1 - c0], ps, [(hnT[:, kc, tsl], w_in[:, kc, base + c0:base + c1]) for kc in range(8)], [w_in, hnT])
                        P.act(lambda e, ps=ps, c0=c0, c1=c1, dst=dst, fn=fn: e.activation(dst[:, c0:c1], ps[:, 0:c1 - c0], fn), reads=[ps], writes=[dst])
                Sbf = Sbfp.next()
                for m in range(3):
                    P.dve(lambda e, m=m, Sbf=Sbf: e.tensor_scalar(Sbf[:, m, :], S[:, m, :], gS[:, m:m + 1], None, ALU.mult), reads=[S, gS], writes=[Sbf])
                Ms = []
                for h in range(6):
                    m, hp = h // 2, h % 2
                    psl = slice(hp * 64, (hp + 1) * 64)
                    pS = P.psum_bank()
                    P.pe(lambda e, pS=pS, m=m, psl=psl, qkT=qkT: e.matmul(pS[:, 0:128], qkT[psl, 3 + m, :], qkT[psl, m, :], start=True, stop=True), reads=[qkT], writes=[pS])
                    M = Mp.next()
                    P.dve(lambda e, pS=pS, M=M: e.tensor_tensor(M[:], pS[:, 0:128], maskT[:], ALU.mult), reads=[pS, maskT], writes=[M])
                    Ms.append(M)
                ysb = ysbp.next()
                for (h0, h1) in ((0, 4), (4, 6)):
                    py = P.psum_bank()
                    for h in range(h0, h1):
                        m, hp = h // 2, h % 2
                        psl = slice(hp * 64, (hp + 1) * 64)
                        osl = slice((h - h0) * 128, (h - h0 + 1) * 128)
                        P.pe(lambda e, py=py, osl=osl, h=h, vs=vs, M=Ms[h]: e.matmul(py[:, osl], M[:], vs[:, h * 128:(h + 1) * 128], start=True, stop=False), reads=[Ms[h], vs], writes=[py])
                        P.pe(lambda e, py=py, osl=osl, m=m, psl=psl, qkT=qkT, Sbf=Sbf: e.matmul(py[:, osl], qkT[psl, m, :], Sbf[psl, m, :], start=False, stop=True), reads=[qkT, Sbf], writes=[py])
                    P.act(lambda e, py=py, h0=h0, h1=h1, ysb=ysb: e.activation(ysb[:, h0 * 128:h1 * 128], py[:, 0:(h1 - h0) * 128], AF.Copy), reads=[py], writes=[ysb])
                for m in range(3):
                    pU = P.psum_bank()
                    for hp in range(2):
                        h = 2 * m + hp
                        P.pe(lambda e, pU=pU, hp=hp, h=h, qks=qks, vs=vs: e.matmul(pU[hp * 64:(hp + 1) * 64, 0:128], qks[:, 384 + h * 64:384 + (h + 1) * 64], vs[:, h * 128:(h + 1) * 128], start=True, stop=True),
                             reads=[qks, vs], writes=[pU])
                    P.dve(lambda e, m=m: e.tensor_scalar(S[:, m, :], S[:, m, :], g128[:, m:m + 1], None, ALU.mult), reads=[S, g128], writes=[S])
                    P.dve(lambda e, m=m, pU=pU: e.scalar_tensor_tensor(S[:, m, :], pU[:, 0:128], g127[:, m:m + 1], S[:, m, :], ALU.mult, ALU.add), reads=[pU, g127, S], writes=[S])
                ysq, s6 = ysqp.next(), st6p.next()
                P.gp(lambda e, ysq=ysq, ysb=ysb: e.tensor_tensor(ysq[:], ysb[:], ysb[:], ALU.mult), reads=[ysb], writes=[ysq])
                P.dve(lambda e, ysq=ysq, s6=s6: e.tensor_reduce(s6[:, 0, :], ysq[:, :].rearrange("p (h d) -> p h d", h=6), AX.X, ALU.add), reads=[ysq], writes=[s6])
                P.act(lambda e, s6=s6: e.activation(s6[:, 1, :], s6[:, 0, :], AF.Sqrt, scale=1.0 / 128.0, bias=C.eps[:, 0:1]), reads=[s6, C.eps], writes=[s6])
                P.dve(lambda e, s6=s6: e.reciprocal(s6[:, 2, :], s6[:, 1, :]), reads=[s6], writes=[s6])
                P.gp(lambda e, ysq=ysq, ysb=ysb, sg=sg: e.tensor_tensor(ysq[:], ysb[:], sg[:], ALU.mult), reads=[ysb, sg], writes=[ysq])
                yn = ynp.next()
                P.dve(lambda e, yn=yn, ysq=ysq, s6=s6: e.tensor_tensor(yn[:, :].rearrange("p (h d) -> p h d", h=6), ysq[:, :].rearrange("p (h d) -> p h d", h=6),
                                                                      s6[:, 2, :].unsqueeze(2).broadcast_to([128, 6, 128]), ALU.mult), reads=[ysq, s6], writes=[yn])
                pt2 = P.psum_tbank()
                for m in range(6):
                    P.pe(lambda e, m=m, pt2=pt2, yn=yn: e.transpose(pt2[:, m * 128:(m + 1) * 128], yn[:, m * 128:(m + 1) * 128], ident[:]), reads=[yn, ident], writes=[pt2])
                P.act(lambda e, pt2=pt2, tsl=tsl: e.activation(ycat[:, 0:6, tsl], pt2[:, 0:768].rearrange("p (m t) -> p m t", m=6), AF.Copy), reads=[pt2], writes=[ycat])
            for j in range(JB):
                for hf in range(2):
                    po = P.psum_bank()
                    mm_acc(P, po[:, :], po, [(ycat[:, kc, j * 128:(j + 1) * 128], w_out[:, kc, hf * 512:(hf + 1) * 512]) for kc in range(8)], [ycat, w_out])
                    P.dve(lambda e, po=po, j=j, hf=hf: e.tensor_tensor(hb[:, j, hf * 512:(hf + 1) * 512], po[:, :], hb[:, j, hf * 512:(hf + 1) * 512], ALU.add), reads=[po, hb], writes=[hb])
            P.dma(h_view(h_out.h, b * BLK, JB), hb[:], reads=[hb], writes=[h_out])


BCAST_LHST = False


def phase_mamba_a(P, L, h_in, yn_out, norm_ap, w_in_ap, conv_w_ap, conv_b_ap, dt_bias_ap, a_log_ap, d_ap):
    NCH = L // 128
    with P.phase():
        C = Ctx(P, stage_w=16, stage_n=1, junk_w=1024)
        ident, identf = C.ident, P._identf
        gn = load_gain(P, norm_ap, 8)
        cbcol = load_gain(P, conv_b_ap, 24)
        w_in = P.sbuf("w_in", [128, 8, 5152], BF16)
        Dg = P.sbuf("Dg", [128, 24, 4, 128], BF16)
        cbrow = P.sbuf("cbrow", [1, 3072], BF16)
        ones1 = P.sbuf("ones1", [1, 128], BF16)
        dtb = P.sbuf("dtb", [128, 32], F32)
        abc = P.sbuf("abc", [128, 32], F32)
        dskb = P.sbuf("dskb", [128, 32], F32)
        tri = P.sbuf("tri", [128, 128], F32)
        ustr = P.sbuf("ustr", [128, 128], F32)
        onesf = P.sbuf("onesf", [128, 128], F32)
        stT = P.sbuf("stT", [128, 4, 512], F32)
        stTb = P.sbuf("stTb", [128, 4, 512], BF16)
        with P.phase():
            CS = Ctx(P, stage_w=2048, stage_n=2)
            load_w(P, CS, w_in, w_in_ap, gn)
            cw = P.sbuf("cw", [128, 24, 4], F32)
            for k in range(4):
                P.dma(cw[:, :, k], conv_w_ap[k].rearrange("(ct p) -> p ct", p=128), writes=[cw], allow_slow_non_contiguous=True)
            for ct in range(24):
                for k in range(4):
                    P.act(lambda e, ct=ct, k=k: e.activation(Dg[:, ct, k, :], identf[:], AF.Copy, scale=cw[:, ct, k:k + 1]), reads=[identf, cw], writes=[Dg])
            cbr = P.sbuf("cbr", [1, 3072], F32)
            P.dma(cbr[:], conv_b_ap.unsqueeze(0), writes=[cbr])
            P.dve(lambda e: e.tensor_copy(cbrow[:], cbr[:]), reads=[cbr], writes=[cbrow])
            P.gp(lambda e: e.memset(ones1[:], 1.0), writes=[ones1])
            P.dma(dtb[:], dt_bias_ap.partition_broadcast(128), writes=[dtb])
            P.dma(abc[:], a_log_ap.partition_broadcast(128), writes=[abc])
            P.dma(dskb[:], d_ap.partition_broadcast(128), writes=[dskb])
            P.act(lambda e: e.activation(abc[:], abc[:], AF.Exp), reads=[abc], writes=[abc])
            P.dve(lambda e: e.tensor_scalar(abc[:], abc[:], -1.0, None, ALU.mult), reads=[abc], writes=[abc])
            P.gp(lambda e: e.memset(tri[:], 1.0), writes=[tri])
            P.gp(lambda e: e.affine_select(tri[:], tri[:], [[1, 128]], ALU.is_ge, 0.0, base=0, channel_multiplier=-1), reads=[tri], writes=[tri])
            P.gp(lambda e: e.memset(ustr[:], 1.0), writes=[ustr])
            P.gp(lambda e: e.affine_select(ustr[:], ustr[:], [[-1, 128]], ALU.is_gt, 0.0, base=0, channel_multiplier=1), reads=[ustr], writes=[ustr])
            P.gp(lambda e: e.memset(onesf[:], 1.0), writes=[onesf])
            P.gp(lambda e: e.memset(stT[:], 0.0), writes=[stT])
            P.gp(lambda e: e.memset(stTb[:], 0.0), writes=[stTb])
        P.begin_sched()
        hpool = P.pool_of("hblk", [128, 1024], F32, 2)
        hnTp = P.pool_of("hnT", [128, 8, 128], BF16, 2)
        zsp = P.pool_of("zs", [128, 2048], BF16, 1)
        xprep = P.pool_of("xpre", [128, 24, 131], BF16, 2)
        xsp = P.pool_of("xstok", [128, 2048], BF16, 1)
        btp = P.pool_of("btok", [128, 512], BF16, 1)
        BTp = P.pool_of("BT", [128, 4, 128], BF16, 1)
        CTp = P.pool_of("CT", [128, 4, 128], BF16, 1)
        cbtp = P.pool_of("cbt", [128, 4, 128], F32, 1)
        dtp = P.pool_of("dts", [128, 6, 32], F32, 2)
        csp = P.pool_of("cs", [128, 96], F32, 2)
        ecp = P.pool_of("ecs", [128, 96], F32, 2)
        Wp = P.pool_of("Wh", [128, 128], F32, 4)
        eLp = P.pool_of("eL", [128, 4, 128], F32, 2)
        MTp = P.pool_of("MT", [128, 4, 128], BF16, 4)
        xdtp = P.pool_of("xdt", [128, 512], BF16, 2)
        xwp = P.pool_of("xw", [128, 512], BF16, 2)
        yop = P.pool_of("yo", [128, 512], F32, 2)
        ygp = P.pool_of("yg", [128, 2048], F32, 1)
        ynp = P.pool_of("ynb", [128, 2048], BF16, 1)

        hb_next = hpool.next()
        P.dma(hb_next[:], h_in.h[0:128, :], reads=[h_in], writes=[hb_next])
        xprev = None
        for ch in range(NCH):
            hb = hb_next
            if ch + 1 < NCH:
                hb_next = hpool.next()
                P.dma(hb_next[:], h_in.h[(ch + 1) * 128:(ch + 2) * 128, :], reads=[h_in], writes=[hb_next])
            hnT = hnTp.next()
            norm_tile(P, C, hb[:, :], hb, hnT, 0)
            zs = zsp.next()
            for q in range(4):
                ps = P.psum_bank()
                mm_acc(P, ps[:, :], ps, [(hnT[:, kc, :], w_in[:, kc, q * 512:(q + 1) * 512]) for kc in range(8)], [hnT, w_in])
                P.act(lambda e, ps=ps, q=q, zs=zs: e.activation(zs[:, q * 512:(q + 1) * 512], ps[:, :], AF.Silu), reads=[ps], writes=[zs])
            xpre = xprep.next()
            if xprev is None:
                P.gp(lambda e, xpre=xpre: e.memset(xpre[:, :, 0:3], 0.0), writes=[xpre])
            else:
                P.gp(lambda e, xpre=xpre, xprev=xprev: e.tensor_copy(xpre[:, :, 0:3], xprev[:, :, 128:131]), reads=[xprev], writes=[xpre])
            for q in range(6):
                ps = P.psum_bank()
                for i in range(4):
                    ct = q * 4 + i
                    mm_acc(P, ps[:, i * 128:(i + 1) * 128], ps, [(w_in[:, kc, 2048 + ct * 128:2048 + (ct + 1) * 128], hnT[:, kc, :]) for kc in range(8)], [hnT, w_in])
                P.act(lambda e, ps=ps, q=q, xpre=xpre: e.activation(xpre[:, q * 4:(q + 1) * 4, 3:131], ps[:, :].rearrange("p (i t) -> p i t", i=4), AF.Copy), reads=[ps], writes=[xpre])
            xprev = xpre
            dts = dtp.next()
            ps = P.psum_bank()
            mm_acc(P, ps[:, 0:32], ps, [(hnT[:, kc, :], w_in[:, kc, 5120:5152]) for kc in range(8)], [hnT, w_in])
            P.dve(lambda e, ps=ps, dts=dts: e.tensor_tensor(dts[:, 0, :], ps[:, 0:32], dtb[:], ALU.add), reads=[ps, dtb], writes=[dts])
            P.act(lambda e, dts=dts: e.activation(dts[:, 0, :], dts[:, 0, :], AF.Exp), reads=[dts], writes=[dts])
            P.act(lambda e, dts=dts: e.activation(dts[:, 1, :], dts[:, 0, :], AF.Ln, bias=P.const(1.0)[:, 0:1]), reads=[dts, P.const(1.0)], writes=[dts])
            P.dve(lambda e, dts=dts: e.tensor_tensor(dts[:, 2, :], dts[:, 1, :], abc[:], ALU.mult), reads=[dts, abc], writes=[dts])
            pc = P.psum_bank()
            for i, mat in enumerate((tri, ustr, onesf)):
                P.pe(lambda e, pc=pc, i=i, mat=mat, dts=dts: e.matmul(pc[:, i * 32:(i + 1) * 32], mat[:], dts[:, 2, :], start=True, stop=True), reads=[mat, dts], writes=[pc])
            cs, ecs = csp.next(), ecp.next()
            P.act(lambda e, pc=pc, cs=cs: e.activation(cs[:], pc[:, 0:96], AF.Copy), reads=[pc], writes=[cs])
            P.act(lambda e, pc=pc, ecs=ecs: e.activation(ecs[:], pc[:, 0:96], AF.Exp), reads=[pc], writes=[ecs])
            P.dve(lambda e, dts=dts, ecs=ecs: e.tensor_tensor(dts[:, 3, :], dts[:, 1, :], ecs[:, 32:64], ALU.mult), reads=[dts, ecs], writes=[dts])
            xs = xsp.next()
            for q in range(4):
                ps = P.psum_bank()
                P.pe(lambda e, ps=ps, q=q: e.matmul(ps[:, :], ones1[0:1, :], cbrow[0:1, q * 512:(q + 1) * 512], start=True, stop=False), reads=[ones1, cbrow], writes=[ps])
                for i in range(4):
                    ct = q * 4 + i
                    for k in range(4):
                        last = (i == 3 and k == 3)
                        P.pe(lambda e, ps=ps, i=i, ct=ct, k=k, last=last, xpre=xpre: e.matmul(ps[:, i * 128:(i + 1) * 128], xpre[:, ct, k:k + 128], Dg[:, ct, k, :], start=False, stop=last, skip_group_check=True),
                             reads=[xpre, Dg], writes=[ps])
                P.act(lambda e, ps=ps, q=q, xs=xs: e.activation(xs[:, q * 512:(q + 1) * 512], ps[:, :], AF.Silu), reads=[ps], writes=[xs])
            btok = btp.next()
            ps = P.psum_bank()
            P.pe(lambda e, ps=ps: e.matmul(ps[:, :], ones1[0:1, :], cbrow[0:1, 2048:2560], start=True, stop=False), reads=[ones1, cbrow], writes=[ps])
            for i in range(4):
                ct = 16 + i
                for k in range(4):
                    last = (i == 3 and k == 3)
                    P.pe(lambda e, ps=ps, i=i, ct=ct, k=k, last=last, xpre=xpre: e.matmul(ps[:, i * 128:(i + 1) * 128], xpre[:, ct, k:k + 128], Dg[:, ct, k, :], start=False, stop=last, skip_group_check=True),
                         reads=[xpre, Dg], writes=[ps])
            P.act(lambda e, ps=ps, btok=btok: e.activation(btok[:], ps[:, :], AF.Silu), reads=[ps], writes=[btok])
            BT, CT = BTp.next(), CTp.next()
            for dst, base in ((BT, 16), (CT, 20)):
                ps = P.psum_bank()
                for i in range(4):
                    ct = base + i
                    mm_acc(P, ps[:, i * 128:(i + 1) * 128], ps, [(Dg[:, ct, k, :], xpre[:, ct, k:k + 128]) for k in range(4)], [xpre, Dg])
                for i in range(4):
                    ct = base + i
                    P.act(lambda e, ps=ps, i=i, ct=ct, dst=dst: e.activation(dst[:, i, :], ps[:, i * 128:(i + 1) * 128], AF.Silu, bias=cbcol[:, ct:ct + 1]), reads=[ps, cbcol], writes=[dst])
            ps = P.psum_bank()
            for g in range(4):
                P.pe(lambda e, ps=ps, g=g, BT=BT, CT=CT: e.matmul(ps[:, g * 128:(g + 1) * 128], BT[:, g, :], CT[:, g, :], start=True, stop=True), reads=[BT, CT], writes=[ps])
            cbt = cbtp.next()
            P.dve(lambda e, ps=ps, cbt=cbt: e.tensor_tensor(cbt[:, :, :], ps[:, :].rearrange("p (g t) -> p g t", g=4), tri[:, :].unsqueeze(1).broadcast_to([128, 4, 128]), ALU.mult),
                  reads=[ps, tri], writes=[cbt])
            yg = ygp.next()
            ygr = [Buf(yg.h, "yg_g%d" % g_) for g_ in range(4)]
            for g in range(4):
                gs = slice(g * 512, (g + 1) * 512)
                xdt, xw = xdtp.next(), xwp.next()
                P.dve(lambda e, xdt=xdt, xs=xs, dts=dts, g=g, gs=gs: e.tensor_tensor(xdt[:, :].rearrange("p (h d) -> p h d", h=8), xs[:, gs].rearrange("p (h d) -> p h d", h=8),
                                                                          dts[:, 1, g * 8:(g + 1) * 8].unsqueeze(2).broadcast_to([128, 8, 64]), ALU.mult), reads=[xs, dts], writes=[xdt])
                P.dve(lambda e, xw=xw, xs=xs, dts=dts, g=g, gs=gs: e.tensor_tensor(xw[:, :].rearrange("p (h d) -> p h d", h=8), xs[:, gs].rearrange("p (h d) -> p h d", h=8),
                                                                         dts[:, 3, g * 8:(g + 1) * 8].unsqueeze(2).broadcast_to([128, 8, 64]), ALU.mult), reads=[xs, dts], writes=[xw])
                MTs = []
                for half in range(2):
                    pr = P.psum_bank()
                    for i in range(4):
                        h = g * 8 + half * 4 + i
                        Wh = Wp.next()
                        P.act(lambda e, Wh=Wh, h=h, dts=dts: e.activation(Wh[:], ustr[:], AF.Copy, scale=dts[:, 2, h:h + 1]), reads=[ustr, dts], writes=[Wh])
                        P.pe(lambda e, pr=pr, i=i, Wh=Wh: e.matmul(pr[:, i * 128:(i + 1) * 128], Wh[:], tri[:], start=True, stop=True), reads=[tri, Wh], writes=[pr])
                    eL = eLp.next()
                    P.act(lambda e, pr=pr, eL=eL: e.activation(eL[:, :, :], pr[:, :].rearrange("p (i t) -> p i t", i=4), AF.Exp), reads=[pr], writes=[eL])
                    MT = MTp.next()
                    P.dve(lambda e, MT=MT, eL=eL, g=g, cbt=cbt: e.tensor_tensor(MT[:, :, :], eL[:, :, :], cbt[:, g, :].unsqueeze(1).broadcast_to([128, 4, 128]), ALU.mult), reads=[eL, cbt], writes=[MT])
                    MTs.append(MT)
                py = P.psum_bank()
                for hh in range(8):
                    h = g * 8 + hh
                    P.pe(lambda e, py=py, hh=hh, h=h, MT=MTs[hh // 4], xdt=xdt: e.matmul(py[:, hh * 64:(hh + 1) * 64], MT[:, hh % 4, :], xdt[:, hh * 64:(hh + 1) * 64], start=True, stop=True),
                         reads=[MTs[hh // 4], xdt], writes=[py])
                po = P.psum_bank()
                P.pe(lambda e, po=po, g=g, CT=CT: e.matmul(po[:, :], CT[:, g, :], stTb[:, g, :], start=True, stop=True), reads=[CT, stTb], writes=[po])
                yo = yop.next()
                P.dve(lambda e, yo=yo, po=po, g=g, ecs=ecs: e.tensor_tensor(yo[:, :].rearrange("p (h d) -> p h d", h=8), po[:, :].rearrange("p (h d) -> p h d", h=8),
                                                                      ecs[:, g * 8:(g + 1) * 8].unsqueeze(2).broadcast_to([128, 8, 64]), ALU.mult), reads=[po, ecs], writes=[yo])
                P.dve(lambda e, yo=yo, py=py: e.tensor_tensor(yo[:], py[:, :], yo[:], ALU.add), reads=[py, yo], writes=[yo])
                yd = yop.next()
                P.dve(lambda e, yd=yd, xs=xs, g=g, gs=gs: e.tensor_tensor(yd[:, :].rearrange("p (h d) -> p h d", h=8), xs[:, gs].rearrange("p (h d) -> p h d", h=8),
                                                                    dskb[:, g * 8:(g + 1) * 8].unsqueeze(2).broadcast_to([128, 8, 64]), ALU.mult), reads=[xs, dskb], writes=[yd])
                P.dve(lambda e, yd=yd, yo=yo: e.tensor_tensor(yd[:], yd[:], yo[:], ALU.add), reads=[yd, yo], writes=[yd])
                P.dve(lambda e, yd=yd, yg=yg, zs=zs, gs=gs: e.tensor_tensor(yg[:, gs], yd[:], zs[:, gs], ALU.mult), reads=[yd, zs], writes=[ygr[g]])
                pu = P.psum_bank()
                P.pe(lambda e, pu=pu, g=g, btok=btok, xw=xw, gs=gs: e.matmul(pu[:, :], btok[:, g * 128:(g + 1) * 128], xw[:, :], start=True, stop=True), reads=[btok, xw], writes=[pu])
                P.dve(lambda e, g=g, ecs=ecs: e.tensor_tensor(stT[:, g, :].rearrange("p (h d) -> p h d", h=8), stT[:, g, :].rearrange("p (h d) -> p h d", h=8),
                                                            ecs[:, 64 + g * 8:64 + (g + 1) * 8].unsqueeze(2).broadcast_to([128, 8, 64]), ALU.mult), reads=[stT, ecs], writes=[stT])
                P.dve(lambda e, g=g, pu=pu: e.tensor_tensor(stT[:, g, :], pu[:, :], stT[:, g, :], ALU.add), reads=[pu, stT], writes=[stT])
                P.act(lambda e, g=g: e.activation(stTb[:, g, :], stT[:, g, :], AF.Copy), reads=[stT], writes=[stTb])
            ss = C.small.next()
            yn = ynp.next()
            P.act(lambda e, yn=yn, ss=ss, yg=yg: e.activation(yn[:], yg[:], AF.Square, accum_out=ss[:, 0:1]), reads=ygr, writes=[yn, ss])
            P.act(lambda e, ss=ss: e.activation(ss[:, 1:2], ss[:, 0:1], AF.Sqrt, scale=1.0 / 2048.0, bias=C.eps[:, 0:1]), reads=[ss, C.eps], writes=[ss])
            P.dve(lambda e, ss=ss: e.reciprocal(ss[:, 2:3], ss[:, 1:2]), reads=[ss], writes=[ss])
            P.dve(lambda e, yn=yn, yg=yg, ss=ss: e.tensor_scalar(yn[:], yg[:], ss[:, 2:3], None, ALU.mult), reads=ygr + [ss], writes=[yn])
            P.dma(yn_out.h[ch * 128:(ch + 1) * 128, :], yn[:], reads=[yn], writes=[yn_out])


def phase_mamba_b(P, L, h_in, h_out, yn_in, odnorm_ap, w_out_ap):
    NCH = L // 128
    with P.phase():
        C = Ctx(P, stage_w=2048, stage_n=2)
        ident = C.ident
        go = load_gain(P, odnorm_ap, 16)
        w_out = P.sbuf("w_out", [128, 16, 1024], BF16)
        load_w(P, C, w_out, w_out_ap, go)
        hpool = P.pool_of("hblk", [128, 1024], F32, 3)
        ynp = P.pool_of("ynl", [128, 2048], BF16, 3)
        ynTp = P.pool_of("ynT", [128, 16, 128], BF16, 2)
        nxt = None
        P.begin_sched()

        def load(ch):
            hb, yn = hpool.next(), ynp.next()
            P.dma(hb[:], h_in.h[ch * 128:(ch + 1) * 128, :], reads=[h_in], writes=[hb])
            P.dma(yn[:], yn_in.h[ch * 128:(ch + 1) * 128, :], reads=[yn_in], writes=[yn])
            return hb, yn
        nxt = load(0)
        for ch in range(NCH):
            hb, yn = nxt
            if ch + 1 < NCH:
                nxt = load(ch + 1)
            ynT = ynTp.next()
            for c0 in (0, 8):
                pt = P.psum_tbank()
                for c in range(8):
                    P.pe(lambda e, c=c, c0=c0, pt=pt, yn=yn: e.transpose(pt[:, c * 128:(c + 1) * 128], yn[:, (c0 + c) * 128:(c0 + c + 1) * 128], ident[:]), reads=[yn, ident], writes=[pt])
                P.act(lambda e, pt=pt, c0=c0, ynT=ynT: e.activation(ynT[:, c0:c0 + 8, :], pt[:].rearrange("p (c t) -> p c t", c=8), AF.Copy), reads=[pt], writes=[ynT])
            for hf in range(2):
                po = P.psum_bank()
                mm_acc(P, po[:, :], po, [(ynT[:, kc, :], w_out[:, kc, hf * 512:(hf + 1) * 512]) for kc in range(16)], [ynT, w_out])
                P.dve(lambda e, po=po, hf=hf, hb=hb: e.tensor_tensor(hb[:, hf * 512:(hf + 1) * 512], po[:, :], hb[:, hf * 512:(hf + 1) * 512], ALU.add), reads=[po, hb], writes=[hb])
            P.dma(h_out.h[ch * 128:(ch + 1) * 128, :], hb[:], reads=[hb], writes=[h_out])


W_SHAPES = {
    "mem_norm": [1024], "norm_mix": [2, 1024], "norm_xattn": [2, 1024], "norm_ffn": [2, 1024],
    "xa_wq": [2, 1024, 1024], "xa_wk": [2, 1024, 1024], "xa_wv": [2, 1024, 1024], "xa_wo": [2, 1024, 1024],
    "ev_w_in": [1, 1024, 2560], "ev_s5_lam_re": [1, 16, 64], "ev_s5_lam_im": [1, 16, 64], "ev_s5_log_dt": [1, 16],
    "ev_s5_b_re": [1, 16, 64, 16], "ev_s5_b_im": [1, 16, 64, 16], "ev_s5_c_re": [1, 16, 16, 64], "ev_s5_c_im": [1, 16, 16, 64],
    "ev_s5_d": [1, 256], "ev_s5_w_glu": [1, 256, 256], "ev_s5_b_glu": [1, 256], "ev_w_out": [1, 1024, 1024],
    "ev_ffn_w1": [1, 1024, 2816], "ev_ffn_w3": [1, 1024, 2816], "ev_ffn_w2": [1, 2816, 1024],
    "od_w_in": [1, 1024, 5152], "od_conv_w": [1, 4, 3072], "od_conv_b": [1, 3072], "od_dt_bias": [1, 32], "od_a_log": [1, 32],
    "od_d": [1, 32], "od_norm": [1, 2048], "od_w_out": [1, 2048, 1024], "od_router": [1, 1024, 8],
    "od_moe_w1": [1, 8, 1024, 3584], "od_moe_w3": [1, 8, 1024, 3584], "od_moe_w2": [1, 8, 3584, 1024], "final_norm": [1024],
}


def build_program(L, phases=None):
    nc = bass.Bass("TRN2", target_bir_lowering=False)
    P = Prog(nc)
    x = P.dram("x", [L, 1024], F32, kind="ExternalInput")
    mem = nc.dram_tensor("mem", [256, 1024], F32, kind="ExternalInput").ap()
    pos = nc.dram_tensor("positions", [L], I32, kind="ExternalInput").ap()
    out = P.dram("out", [L, 1024], F32, kind="ExternalOutput")
    w = {n: nc.dram_tensor(n, s, F32, kind="ExternalInput").ap() for n, s in W_SHAPES.items()}
    hA = P.dram("hA", [L, 1024], F32)
    hB = P.dram("hB", [L, 1024], F32)
    yn = P.dram("yn", [L, 2048], BF16)
    steps = [
        lambda i, o: phase_even_mixer(P, L, i, o, pos, w["norm_mix"][0], w["ev_w_in"][0], w["ev_s5_lam_re"][0], w["ev_s5_lam_im"][0], w["ev_s5_log_dt"][0],
                                      w["ev_s5_b_re"][0], w["ev_s5_b_im"][0], w["ev_s5_c_re"][0], w["ev_s5_c_im"][0], w["ev_s5_d"][0], w["ev_s5_w_glu"][0],
                                      w["ev_s5_b_glu"][0], w["ev_w_out"][0]),
        lambda i, o: phase_xattn(P, L, i, o, mem, w["mem_norm"], w["norm_xattn"][0], w["xa_wq"][0], w["xa_wk"][0], w["xa_wv"][0], w["xa_wo"][0]),
        lambda i, o: phase_ffn(P, L, i, o, w["norm_ffn"][0], [w["ev_ffn_w1"][0]], [w["ev_ffn_w3"][0]], [w["ev_ffn_w2"][0]], 2816),
        lambda i, o: (phase_mamba_a(P, L, i, yn, w["norm_mix"][1], w["od_w_in"][0], w["od_conv_w"][0], w["od_conv_b"][0], w["od_dt_bias"][0], w["od_a_log"][0], w["od_d"][0]),
                      phase_mamba_b(P, L, i, o, yn, w["od_norm"][0], w["od_w_out"][0])),
        lambda i, o: phase_xattn(P, L, i, o, mem, w["mem_norm"], w["norm_xattn"][1], w["xa_wq"][1], w["xa_wk"][1], w["xa_wv"][1], w["xa_wo"][1]),
        lambda i, o: phase_ffn(P, L, i, None, w["norm_ffn"][1], [w["od_moe_w1"][0][e] for e in range(8)], [w["od_moe_w3"][0][e] for e in range(8)],
                               [w["od_moe_w2"][0][e] for e in range(8)], 3584, router_ap=w["od_router"][0], final_norm_ap=w["final_norm"], out_ap=o.h),
    ]
    if phases is None:
        phases = list(range(len(steps)))
    cur = x
    scratch = [hA, hB]
    for n, pi in enumerate(phases):
        dst = out if n == len(phases) - 1 else scratch[n % 2]
        steps[pi](cur, dst)
        cur = dst
    P.finish()
    return nc, P


def kernel(_phases=None, **inputs):
    x = np.asarray(inputs["x"], dtype=np.float32)
    B, L, _ = x.shape
    nc, P = build_program(L, _phases)
    shared = {n: np.ascontiguousarray(np.asarray(inputs[n], dtype=np.float32)) for n in W_SHAPES}
    mem = np.asarray(inputs["mem"], dtype=np.float32)
    pos = np.asarray(inputs["positions"], dtype=np.int32)
    in_maps = []
    for b in range(B):
        m = dict(shared)
        m["x"] = np.ascontiguousarray(x[b])
        m["mem"] = np.ascontiguousarray(mem[b])
        m["positions"] = np.ascontiguousarray(pos[b])
        in_maps.append(m)
    res = run_bass_kernel_spmd(nc, in_maps, core_ids=list(range(B)))
    return np.stack([np.asarray(r["out"], dtype=np.float32) for r in res.results], axis=0)
```
